# Optimizing a Trainium2 kernel written in Bass

```python
import jax, jax.numpy as jnp
from jax import lax
import numpy as np


D_MODEL = 1024
BATCH = 16
SEQ = 2048
DEPTH = 2

RET_HEADS = 4
RET_DK = 128
RET_DV = 256
RET_CHUNK = 128
GLA_HEADS = 4
GLA_DK = 128
GLA_DV = 256
GLA_RANK = 16
GLA_TAU = 16.0
GLA_LOG_GATE_MIN = -1.0
GLA_CHUNK = 64
SWA_Q_HEADS = 16
SWA_KV_HEADS = 4
SWA_HEAD_DIM = 64
SWA_WINDOW = 128
N_BRANCH = 3
BRANCH_WIDTH = RET_HEADS * RET_DV
D_FF = -(-8 * D_MODEL // (3 * 256)) * 256
NORM_EPS = 1e-6
GN_EPS = 1e-5

IN_SPLITS = (
    RET_HEADS * RET_DK, RET_HEADS * RET_DK, RET_HEADS * RET_DV, RET_HEADS * RET_DV,
    GLA_HEADS * GLA_DK, GLA_HEADS * GLA_DK, GLA_HEADS * GLA_DV, GLA_HEADS * GLA_DV, GLA_RANK,
    SWA_Q_HEADS * SWA_HEAD_DIM, SWA_KV_HEADS * SWA_HEAD_DIM, SWA_KV_HEADS * SWA_HEAD_DIM,
    N_BRANCH * D_MODEL,
)
D_IN = sum(IN_SPLITS)

kernel_name = "hybrid_retnet_gla_swa_gated_block"


def _split_points():
    return [int(o) for o in np.cumsum(IN_SPLITS)[:-1]]


def rmsnorm(x, w):
    xf = x.astype(jnp.float32)
    y = xf * lax.rsqrt(jnp.mean(xf * xf, axis=-1, keepdims=True) + NORM_EPS)
    return (y * w.astype(jnp.float32)).astype(x.dtype)


def head_groupnorm(o, w, b):
    B, S, H, dv = o.shape
    of = o.astype(jnp.float32)
    mu = jnp.mean(of, axis=-1, keepdims=True)
    var = jnp.mean(jnp.square(of - mu), axis=-1, keepdims=True)
    y = ((of - mu) * lax.rsqrt(var + GN_EPS)).reshape(B, S, H * dv)
    return y * w.astype(jnp.float32) + b.astype(jnp.float32)


def head_rmsnorm(o, w):
    B, S, H, dv = o.shape
    of = o.astype(jnp.float32)
    y = of * lax.rsqrt(jnp.mean(of * of, axis=-1, keepdims=True) + NORM_EPS)
    return y.reshape(B, S, H * dv) * w.astype(jnp.float32)


def retention(q, k, v):
    B, S, H, dk = q.shape
    dv = v.shape[-1]
    C = RET_CHUNK
    N = S // C
    log_g = jnp.log1p(-jnp.exp2(-5.0 - jnp.arange(H, dtype=jnp.float32)))
    q = (q * dk ** -0.5).reshape(B, N, C, H, dk)
    k = k.reshape(B, N, C, H, dk)
    v = v.reshape(B, N, C, H, dv)
    pos = jnp.arange(C, dtype=jnp.float32)
    rel = pos[:, None] - pos[None, :]
    decay = jnp.exp(jnp.where((rel >= 0)[None], rel[None] * log_g[:, None, None], -jnp.inf))
    scores = jnp.einsum('bnihd,bnjhd->bnhij', q, k) * decay
    intra = jnp.einsum('bnhij,bnjhe->bnihe', scores, v)
    zeta = jnp.exp((C - 1 - pos)[None, :] * log_g[:, None])
    kv = jnp.einsum('bnjhd,bnjhe,hj->nbhde', k, v, zeta)
    gamma_c = jnp.exp(C * log_g)[None, :, None, None]

    def step(R, kv_n):
        return gamma_c * R + kv_n, R

    _, states = lax.scan(step, jnp.zeros(kv.shape[1:], kv.dtype), kv)
    xi = jnp.exp((pos + 1.0)[:, None] * log_g[None, :])
    inter = jnp.einsum('bnihd,nbhde->bnihe', q * xi[:, :, None], states)
    return (intra + inter).reshape(B, S, H, dv)


def gla(q, k, v, log_a):
    B, S, H, dk = q.shape
    dv = v.shape[-1]
    C = GLA_CHUNK
    N = S // C
    q = (q * dk ** -0.5).reshape(B, N, C, H, dk)
    k = k.reshape(B, N, C, H, dk)
    v = v.reshape(B, N, C, H, dv)
    b = jnp.cumsum(log_a.reshape(B, N, C, H, dk), axis=2)
    b_last = b[:, :, -1:]
    q_dec = q * jnp.exp(b)
    k_inv = k * jnp.exp(-b)
    causal = jnp.tril(jnp.ones((C, C), dtype=bool))
    A = jnp.where(causal, jnp.einsum('bnihd,bnjhd->bnhij', q_dec, k_inv), 0.0)
    intra = jnp.einsum('bnhij,bnjhe->bnihe', A, v)
    kv = jnp.einsum('bnjhd,bnjhe->nbhde', k * jnp.exp(b_last - b), v)
    chunk_decay = jnp.moveaxis(jnp.exp(b_last[:, :, 0]), 1, 0)

    def step(St, inp):
        kv_n, a_n = inp
        return a_n[..., None] * St + kv_n, St

    _, states = lax.scan(step, jnp.zeros(kv.shape[1:], kv.dtype), (kv, chunk_decay))
    inter = jnp.einsum('bnihd,nbhde->bnihe', q_dec, states)
    return (intra + inter).reshape(B, S, H, dv)


def swa_sinks(q, k, v, sinks):
    B, S, Hq, dh = q.shape
    Hkv = k.shape[2]
    G = Hq // Hkv
    W = SWA_WINDOW
    N = S // W
    slopes = jnp.exp2(-8.0 * jnp.arange(1, Hq + 1, dtype=jnp.float32) / Hq).reshape(Hkv, G)
    qb = q.reshape(B, N, W, Hkv, G, dh)
    kb = k.reshape(B, N, W, Hkv, dh)
    vb = v.reshape(B, N, W, Hkv, dh)

    def with_prev(t):
        prev = jnp.pad(t[:, :-1], ((0, 0), (1, 0), (0, 0), (0, 0), (0, 0)))
        return jnp.concatenate([prev, t], axis=2)

    kk, vv = with_prev(kb), with_prev(vb)
    scores = jnp.einsum('bnqhgd,bnkhd->bnhgqk', qb, kk).astype(jnp.float32) * dh ** -0.5
    qpos = W + jnp.arange(W)
    kpos = jnp.arange(2 * W)
    dist = qpos[:, None] - kpos[None, :]
    blk = jnp.arange(N)
    valid = ((dist >= 0) & (dist < W))[None] & ((blk[:, None, None] > 0) | (kpos[None, None, :] >= W))
    logits = scores - slopes[:, :, None, None] * dist.astype(jnp.float32)
    logits = jnp.where(valid[None, :, None, None], logits, -jnp.inf)
    sink = sinks.astype(jnp.float32).reshape(Hkv, G)[:, :, None, None]
    m = jnp.maximum(jnp.max(logits, axis=-1, keepdims=True), sink)
    p = jnp.exp(logits - m)
    probs = p / (jnp.sum(p, axis=-1, keepdims=True) + jnp.exp(sink - m))
    out = jnp.einsum('bnhgqk,bnkhd->bnqhgd', probs.astype(v.dtype), vv)
    return out.reshape(B, S, Hq * dh)


def setup_inputs(seed: int = 0) -> dict:
    key = jax.random.key(seed)
    ks = jax.random.split(key, 17)
    f32 = jnp.float32

    def nrm(k, shape, scale):
        return jax.random.normal(k, shape, f32) * scale

    def gain(k, n):
        return 1.0 + nrm(k, (DEPTH, n), 0.05)

    return {
        'x': nrm(ks[0], (BATCH, SEQ, D_MODEL), 1.0),
        'norm_mix_pre': gain(ks[1], D_MODEL),
        'norm_mix_post': gain(ks[2], D_MODEL),
        'w_in': nrm(ks[3], (DEPTH, D_MODEL, D_IN), D_MODEL ** -0.5),
        'ret_norm_w': gain(ks[4], RET_HEADS * RET_DV),
        'ret_norm_b': nrm(ks[5], (DEPTH, RET_HEADS * RET_DV), 0.02),
        'gla_w_alpha2': nrm(ks[6], (DEPTH, GLA_RANK, GLA_HEADS * GLA_DK), GLA_RANK ** -0.5),
        'gla_b_alpha': nrm(ks[7], (DEPTH, GLA_HEADS * GLA_DK), 0.1),
        'gla_norm_w': gain(ks[8], GLA_HEADS * GLA_DV),
        'attn_sinks': nrm(ks[9], (DEPTH, SWA_Q_HEADS), 0.5),
        'w_branch': nrm(ks[10], (DEPTH, N_BRANCH, BRANCH_WIDTH, D_MODEL), BRANCH_WIDTH ** -0.5),
        'w_out': nrm(ks[11], (DEPTH, D_MODEL, D_MODEL), D_MODEL ** -0.5),
        'norm_ffn_pre': gain(ks[12], D_MODEL),
        'norm_ffn_post': gain(ks[13], D_MODEL),
        'ffn_w_gate': nrm(ks[14], (DEPTH, D_MODEL, D_FF), D_MODEL ** -0.5),
        'ffn_w_up': nrm(ks[15], (DEPTH, D_MODEL, D_FF), D_MODEL ** -0.5),
        'ffn_w_down': nrm(ks[16], (DEPTH, D_FF, D_MODEL), D_FF ** -0.5),
    }


def reference(x, norm_mix_pre, norm_mix_post, w_in, ret_norm_w, ret_norm_b, gla_w_alpha2,
              gla_b_alpha, gla_norm_w, attn_sinks, w_branch, w_out, norm_ffn_pre, norm_ffn_post,
              ffn_w_gate, ffn_w_up, ffn_w_down):
    B, S, _ = x.shape
    split_points = _split_points()
    for l in range(DEPTH):
        h = rmsnorm(x, norm_mix_pre[l])
        proj = h @ w_in[l]
        (rq, rk, rv, rg, gq, gk, gv, gg, ga, sq, sk, sv, gate_logits) = jnp.split(proj, split_points, axis=-1)

        o_ret = retention(rq.reshape(B, S, RET_HEADS, RET_DK), rk.reshape(B, S, RET_HEADS, RET_DK),
                          rv.reshape(B, S, RET_HEADS, RET_DV))
        o_ret = (head_groupnorm(o_ret, ret_norm_w[l], ret_norm_b[l]) * jax.nn.silu(rg)).astype(x.dtype)

        z = (ga @ gla_w_alpha2[l] + gla_b_alpha[l]).astype(jnp.float32)
        log_a = jnp.maximum(jax.nn.log_sigmoid(z) / GLA_TAU, GLA_LOG_GATE_MIN)
        o_gla = gla(gq.reshape(B, S, GLA_HEADS, GLA_DK), gk.reshape(B, S, GLA_HEADS, GLA_DK),
                    gv.reshape(B, S, GLA_HEADS, GLA_DV), log_a.reshape(B, S, GLA_HEADS, GLA_DK))
        o_gla = (head_rmsnorm(o_gla, gla_norm_w[l]) * jax.nn.silu(gg)).astype(x.dtype)

        o_swa = swa_sinks(sq.reshape(B, S, SWA_Q_HEADS, SWA_HEAD_DIM),
                          sk.reshape(B, S, SWA_KV_HEADS, SWA_HEAD_DIM),
                          sv.reshape(B, S, SWA_KV_HEADS, SWA_HEAD_DIM), attn_sinks[l]).astype(x.dtype)

        branches = jnp.stack([o_ret, o_gla, o_swa], axis=2)
        projected = jnp.einsum('bsnc,ncd->bsnd', branches, w_branch[l])
        gates = jax.nn.sigmoid(gate_logits.reshape(B, S, N_BRANCH, D_MODEL))
        merged = jnp.sum(gates * projected, axis=2)
        x = x + rmsnorm(merged @ w_out[l], norm_mix_post[l])

        h = rmsnorm(x, norm_ffn_pre[l])
        f = (jax.nn.silu(h @ ffn_w_gate[l]) * (h @ ffn_w_up[l])) @ ffn_w_down[l]
        x = x + rmsnorm(f, norm_ffn_post[l])
    return x
```

```python
import contextlib
import os
import numpy as np
import ml_dtypes
import concourse.bass as bass
import concourse.mybir as mybir
from concourse.bass_utils import run_bass_kernel_spmd

F32 = mybir.dt.float32
BF16 = mybir.dt.bfloat16
AF = mybir.ActivationFunctionType
ALU = mybir.AluOpType

PE, ACT, DVE, POOL, SP = "pe", "act", "dve", "pool", "sp"
ENGS = (PE, ACT, DVE, POOL, SP)

D = 1024
SEQ = 2048
L = 2
T = 512
NT = T // 128
DIN = 10768
DFF = 2816
C_RQ, C_RK, C_RV, C_RG = 0, 512, 1024, 2048
C_GQ, C_GK, C_GV, C_GG, C_GA = 3072, 3584, 4096, 5120, 6144
C_SQ, C_SK, C_SV, C_GATE = 6160, 7184, 7440, 7696
NPAR = 64
WPL = (D * DIN + 3 * D * D + D * D + 3 * D * DFF) // 128
P_MIXPRE, P_MIXPOST, P_FFNPRE, P_FFNPOST, P_RETW, P_RETB, P_GLAW, P_SINK = 0, 8, 16, 24, 32, 40, 48, 56


class Buf:
    __slots__ = ("name", "w", "r")

    def __init__(self, name=""):
        self.name = name
        self.w = None
        self.r = {}


class PsH:
    __slots__ = ("bank", "gen", "t", "buf")

    def __init__(self, bank, gen, t, buf):
        self.bank, self.gen, self.t, self.buf = bank, gen, t, buf


class Sched:
    def __init__(self, nc):
        self.nc = nc
        self.q = {e: [] for e in ENGS}
        self.cnt = {e: 0 for e in ENGS}
        self.seen = {e: {} for e in ENGS}
        self.dma_sems = []
        self.nops = 0
        self.nwaits = 0
        self.psgen = {}
        self.phase = ''
        self.pe_phase = []

    def new_dma_sem(self, name):
        self.dma_sems.append(name)
        self.cnt[name] = 0
        return name

    def _buf(self, b):
        if isinstance(b, PsH):
            assert self.psgen[b.bank] == b.gen, "stale PSUM handle bank %d" % b.bank
            return b.buf
        return b

    def op(self, eng, fn, reads=(), writes=(), dma_sem=None):
        need = {}

        def req(dep, skip_own):
            if dep is None:
                return
            k, c = dep
            if skip_own and k == eng and dma_sem is None and eng == PE:
                return
            if c > need.get(k, 0):
                need[k] = c

        ps_reads = [self._buf(b) for b in reads if isinstance(b, PsH)]
        reads = [self._buf(b) for b in reads]
        writes = [self._buf(b) for b in writes]
        for b in reads:
            req(b.w, False)
        for b in ps_reads:
            for k, c in b.r.items():
                if k != eng:
                    req((k, c), False)
        for b in writes:
            req(b.w, True)
            for k, c in b.r.items():
                req((k, c), True)
        waits = []
        seen = self.seen[eng]
        for k, c in need.items():
            if seen.get(k, 0) < c:
                seen[k] = c
                waits.append((k, c))
        if dma_sem is None:
            key = eng
            self.cnt[eng] += 1
            c = self.cnt[eng]
        else:
            key = dma_sem
            self.cnt[dma_sem] += 16
            c = self.cnt[dma_sem]
        for b in writes:
            b.w = (key, c)
            b.r = {}
        for b in reads:
            if b.r.get(key, 0) < c:
                b.r[key] = c
        self.q[eng].append((waits, fn, key))
        if eng == PE:
            self.pe_phase.append(self.phase)
        self.nops += 1
        self.nwaits += len(waits)
        return (key, c)

    def final_wait(self, eng, deps):
        waits = []
        for k, c in deps:
            if self.seen[eng].get(k, 0) < c:
                self.seen[eng][k] = c
                waits.append((k, c))
        self.q[eng].append((waits, None, None))

    def emit(self):
        nc = self.nc
        with contextlib.ExitStack() as st:
            sems = {}
            for k in list(ENGS) + self.dma_sems:
                sems[k] = st.enter_context(nc.semaphore("s_" + k))
            block = st.enter_context(nc.Block())

            def run(engname):
                def body(e):
                    for waits, fn, key in self.q[engname]:
                        if fn is None:
                            for k, c in waits:
                                e.wait_ge(sems[k], c)
                            continue
                        for k, c in waits[1:]:
                            e.wait_ge(sems[k], c)
                        ins = fn(e)
                        if waits:
                            ins._wait_ge(sems[waits[0][0]], waits[0][1])
                        ins.then_inc(sems[key], 1 if key in ENGS else 16)
                return body

            block.tensor(run(PE))
            block.scalar(run(ACT))
            block.vector(run(DVE))
            block.gpsimd(run(POOL))
            block.sync(run(SP))


def make_consts():
    f = np.float32
    c = {}
    c["ident_f"] = np.eye(128, dtype=f)
    c["ident_b"] = np.eye(128, dtype=f).astype(ml_dtypes.bfloat16)
    c["ones_b"] = np.ones((128, 128), dtype=f).astype(ml_dtypes.bfloat16)
    H = 4
    logg = np.log1p(-np.exp2(-5.0 - np.arange(H, dtype=np.float64)))
    pos = np.arange(128, dtype=np.float64)
    scale = 128.0 ** -0.5
    rel = pos[None, :] - pos[:, None]
    DTm = np.zeros((128, H, 128), dtype=np.float64)
    for h in range(H):
        DTm[:, h, :] = np.where(rel >= 0, scale * np.exp(rel * logg[h]), 0.0)
    c["ret_dt"] = DTm.astype(f).reshape(128, H * 128)
    XI = np.zeros((128, H, 128))
    ZT = np.zeros((128, H, 128))
    for h in range(H):
        XI[:, h, :] = (scale * np.exp((pos + 1.0) * logg[h]))[None, :]
        ZT[:, h, :] = np.exp((127.0 - pos) * logg[h])[None, :]
    c["ret_xi"] = XI.astype(f).reshape(128, H * 128)
    c["ret_zt"] = ZT.astype(f).reshape(128, H * 128)
    blk = (np.arange(128) // 64)
    same = blk[:, None] == blk[None, :]
    jj = np.arange(128)[:, None]
    ii = np.arange(128)[None, :]
    c["gla_tri"] = (same & (jj <= ii)).astype(f)
    c["gla_sl"] = (same & (jj > ii)).astype(f)
    Hq = 16
    slopes = np.exp2(-8.0 * np.arange(1, Hq + 1, dtype=np.float64) / Hq)
    M = np.zeros((128, Hq, 2, 128))
    k = np.arange(128)[:, None].astype(np.float64)
    q = np.arange(128)[None, :].astype(np.float64)
    for h in range(Hq):
        d1 = q - k
        M[:, h, 1, :] = np.where(d1 >= 0, np.exp(-slopes[h] * d1), 0.0)
        d0 = q + 128.0 - k
        M[:, h, 0, :] = np.where(d0 < 128, np.exp(-slopes[h] * d0), 0.0)
    M2 = M.reshape(128, 8, 2, 2, 128).transpose(0, 2, 1, 3, 4)
    c["swa_mask"] = np.ascontiguousarray(M2).astype(f).astype(ml_dtypes.bfloat16).reshape(128, Hq * 2 * 128)
    return c


RET_G128 = [float(np.exp(128.0 * np.log1p(-np.exp2(-5.0 - h)))) for h in range(4)]


def col(v):
    return np.ascontiguousarray(np.asarray(v, dtype=np.float32).reshape(8, 128).T)


def build(n_seq=2, n_mt=4, n_layers=2, dbg=None):
    nc = bass.Bass("TRN2", target_bir_lowering=False)
    st = contextlib.ExitStack()

    def dram(name, shape, dt=F32, kind="ExternalInput"):
        return nc.dram_tensor(name, list(shape), dt, kind=kind).ap()

    x_d = dram("x", [n_seq, SEQ, D])
    out_d = dram("out", [n_seq, SEQ, D], kind="ExternalOutput")
    win_d = dram("w_in", [L, D, DIN])
    wbr_d = dram("w_branch", [L, 3, D, D])
    wout_d = dram("w_out", [L, D, D])
    wg_d = dram("ffn_w_gate", [L, D, DFF])
    wu_d = dram("ffn_w_up", [L, D, DFF])
    wd_d = dram("ffn_w_down", [L, DFF, D])
    par_d = dram("par", [L, 128, NPAR])
    w2_d = dram("w2aug", [L, 17, 512])
    wscr_d = dram("wscr", [128, L * WPL], BF16, kind="Internal")
    cd = {}
    for name, shp, dt in (("ident_f", [128, 128], F32), ("ident_b", [128, 128], BF16), ("ones_b", [128, 128], BF16),
                          ("ret_dt", [128, 512], F32), ("ret_xi", [128, 512], F32), ("ret_zt", [128, 512], F32),
                          ("gla_tri", [128, 128], F32), ("gla_sl", [128, 128], F32),
                          ("swa_mask", [128, 4096], BF16)):
        cd[name] = dram(name, shp, dt)
    if dbg:
        dbg_d = dram("dbg", [128, 8 * T], kind="ExternalOutput")

    def sb(name, shape, dt):
        return st.enter_context(nc.sbuf_tensor(name, list(shape), dt))

    S = Sched(nc)
    bufs = {}

    def B(*key):
        b = bufs.get(key)
        if b is None:
            b = bufs[key] = Buf(str(key))
        return b

    xT = sb("xT", [128, 8, T], F32)
    hT = sb("hT", [128, 8, T], BF16)
    sqr = sb("sqr", [128, 2, T], BF16)
    rstd = sb("rstd", [128, T], F32)
    tmpf = sb("tmpf", [128, T], F32)
    NST, NBF = 2, 3
    wst = [sb("wst%d" % i, [128, 2048], F32) for i in range(NST)]
    wbf = [sb("wbf%d" % i, [128, 2048], BF16) for i in range(NBF)]
    merged = sb("merged", [128, 8, T], BF16)
    qT = sb("qT", [128, 4, T], BF16)
    kT = sb("kT", [128, 4, T], BF16)
    kzT = sb("kzT", [128, 4, T], BF16)
    vtok = sb("vtok", [128, NT, 1024], BF16)
    sgr = sb("sgr", [128, 2, T], BF16)
    osb = sb("osb", [128, 8, T], F32)
    obf = sb("obf", [128, 8, T], BF16)
    AT = [sb("AT%d" % i, [128, 512], BF16) for i in range(2)]
    ktok = [sb("ktok%d" % i, [128, 512], BF16) for i in range(2)]
    retS = [sb("retS%d" % l, [128, 4, 256], F32) for l in range(L)]
    retSb = [sb("retSb%d" % l, [128, 4, 256], BF16) for l in range(L)]
    glaS = [sb("glaS%d" % l, [128, 4, 256], F32) for l in range(L)]
    glaSb = [sb("glaSb%d" % l, [128, 4, 256], BF16) for l in range(L)]
    gE = [sb("gE%d" % i, [128, T], F32) for i in range(2)]
    gEi = [sb("gEi%d" % i, [128, T], F32) for i in range(2)]
    gEs = [sb("gEs%d" % i, [128, T], F32) for i in range(2)]
    gaT = sb("gaT", [32, T], BF16)
    w2f = [sb("w2f%d" % l, [17, 512], F32) for l in range(L)]
    w2b = [sb("w2b%d" % l, [17, 512], BF16) for l in range(L)]
    KK = sb("KK", [128, 4, 128 + T], BF16)
    svt = sb("svt", [128, NT + 1, 256], BF16)
    ETb = [sb("ET%d" % i, [128, 512], BF16) for i in range(2)]
    PTs = sb("PTs", [128, 2, 4, 2, 128], BF16)
    den = sb("den", [128, 4, 128], F32)
    kkprev = [sb("kkprev%d" % l, [128, 4, 128], BF16) for l in range(L)]
    svprev = [sb("svprev%d" % l, [128, 256], BF16) for l in range(L)]
    xin = sb("xin", [128, D], F32)
    par = [sb("par%d" % l, [128, NPAR], F32) for l in range(L)]
    es = [sb("es%d" % l, [128, 8], F32) for l in range(L)]
    ct = {}
    for name, ap in cd.items():
        ct[name] = sb("c_" + name, list(ap.shape), ap.dtype)
    onesb = sb("onesb32", [32, T], BF16)

    NPS = 7
    pst = [st.enter_context(nc.psum_tensor("ps%d" % i, [128, 512], F32)) for i in range(NPS)]
    ptr = st.enter_context(nc.psum_tensor("ptr", [128, 1024], BF16))
    psbuf = [Buf("ps%d" % i) for i in range(NPS)]
    for i in range(NPS):
        S.psgen[i] = 0
    psrr = [0]

    def ps_alloc():
        i = psrr[0] % NPS
        psrr[0] += 1
        S.psgen[i] += 1
        return PsH(i, S.psgen[i], pst[i], psbuf[i])

    def mm(out, lhsT, rhs, start, stop, reads, writes):
        S.op(PE, lambda e: e.matmul(out, lhsT=lhsT, rhs=rhs, start=start, stop=stop), reads=reads, writes=writes)

    def tr(out, in_, ident, reads, writes):
        S.op(PE, lambda e: e.transpose(out=out, in_=in_, identity=ident), reads=reads, writes=writes)

    def act(out, in_, func, reads, writes, **kw):
        S.op(ACT, lambda e: e.activation(out=out, in_=in_, func=func, **kw), reads=reads, writes=writes)

    def tt(out, in0, in1, op, reads, writes, eng=DVE):
        S.op(eng, lambda e: e.tensor_tensor(out=out, in0=in0, in1=in1, op=op), reads=reads, writes=writes)

    def ts(out, in0, s1, s2, op0, op1, reads, writes, eng=DVE):
        if s2 is None:
            S.op(eng, lambda e: e.tensor_scalar(out=out, in0=in0, scalar1=s1, scalar2=None, op0=op0),
                 reads=reads, writes=writes)
        else:
            S.op(eng, lambda e: e.tensor_scalar(out=out, in0=in0, scalar1=s1, scalar2=s2, op0=op0, op1=op1),
                 reads=reads, writes=writes)

    def stt(out, in0, scalar, in1, op0, op1, reads, writes):
        S.op(DVE, lambda e: e.scalar_tensor_tensor(out=out, in0=in0, scalar=scalar, in1=in1, op0=op0, op1=op1),
             reads=reads, writes=writes)

    def vcopy(out, in_, reads, writes):
        S.op(DVE, lambda e: e.tensor_copy(out=out, in_=in_), reads=reads, writes=writes)

    def recip(out, in_, reads, writes):
        S.op(DVE, lambda e: e.reciprocal(out=out, in_=in_), reads=reads, writes=writes)

    dsem_c = S.new_dma_sem("dc")
    dsem_x = S.new_dma_sem("dx")
    dsem_o = S.new_dma_sem("do")

    def dma(out, in_, reads, writes, sem, eng=SP):
        return S.op(eng, lambda e: e.dma_start(out=out, in_=in_), reads=reads, writes=writes, dma_sem=sem)

    for name in cd:
        dma(ct[name][:], cd[name], [], [B("c", name)], S.new_dma_sem("dc_" + name))
    for l in range(n_layers):
        dma(par[l][:], par_d[l], [], [B("par", l)], S.new_dma_sem("dc_par%d" % l))
        dma(w2f[l][:], w2_d[l], [], [B("w2f", l)], S.new_dma_sem("dc_w2%d" % l))
        vcopy(w2b[l][:], w2f[l][:], [B("w2f", l)], [B("w2b", l)])
        act(es[l][:], par[l][:, P_SINK:P_SINK + 8], AF.Exp, [B("par", l)], [B("es", l)])
    S.op(DVE, lambda e: e.memset(onesb[:], 1.0), writes=[B("onesb")])
    identf, identb, ones = ct["ident_f"], ct["ident_b"], ct["ones_b"]
    Bif, Bib, Bones = B("c", "ident_f"), B("c", "ident_b"), B("c", "ones_b")

    wsem = [S.new_dma_sem("dw%d" % i) for i in range(NST)]
    NRING = NBF + 2 * NST
    rsem = [S.new_dma_sem("dr%d" % i) for i in range(NRING)]
    dws = S.new_dma_sem("dws")
    wlist = []
    wstate = {"dma": 0, "cast": 0, "i0": None, "nconv": 0}
    wst_bf = [w_[:, :].bitcast(BF16) for w_ in wst]

    def ring_slot(r):
        if r < NBF:
            return wbf[r][:, :], [B("wbf", r)]
        s_, h_ = (r - NBF) // 2, (r - NBF) % 2
        return wst_bf[s_][:, h_ * 2048:(h_ + 1) * 2048], [B("wsth", s_, h_)]

    def wst_bufs(slot):
        return [B("wsth", slot, 0), B("wsth", slot, 1)]

    def w_issue_dma(i):
        view, KC, ncols, off, conv = wlist[i]
        n = KC * ncols
        if conv:
            slot = i % NST
            dst = wst[slot][:, 0:n].rearrange("p (k n) -> p k n", k=KC)
            dma(dst, view, [], wst_bufs(slot), wsem[slot])
        else:
            if wstate["i0"] is None:
                wstate["i0"] = i
            r = (i - wstate["i0"]) % NRING
            tl, bb = ring_slot(r)
            dma(tl[:, 0:n], wscr_d[:, off:off + n], [B("wscr_all")], bb, rsem[r])

    def w_issue_cast(i):
        view, KC, ncols, off, conv = wlist[i]
        if not conv:
            return
        slot, bslot = i % NST, i % NBF
        n = KC * ncols
        if i % 5 in (0, 2):
            vcopy(wbf[bslot][:, 0:n], wst[slot][:, 0:n], wst_bufs(slot), [B("wbf", bslot)])
        else:
            act(wbf[bslot][:, 0:n], wst[slot][:, 0:n], AF.Copy, wst_bufs(slot), [B("wbf", bslot)])
        dma(wscr_d[:, off:off + n], wbf[bslot][:, 0:n], [B("wbf", bslot)], [B("wscr_all")], dws)

    def wget(i):
        assert i == wstate.get("last", -1) + 1, ("weight groups must be consumed in list order", i, wstate.get("last"))
        wstate["last"] = i
        nconv = wstate.get("n_conv")
        if nconv is None:
            nconv = wstate["n_conv"] = sum(1 for w_ in wlist if w_[4])

        def dma_ok(j):
            if wlist[j][4]:
                return j < i + NST
            return i >= nconv and j < i + NRING - 1
        while wstate["cast"] < min(len(wlist), i + 2):
            while wstate["dma"] <= wstate["cast"] and wlist[wstate["dma"]][4]:
                w_issue_dma(wstate["dma"])
                wstate["dma"] += 1
            w_issue_cast(wstate["cast"])
            wstate["cast"] += 1
        while wstate["dma"] < len(wlist) and dma_ok(wstate["dma"]):
            w_issue_dma(wstate["dma"])
            wstate["dma"] += 1
        assert wstate["dma"] > i
        view, KC, ncols, off, conv = wlist[i]
        n = KC * ncols
        if conv:
            return wbf[i % NBF][:, 0:n].rearrange("p (k n) -> p k n", k=KC), B("wbf", i % NBF)
        tl, bb = ring_slot((i - wstate["i0"]) % NRING)
        return tl[:, 0:n].rearrange("p (k n) -> p k n", k=KC), bb[0]

    scr_off = {}

    def layer_groups(l, conv):
        g = {}
        win = win_d[l].rearrange("(kc p) n -> p kc n", p=128)
        cursor = [l * WPL]

        def add(name, view, KC, ncols):
            g[name] = len(wlist)
            wlist.append((view, KC, ncols, cursor[0], conv))
            cursor[0] += KC * ncols
            assert cursor[0] <= (l + 1) * WPL

        def add_in(name, c0, n):
            for i in range(0, n, 256):
                w = min(256, n - i)
                add((name, i // 256), win[:, :, c0 + i:c0 + i + w], 8, w)
        add_in("rq", C_RQ, 512); add_in("rk", C_RK, 512); add_in("rv", C_RV, 1024); add_in("rg", C_RG, 1024)
        for n in range(3):
            if n == 1:
                add_in("ga", C_GA, 16); add_in("gv", C_GV, 1024)
                for i_ in range(2):
                    add(("gq", i_), win[:, :, C_GQ + i_ * 256:C_GQ + (i_ + 1) * 256], 8, 256)
                    add(("gk", i_), win[:, :, C_GK + i_ * 256:C_GK + (i_ + 1) * 256], 8, 256)
                add_in("gg", C_GG, 1024)
            if n == 2:
                add_in("sk", C_SK, 256); add_in("sv", C_SV, 256); add_in("sq", C_SQ, 1024)
            wb = wbr_d[l, n].rearrange("(kc p) n -> p kc n", p=128)
            for i in range(4):
                add(("gate", n, i), win[:, :, C_GATE + n * 1024 + i * 256:C_GATE + n * 1024 + (i + 1) * 256], 8, 256)
                add(("br", n, i), wb[:, :, i * 256:(i + 1) * 256], 8, 256)
        wo = wout_d[l].rearrange("(kc p) n -> p kc n", p=128)
        for i in range(4):
            add(("wo", i), wo[:, :, i * 256:(i + 1) * 256], 8, 256)
        wgv = wg_d[l].rearrange("(kc p) n -> p kc n", p=128)
        wuv = wu_d[l].rearrange("(kc p) n -> p kc n", p=128)
        wdv = wd_d[l].rearrange("(kc p) n -> p kc n", p=128)
        for half in range(2):
            for i in range(6):
                w = 256 if i < 5 else 128
                c0 = half * 1408 + i * 256
                add(("fg", half, i), wgv[:, :, c0:c0 + w], 8, w)
                add(("fu", half, i), wuv[:, :, c0:c0 + w], 8, w)
            for c in range(8):
                add(("fd", half, c), wdv[:, half * 11:(half + 1) * 11, c * 128:(c + 1) * 128], 11, 128)
        return g

    def proj_fm(gidx, ncols_chunks, evac, src=None, srcB=None):
        if src is None:
            src, srcB = hT, [B("hT", c) for c in range(8)]
        wv, wb = wget(gidx)
        KC = wv.shape[1]
        ncols = wv.shape[2]
        j = 0
        c0 = 0
        while c0 < ncols:
            w = min(128, ncols - c0)
            p = ps_alloc()
            for kc in range(KC):
                if os.environ.get("KDBG_NOMM"):
                    continue
                mm(p.t[0:w, :], wv[:, kc, c0:c0 + w], src[:, kc, :], kc == 0, kc == KC - 1,
                   [wb, srcB[kc]], [p])
            if not os.environ.get("KDBG_NOEVAC"):
                evac(j, p, w)
            j += 1
            c0 += w

    def proj_tm(gidx, evac):
        wv, wb = wget(gidx)
        ncols = wv.shape[2]
        for t in range(NT):
            p = ps_alloc()
            for kc in range(8):
                mm(p.t[:, 0:ncols], hT[:, kc, t * 128:(t + 1) * 128], wv[:, kc, :], kc == 0, kc == 7,
                   [wb, B("hT", kc)], [p])
            evac(t, p, ncols)

    sq_rr = [0]

    def rmsnorm_stats(srcT, srcB, width_scale, eps):
        p = ps_alloc()
        for c in range(8):
            s = sq_rr[0] % 2
            sq_rr[0] += 1
            act(sqr[:, s, :], srcT[:, c, :], AF.Square, [srcB[c]], [B("sqr", s)])
            mm(p.t[:, :], ones[:, :], sqr[:, s, :], c == 0, c == 7, [Bones, B("sqr", s)], [p])
        act(tmpf[:], p.t[:, :], AF.Sqrt, [p, Beps(eps)], [B("tmpf")], scale=width_scale, bias=eps_ap(eps))
        recip(rstd[:], tmpf[:], [B("tmpf")], [B("rstd")])

    eps_tiles = {}

    def eps_ap(v):
        if v not in eps_tiles:
            t_ = sb("eps%d" % len(eps_tiles), [128, 1], F32)
            S.op(DVE, lambda e: e.memset(t_[:], float(v)), writes=[B("eps", v)])
            eps_tiles[v] = t_
        return eps_tiles[v][:, 0:1]

    for v_ in (1e-6, 1e-5, 1.0):
        eps_ap(v_)

    def Beps(v):
        return B("eps", v)

    def pre_norm(l, poff):
        rmsnorm_stats(xT, [B("xT", c) for c in range(8)], 1.0 / D, 1e-6)
        for c in range(8):
            stt(hT[:, c, :], xT[:, c, :], par[l][:, poff + c:poff + c + 1], rstd[:], ALU.mult, ALU.mult,
                [B("xT", c), B("par", l), B("rstd")], [B("hT", c)])

    def post_norm_residual(l, poff):
        rmsnorm_stats(osb, [B("osb", c) for c in range(8)], 1.0 / D, 1e-6)
        for c in range(8):
            stt(osb[:, c, :], osb[:, c, :], par[l][:, poff + c:poff + c + 1], rstd[:], ALU.mult, ALU.mult,
                [B("osb", c), B("par", l), B("rstd")], [B("osb", c)])
            tt(xT[:, c, :], xT[:, c, :], osb[:, c, :], ALU.add, [B("xT", c), B("osb", c)], [B("xT", c)])

    _act = act

    def ret_branch(l, g, first_mt):
        DTt, XIt, ZTt = ct["ret_dt"], ct["ret_xi"], ct["ret_zt"]
        XIb = XIt[:, :].rearrange("p (h i) -> p h i", h=4)
        ZTb = ZTt[:, :].rearrange("p (h i) -> p h i", h=4)

        def ev_q(gi):
            def f(j, p, w):
                h = gi * 2 + j
                for t_ in range(NT):
                    tt(qT[:, h, t_ * 128:(t_ + 1) * 128], p.t[:, t_ * 128:(t_ + 1) * 128], XIt[:, h * 128:(h + 1) * 128], ALU.mult,
                       [p, B("c", "ret_xi")], [B("qT", h)])
            return f

        def ev_k(gi):
            def f(j, p, w):
                h = gi * 2 + j
                act(kT[:, h, :], p.t[:, :], AF.Copy, [p], [B("kT", h)])
                for t_ in range(NT):
                    tt(kzT[:, h, t_ * 128:(t_ + 1) * 128], p.t[:, t_ * 128:(t_ + 1) * 128], ZTt[:, h * 128:(h + 1) * 128], ALU.mult,
                       [p, B("c", "ret_zt")], [B("kzT", h)])
            return f

        def ev_q2(gi):
            def f(j, p, w):
                h = gi * 2 + j
                act(obf[:, h, :], p.t[:, :], AF.Copy, [p], [B("obf", h)])
                ev_q(gi)(j, p, w)
            return f
        for gi in range(2):
            proj_fm(g[("rq", gi)], 2, ev_q2(gi))
        for gi in range(2):
            proj_fm(g[("rk", gi)], 2, ev_k(gi))

        dbg_dump("r1", xT, [B("xT", c_) for c_ in range(8)])

        def ev_v(gi):
            def f(t, p, n):
                act(vtok[:, t, gi * 256:(gi + 1) * 256], p.t[:, 0:256], AF.Copy, [p], [B("vtok", t)])
            return f
        for gi in range(4):
            proj_tm(g[("rv", gi)], ev_v(gi))
        dbg_dump("r2", xT, [B("xT", c_) for c_ in range(8)])
        if first_mt:
            S.op(DVE, lambda e: e.memset(retS[l][:], 0.0), writes=[B("retS", l)])
            S.op(DVE, lambda e: e.memset(retSb[l][:], 0.0), writes=[B("retSb", l)])
        for t in range(NT):
            tok = slice(t * 128, (t + 1) * 128)
            a = t % 2
            p = ps_alloc()
            for h in range(4):
                mm(p.t[:, h * 128:(h + 1) * 128], kT[:, h, tok], obf[:, h, tok], True, True,
                   [B("kT", h), B("obf", h)], [p])
            tt(AT[a][:], p.t[:, :], DTt[:, :], ALU.mult, [p, B("c", "ret_dt")], [B("AT", a)])
            dbg_dump("r3", xT, [B("xT", c_) for c_ in range(8)])
            for h in range(4):
                tr(ptr[:, h * 128:(h + 1) * 128], kzT[:, h, tok], identb[:], [B("kzT", h), Bib], [B("ptr", 0)])
            act(ktok[a][:], ptr[:, 0:512], AF.Copy, [B("ptr", 0)], [B("ktok", a)])
            dbg_dump("r4", xT, [B("xT", c_) for c_ in range(8)])
            for hh in range(2):
                p = ps_alloc()
                for h2 in range(2):
                    h = hh * 2 + h2
                    for ec in range(2):
                        o_ = p.t[:, (h2 * 2 + ec) * 128:(h2 * 2 + ec + 1) * 128]
                        mm(o_, vtok[:, t, h * 256 + ec * 128:h * 256 + (ec + 1) * 128], AT[a][:, h * 128:(h + 1) * 128],
                           True, False, [B("vtok", t), B("AT", a)], [p])
                        mm(o_, retSb[l][:, h, ec * 128:(ec + 1) * 128], qT[:, h, tok], False, True,
                           [B("retSb", l), B("qT", h)], [p])
                act(osb[:, hh * 4:(hh + 1) * 4, tok], p.t[:, :].rearrange("p (c i) -> p c i", c=4), AF.Copy,
                    [p], [B("osb", c) for c in range(hh * 4, hh * 4 + 4)])
            dbg_dump("r5", xT, [B("xT", c_) for c_ in range(8)])
            for hh in range(2):
                p = ps_alloc()
                for h2 in range(2):
                    h = hh * 2 + h2
                    mm(p.t[:, h2 * 256:(h2 + 1) * 256], ktok[a][:, h * 128:(h + 1) * 128], vtok[:, t, h * 256:(h + 1) * 256],
                       True, True, [B("ktok", a), B("vtok", t)], [p])
                for h2 in range(2):
                    h = hh * 2 + h2
                    stt(retS[l][:, h, :], retS[l][:, h, :], RET_G128[h], p.t[:, h2 * 256:(h2 + 1) * 256], ALU.mult, ALU.add,
                        [B("retS", l), p], [B("retS", l)])
                act(retSb[l][:, hh * 2:hh * 2 + 2, :], retS[l][:, hh * 2:hh * 2 + 2, :], AF.Copy,
                    [B("retS", l)], [B("retSb", l)])
        dbg_dump("r6", xT, [B("xT", c_) for c_ in range(8)])
        for h in range(4):
            pm = ps_alloc()
            pq = ps_alloc()
            for ec in range(2):
                c = h * 2 + ec
                s = sq_rr[0] % 2
                sq_rr[0] += 1
                act(sqr[:, s, :], osb[:, c, :], AF.Copy, [B("osb", c)], [B("sqr", s)])
                mm(pm.t[:, :], ones[:, :], sqr[:, s, :], ec == 0, ec == 1, [Bones, B("sqr", s)], [pm])
                s = sq_rr[0] % 2
                sq_rr[0] += 1
                act(sqr[:, s, :], osb[:, c, :], AF.Square, [B("osb", c)], [B("sqr", s)])
                mm(pq.t[:, :], ones[:, :], sqr[:, s, :], ec == 0, ec == 1, [Bones, B("sqr", s)], [pq])
            act(tmpf[:], pm.t[:, :], AF.Square, [pm], [B("tmpf")], scale=1.0 / 256)
            stt(tmpf[:], pq.t[:, :], 1.0 / 256, tmpf[:], ALU.mult, ALU.subtract, [pq, B("tmpf")], [B("tmpf")])
            act(tmpf[:], tmpf[:], AF.Sqrt, [B("tmpf"), Beps(1e-5)], [B("tmpf")], bias=eps_ap(1e-5))
            recip(rstd[:], tmpf[:], [B("tmpf")], [B("rstd")])
            stt(tmpf[:], pm.t[:, :], -1.0 / 256, rstd[:], ALU.mult, ALU.mult, [pm, B("rstd")], [B("tmpf")])
            for ec in range(2):
                c = h * 2 + ec
                tt(osb[:, c, :], osb[:, c, :], rstd[:], ALU.mult, [B("osb", c), B("rstd")], [B("osb", c)])
                tt(osb[:, c, :], osb[:, c, :], tmpf[:], ALU.add, [B("osb", c), B("tmpf")], [B("osb", c)])
                act(osb[:, c, :], osb[:, c, :], AF.Identity, [B("osb", c), B("par", l)], [B("osb", c)],
                    scale=par[l][:, P_RETW + c:P_RETW + c + 1], bias=par[l][:, P_RETB + c:P_RETB + c + 1])

        dbg_dump("r7", xT, [B("xT", c_) for c_ in range(8)])

        def ev_g(gi):
            def f(j, p, w):
                c = gi * 2 + j
                s = c % 2
                act(sgr[:, s, :], p.t[:, :], AF.Silu, [p], [B("sgr", s)])
                tt(obf[:, c, :], osb[:, c, :], sgr[:, s, :], ALU.mult, [B("osb", c), B("sgr", s)], [B("obf", c)])
            return f
        for gi in range(4):
            proj_fm(g[("rg", gi)], 2, ev_g(gi))

    def gla_branch2(l, g, first_mt):
        TRI, SLm = ct["gla_tri"], ct["gla_sl"]
        vcopy(gaT[:, :], onesb[:, :], [B("onesb")], [B("gaT")])

        def ev_ga(j, p, w):
            act(gaT[0:16, :], p.t[0:16, :], AF.Copy, [p], [B("gaT")])
        proj_fm(g[("ga", 0)], 1, ev_ga)

        def ev_v(gi):
            def f(t, p, n):
                act(vtok[:, t, gi * 256:(gi + 1) * 256], p.t[:, 0:256], AF.Copy, [p], [B("vtok", t)])
            return f
        for gi in range(4):
            proj_tm(g[("gv", gi)], ev_v(gi))
        if first_mt:
            S.op(DVE, lambda e: e.memset(glaS[l][:], 0.0), writes=[B("glaS", l, h_) for h_ in range(4)])
            S.op(DVE, lambda e: e.memset(glaSb[l][:], 0.0), writes=[B("glaSb", l, h_) for h_ in range(4)])
        for t in range(NT):
            pz = ps_alloc()
            mm(pz.t[:, :], gaT[0:17, t * 128:(t + 1) * 128], w2b[l][0:17, :], True, True,
               [B("gaT"), B("w2b", l)], [pz])
            act(osb[:, t, :], pz.t[:, :], AF.Exp, [pz], [B("osb", t)], scale=-1.0)
            act(osb[:, t, :], osb[:, t, :], AF.Ln, [B("osb", t), Beps(1.0)], [B("osb", t)], bias=eps_ap(1.0))
            ts(osb[:, t, :], osb[:, t, :], -1.0 / 16, -1.0, ALU.mult, ALU.max, [B("osb", t)], [B("osb", t)])
        for gi in range(2):
            for j in range(2):
                h = gi * 2 + j
                pb_ = ps_alloc()
                pl_ = ps_alloc()
                for t in range(NT):
                    mm(pb_.t[:, t * 128:(t + 1) * 128], osb[:, t, h * 128:(h + 1) * 128], TRI[:, :], True, True,
                       [B("osb", t), B("c", "gla_tri")], [pb_])
                    mm(pl_.t[:, t * 128:(t + 1) * 128], osb[:, t, h * 128:(h + 1) * 128], SLm[:, :], True, True,
                       [B("osb", t), B("c", "gla_sl")], [pl_])
                act(gE[j][:], pb_.t[:, :], AF.Exp, [pb_], [B("gE", j)])
                act(gEi[j][:], pb_.t[:, :], AF.Exp, [pb_], [B("gEi", j)], scale=-1.0)
                act(gEs[j][:], pl_.t[:, :], AF.Exp, [pl_], [B("gEs", j)])
                vcopy(gdec[:, h, :], gE[j][:, :].rearrange("p (b i) -> p b i", i=64)[:, :, 63], [B("gE", j)], [B("gdec")])
            wv, wb = wget(g[("gq", gi)])
            for j in range(2):
                h = gi * 2 + j
                p = ps_alloc()
                for kc in range(8):
                    mm(p.t[:, :], wv[:, kc, j * 128:(j + 1) * 128], hT[:, kc, :], kc == 0, kc == 7, [wb, B("hT", kc)], [p])
                stt(qT[:, h, :], p.t[:, :], 128.0 ** -0.5, gE[j][:], ALU.mult, ALU.mult, [p, B("gE", j)], [B("qT", h)])
            wv, wb = wget(g[("gk", gi)])
            for j in range(2):
                h = gi * 2 + j
                p = ps_alloc()
                for kc in range(8):
                    mm(p.t[:, :], wv[:, kc, j * 128:(j + 1) * 128], hT[:, kc, :], kc == 0, kc == 7, [wb, B("hT", kc)], [p])
                tt(kT[:, h, :], p.t[:, :], gEi[j][:], ALU.mult, [p, B("gEi", j)], [B("kT", h)])
                tt(kzT[:, h, :], p.t[:, :], gEs[j][:], ALU.mult, [p, B("gEs", j)], [B("kzT", h)])
        TRIb = TRI[:, :].unsqueeze(1).to_broadcast([128, 4, 128])
        for t in range(NT):
            tok = slice(t * 128, (t + 1) * 128)
            a = t % 2
            p = ps_alloc()
            for h in range(4):
                mm(p.t[:, h * 128:(h + 1) * 128], kT[:, h, tok], qT[:, h, tok], True, True,
                   [B("kT", h), B("qT", h)], [p])
            tt(AT[a][:].rearrange("p (h i) -> p h i", h=4), p.t[:, :].rearrange("p (h i) -> p h i", h=4), TRIb, ALU.mult,
               [p, B("c", "gla_tri")], [B("AT", a)])
            for h in range(4):
                tr(ptr[:, h * 128:(h + 1) * 128], kzT[:, h, tok], identb[:], [B("kzT", h), Bib], [B("ptr", 0)])
            act(ktok[a][:], ptr[:, 0:512], AF.Copy, [B("ptr", 0)], [B("ktok", a)])
            po = [ps_alloc(), ps_alloc()]
            for blk in range(2):
                bt = slice(t * 128 + blk * 64, t * 128 + blk * 64 + 64)
                for h in range(4):
                    for ec in range(2):
                        base = ((h % 2) * 2 + ec) * 128 + blk * 64
                        o_ = po[h // 2].t[:, base:base + 64]
                        mm(o_, vtok[:, t, h * 256 + ec * 128:h * 256 + (ec + 1) * 128],
                           AT[a][:, h * 128 + blk * 64:h * 128 + blk * 64 + 64],
                           True, False, [B("vtok", t), B("AT", a)], [po[h // 2]])
                        mm(o_, glaSb[l][:, h, ec * 128:(ec + 1) * 128], qT[:, h, bt],
                           False, True, [B("glaSb", l, h), B("qT", h)], [po[h // 2]])
                for hh in range(2):
                    pk = ps_alloc()
                    for h2 in range(2):
                        h = hh * 2 + h2
                        mm(pk.t[:, h2 * 256:(h2 + 1) * 256], ktok[a][blk * 64:(blk + 1) * 64, h * 128:(h + 1) * 128],
                           vtok[blk * 64:(blk + 1) * 64, t, h * 256:(h + 1) * 256], True, True,
                           [B("ktok", a), B("vtok", t)], [pk])
                    for h2 in range(2):
                        h = hh * 2 + h2
                        stt(glaS[l][:, h, :], glaS[l][:, h, :], gdec[:, h, t * 2 + blk:t * 2 + blk + 1],
                            pk.t[:, h2 * 256:(h2 + 1) * 256], ALU.mult, ALU.add,
                            [B("glaS", l, h), B("gdec"), pk], [B("glaS", l, h)])
                        act(glaSb[l][:, h, :], glaS[l][:, h, :], AF.Copy, [B("glaS", l, h)], [B("glaSb", l, h)])
            for hh in range(2):
                act(obf_raw(hh, tok), po[hh].t[:, :].rearrange("p (c i) -> p c i", c=4), AF.Copy,
                    [po[hh]], [B("osb", c) for c in range(hh * 4, hh * 4 + 4)])
        for h in range(4):
            pq = ps_alloc()
            for ec in range(2):
                c = h * 2 + ec
                s = sq_rr[0] % 2
                sq_rr[0] += 1
                act(sqr[:, s, :], osb[:, c, :], AF.Square, [B("osb", c)], [B("sqr", s)])
                mm(pq.t[:, :], ones[:, :], sqr[:, s, :], ec == 0, ec == 1, [Bones, B("sqr", s)], [pq])
            act(tmpf[:], pq.t[:, :], AF.Sqrt, [pq, Beps(1e-6)], [B("tmpf")], scale=1.0 / 256, bias=eps_ap(1e-6))
            recip(rstd[:], tmpf[:], [B("tmpf")], [B("rstd")])
            for ec in range(2):
                c = h * 2 + ec
                stt(osb[:, c, :], osb[:, c, :], par[l][:, P_GLAW + c:P_GLAW + c + 1], rstd[:], ALU.mult, ALU.mult,
                    [B("osb", c), B("par", l), B("rstd")], [B("osb", c)])

        def ev_g(gi):
            def f(j, p, w):
                c = gi * 2 + j
                s = c % 2
                act(sgr[:, s, :], p.t[:, :], AF.Silu, [p], [B("sgr", s)])
                tt(obf[:, c, :], osb[:, c, :], sgr[:, s, :], ALU.mult, [B("osb", c), B("sgr", s)], [B("obf", c)])
            return f
        for gi in range(4):
            proj_fm(g[("gg", gi)], 2, ev_g(gi))

    gdec = sb("gdec", [128, 4, 2 * NT], F32)

    def obf_raw(hh, tok):
        return osb[:, hh * 4:(hh + 1) * 4, tok]

    def swa_branch(l, g, first_mt):
        MASK = ct["swa_mask"][:, :].rearrange("p (s c k q) -> p s c k q", s=2, c=8, k=2)
        wv, wb = wget(g[("sk", 0)])
        if first_mt:
            S.op(DVE, lambda e: e.memset(KK[:, :, 0:128], 0.0), writes=[B("KK", gg) for gg in range(4)])
            S.op(DVE, lambda e: e.memset(svt[:, 0, :], 0.0), writes=[B("svt", 0)])
        else:
            vcopy(KK[:, :, 0:128], kkprev[l][:, :, :], [B("kkprev", l)], [B("KK", gg) for gg in range(4)])
            vcopy(svt[:, 0, :], svprev[l][:, :], [B("svprev", l)], [B("svt", 0)])
        for gg in range(4):
            p = ps_alloc()
            for half in range(2):
                for kc in range(8):
                    mm(p.t[half * 64:(half + 1) * 64, :], wv[:, kc, gg * 64:(gg + 1) * 64], hT[:, kc, :], kc == 0, kc == 7,
                       [wb, B("hT", kc)], [p])
            act(KK[:, gg, 128:128 + T], p.t[:, :], AF.Copy, [p], [B("KK", gg)])
        wv, wb = wget(g[("sv", 0)])
        for t in range(NT):
            p = ps_alloc()
            for kc in range(8):
                mm(p.t[:, 0:256], hT[:, kc, t * 128:(t + 1) * 128], wv[:, kc, :], kc == 0, kc == 7, [wb, B("hT", kc)], [p])
            act(svt[:, t + 1, :], p.t[:, 0:256], AF.Copy, [p], [B("svt", t + 1)])
        def qdst(c):
            return (qT, "qT", c) if c < 4 else (kT, "kT", c - 4)

        def ev_q(gi):
            def f(j, p, w):
                c = gi * 2 + j
                tl, nm, ci = qdst(c)
                act(tl[:, ci, :], p.t[:, :], AF.Copy, [p], [B(nm, ci)])
            return f
        for gi in range(4):
            proj_fm(g[("sq", gi)], 2, ev_q(gi))
        vcopy(kkprev[l][:, :, :], KK[:, :, T:T + 128], [B("KK", gg) for gg in range(4)], [B("kkprev", l)])
        vcopy(svprev[l][:, :], svt[:, NT, :], [B("svt", NT)], [B("svprev", l)])
        esb = es[l]
        for t in range(NT):
            tok = slice(t * 128, (t + 1) * 128)
            kts = [1] if (first_mt and t == 0) else [0, 1]
            for half in range(2):
                for s in range(2):
                    for pair in range(2):
                        p = ps_alloc()
                        for ci2 in range(2):
                            cc = pair * 2 + ci2
                            c = half * 4 + cc
                            tl, nm, ci = qdst(c)
                            gkv = c // 2
                            for kt in kts:
                                kcols = slice(t * 128 + kt * 128, t * 128 + kt * 128 + 128)
                                mm(p.t[:, (ci2 * 2 + kt) * 128:(ci2 * 2 + kt + 1) * 128], KK[s * 64:(s + 1) * 64, gkv, kcols],
                                   tl[s * 64:(s + 1) * 64, ci, tok], True, True, [B("KK", gkv), B(nm, ci)], [p])
                        e_ = pair
                        c0 = half * 4 + pair * 2
                        if len(kts) == 2:
                            act(ETb[e_][:], p.t[:, :], AF.Exp, [p], [B("ET", e_)], scale=0.125)
                            tt(PTs[:, s, pair * 2:pair * 2 + 2, :, :], ETb[e_][:].rearrange("p (h k q) -> p h k q", h=2, k=2),
                               MASK[:, s, c0:c0 + 2, :, :], ALU.mult, [B("ET", e_), B("c", "swa_mask")], [B("PTs", s, pair)])
                        else:
                            for ci2 in range(2):
                                act(ETb[e_][:, ci2 * 256 + 128:ci2 * 256 + 256], p.t[:, ci2 * 256 + 128:ci2 * 256 + 256], AF.Exp,
                                    [p], [B("ET", e_)], scale=0.125)
                            tt(PTs[:, s, pair * 2:pair * 2 + 2, 1, :],
                               ETb[e_][:].rearrange("p (h k q) -> p h k q", h=2, k=2)[:, :, 1, :],
                               MASK[:, s, c0:c0 + 2, 1, :], ALU.mult, [B("ET", e_), B("c", "swa_mask")], [B("PTs", s, pair)])
                po = ps_alloc()
                pd = ps_alloc()
                for cc in range(4):
                    c = half * 4 + cc
                    gkv = c // 2
                    for s in range(2):
                        for i, kt in enumerate(kts):
                            mm(po.t[s * 64:(s + 1) * 64, cc * 128:(cc + 1) * 128], svt[:, t + kt, gkv * 64:(gkv + 1) * 64],
                               PTs[:, s, cc, kt, :], i == 0, i == len(kts) - 1, [B("svt", t + kt), B("PTs", s, cc // 2)], [po])
                        for i, kt in enumerate(kts):
                            mm(pd.t[s * 64:(s + 1) * 64, cc * 128:(cc + 1) * 128], ones[:, 0:64],
                               PTs[:, s, cc, kt, :], i == 0, i == len(kts) - 1, [Bones, B("PTs", s, cc // 2)], [pd])
                tt(den[:, :, :], pd.t[:, :].rearrange("p (c q) -> p c q", c=4),
                   esb[:, half * 4:(half + 1) * 4].unsqueeze(2).to_broadcast([128, 4, 128]), ALU.add,
                   [pd, B("es", l)], [B("den")])
                recip(den[:, :, :], den[:, :, :], [B("den")], [B("den")])
                tt(obf[:, half * 4:(half + 1) * 4, tok], po.t[:, :].rearrange("p (c q) -> p c q", c=4), den[:, :, :], ALU.mult,
                   [po, B("den")], [B("obf", c) for c in range(half * 4, half * 4 + 4)])

    def branch_merge(l, g, n):
        for i in range(4):
            wvg, wbg = wget(g[("gate", n, i)])
            gates = []
            for j in range(2):
                c = i * 2 + j
                p = ps_alloc()
                for kc in range(8):
                    mm(p.t[:, :], wvg[:, kc, j * 128:(j + 1) * 128], hT[:, kc, :], kc == 0, kc == 7, [wbg, B("hT", kc)], [p])
                s = c % 2
                act(sgr[:, s, :], p.t[:, :], AF.Sigmoid, [p], [B("sgr", s)])
            wvb, wbb = wget(g[("br", n, i)])
            for j in range(2):
                c = i * 2 + j
                s = c % 2
                p = ps_alloc()
                for kc in range(8):
                    mm(p.t[:, :], wvb[:, kc, j * 128:(j + 1) * 128], obf[:, kc, :], kc == 0, kc == 7, [wbb, B("obf", kc)], [p])
                if n == 0:
                    tt(merged[:, c, :], p.t[:, :], sgr[:, s, :], ALU.mult, [p, B("sgr", s)], [B("merged", c)])
                else:
                    tt(tmpf[:], p.t[:, :], sgr[:, s, :], ALU.mult, [p, B("sgr", s)], [B("tmpf")])
                    tt(merged[:, c, :], merged[:, c, :], tmpf[:], ALU.add, [B("merged", c), B("tmpf")], [B("merged", c)])

    def out_proj(l, g):
        for i in range(4):
            wv, wb = wget(g[("wo", i)])
            for j in range(2):
                c = i * 2 + j
                p = ps_alloc()
                for kc in range(8):
                    mm(p.t[:, :], wv[:, kc, j * 128:(j + 1) * 128], merged[:, kc, :], kc == 0, kc == 7,
                       [wb, B("merged", kc)], [p])
                act(osb[:, c, :], p.t[:, :], AF.Copy, [p], [B("osb", c)])

    def fdst(j):
        return (obf, "obf", j) if j < 8 else (kzT, "kzT", j - 8)

    def ffn(l, g):
        for half in range(2):
            for i in range(6):
                wvg, wbg = wget(g[("fg", half, i)])
                nj = wvg.shape[2] // 128
                for jj in range(nj):
                    j = i * 2 + jj
                    p = ps_alloc()
                    for kc in range(8):
                        mm(p.t[:, :], wvg[:, kc, jj * 128:(jj + 1) * 128], hT[:, kc, :], kc == 0, kc == 7, [wbg, B("hT", kc)], [p])
                    s = j % 2
                    act(sgr[:, s, :], p.t[:, :], AF.Silu, [p], [B("sgr", s)])
                wvu, wbu = wget(g[("fu", half, i)])
                for jj in range(nj):
                    j = i * 2 + jj
                    s = j % 2
                    tl, nm, ci = fdst(j)
                    p = ps_alloc()
                    for kc in range(8):
                        mm(p.t[:, :], wvu[:, kc, jj * 128:(jj + 1) * 128], hT[:, kc, :], kc == 0, kc == 7, [wbu, B("hT", kc)], [p])
                    tt(tl[:, ci, :], p.t[:, :], sgr[:, s, :], ALU.mult, [p, B("sgr", s)], [B(nm, ci)])
            for c in range(8):
                wv, wb = wget(g[("fd", half, c)])
                p = ps_alloc()
                for j in range(11):
                    tl, nm, ci = fdst(j)
                    mm(p.t[:, :], wv[:, j, :], tl[:, ci, :], j == 0, j == 10, [wb, B(nm, ci)], [p])
                if half == 0:
                    act(osb[:, c, :], p.t[:, :], AF.Copy, [p], [B("osb", c)])
                else:
                    tt(osb[:, c, :], osb[:, c, :], p.t[:, :], ALU.add, [B("osb", c), p], [B("osb", c)])

    def load_x(sq, mt):
        for t in range(NT):
            r0 = mt * T + t * 128
            dma(xin[:], x_d[sq, r0:r0 + 128, :], [], [B("xin")], dsem_x)
            for hh in range(2):
                p = ps_alloc()
                for c4 in range(4):
                    c = hh * 4 + c4
                    tr(p.t[:, c4 * 128:(c4 + 1) * 128], xin[:, c * 128:(c + 1) * 128], identf[:], [B("xin"), Bif], [p])
                act(xT[:, hh * 4:(hh + 1) * 4, t * 128:(t + 1) * 128], p.t[:, :].rearrange("p (c i) -> p c i", c=4), AF.Copy,
                    [p], [B("xT", c) for c in range(hh * 4, hh * 4 + 4)])

    out_deps = []

    def store_x(sq, mt):
        for t in range(NT):
            r0 = mt * T + t * 128
            for hh in range(2):
                p = ps_alloc()
                for c4 in range(4):
                    c = hh * 4 + c4
                    tr(p.t[:, c4 * 128:(c4 + 1) * 128], xT[:, c, t * 128:(t + 1) * 128], identf[:], [B("xT", c), Bif], [p])
                act(xin[:, hh * 512:(hh + 1) * 512], p.t[:, :], AF.Copy, [p], [B("xin")])
            out_deps.append(dma(out_d[sq, r0:r0 + 128, :], xin[:], [B("xin")], [], dsem_o))

    class StopBuild(Exception):
        pass

    def dbg_dump(stage, tile, names):
        if not dbg or dbg.get("stage") != stage:
            return
        dsem_d = S.new_dma_sem("ddbg")
        if tile.dtype == BF16:
            dbgt = sb("dbgt", [128, 4, T], F32)
            for hf in range(2):
                vcopy(dbgt[:], tile[:, hf * 4:(hf + 1) * 4, :], names, [B("dbgt")])
                dep = dma(dbg_d[:, hf * 4 * T:(hf + 1) * 4 * T], dbgt[:].rearrange("p c t -> p (c t)"), [B("dbgt")], [], dsem_d)
        else:
            dep = dma(dbg_d, tile[:].rearrange("p c t -> p (c t)"), names, [], dsem_d)
        out_deps.append(dep)
        raise StopBuild()

    groups = {}
    plan = []
    for sq in range(n_seq):
        for mt in range(n_mt):
            for l in range(n_layers):
                groups[(sq, mt, l)] = layer_groups(l, conv=(sq == 0 and mt == 0))
    only = (dbg or {}).get("only")
    c8 = list(range(8))
    try:
      for sq in range(n_seq):
        for mt in range(n_mt):
            S.phase = 'load_x'
            load_x(sq, mt)
            dbg_dump("x", xT, [B("xT", c) for c in c8])
            for l in range(n_layers):
                g = groups[(sq, mt, l)]
                first = (mt == 0)
                S.phase = 'pre_norm_P_MIXPRE'
                pre_norm(l, P_MIXPRE)
                dbg_dump("h", hT, [B("hT", c) for c in c8])
                S.phase = 'ret_branch'
                ret_branch(l, g, first)
                dbg_dump("o_ret", obf, [B("obf", c) for c in c8])
                S.phase = 'branch_merge_0'
                branch_merge(l, g, 0)
                S.phase = 'gla_branch2'
                gla_branch2(l, g, first)
                dbg_dump("o_gla", obf, [B("obf", c) for c in c8])
                S.phase = 'branch_merge_1'
                branch_merge(l, g, 1)
                S.phase = 'swa_branch'
                swa_branch(l, g, first)
                dbg_dump("o_swa", obf, [B("obf", c) for c in c8])
                S.phase = 'branch_merge_2'
                branch_merge(l, g, 2)
                dbg_dump("merged", merged, [B("merged", c) for c in c8])
                S.phase = 'out_proj'
                out_proj(l, g)
                S.phase = 'post_norm_residual_P_MIXPOST'
                post_norm_residual(l, P_MIXPOST)
                dbg_dump("x1", xT, [B("xT", c) for c in c8])
                S.phase = 'pre_norm_P_FFNPRE'
                pre_norm(l, P_FFNPRE)
                S.phase = 'ffn'
                ffn(l, g)
                S.phase = 'post_norm_residual_P_FFNPOST'
                post_norm_residual(l, P_FFNPOST)
            S.phase = 'store_x'
            store_x(sq, mt)
    except StopBuild:
        pass
    S.final_wait(SP, out_deps)
    S.emit()
    st.close()
    return nc, S


def host_inputs(inputs):
    f = np.float32
    shared = dict(make_consts())
    for k in ("w_in", "w_branch", "w_out", "ffn_w_gate", "ffn_w_up", "ffn_w_down"):
        shared[k] = np.ascontiguousarray(np.asarray(inputs[k], dtype=f))
    par = np.zeros((L, 128, NPAR), dtype=f)
    for l in range(L):
        par[l, :, P_MIXPRE:P_MIXPRE + 8] = col(inputs["norm_mix_pre"][l])
        par[l, :, P_MIXPOST:P_MIXPOST + 8] = col(inputs["norm_mix_post"][l])
        par[l, :, P_FFNPRE:P_FFNPRE + 8] = col(inputs["norm_ffn_pre"][l])
        par[l, :, P_FFNPOST:P_FFNPOST + 8] = col(inputs["norm_ffn_post"][l])
        par[l, :, P_RETW:P_RETW + 8] = col(inputs["ret_norm_w"][l])
        par[l, :, P_RETB:P_RETB + 8] = col(inputs["ret_norm_b"][l])
        par[l, :, P_GLAW:P_GLAW + 8] = col(inputs["gla_norm_w"][l])
        sk = np.asarray(inputs["attn_sinks"][l], dtype=f)
        par[l, :, P_SINK:P_SINK + 8] = sk.reshape(8, 2).T[np.arange(128) // 64, :]
    shared["par"] = par
    w2 = np.concatenate([np.asarray(inputs["gla_w_alpha2"], dtype=f),
                         np.asarray(inputs["gla_b_alpha"], dtype=f)[:, None, :]], axis=1)
    shared["w2aug"] = np.ascontiguousarray(w2)
    return shared


_CACHE = {}


def kernel(**inputs):
    x = np.asarray(inputs["x"], dtype=np.float32)
    n = 8
    shared = host_inputs(inputs)
    if "nc" not in _CACHE:
        _CACHE["nc"] = build()[0]
    nc = _CACHE["nc"]
    in_maps = []
    for i in range(n):
        m = dict(shared)
        m["x"] = np.ascontiguousarray(x[2 * i:2 * i + 2])
        in_maps.append(m)
    res = run_bass_kernel_spmd(nc, in_maps, core_ids=list(range(n)))
    out = np.concatenate([np.asarray(r["out"], dtype=np.float32) for r in res.results], axis=0)
    return out
```

```python
import contextlib
import os
import numpy as np
import ml_dtypes
import concourse.bass as bass
import concourse.mybir as mybir
from concourse.bass_utils import run_bass_kernel_spmd

F32 = mybir.dt.float32
BF16 = mybir.dt.bfloat16
AF = mybir.ActivationFunctionType
ALU = mybir.AluOpType

PE, ACT, DVE, POOL, SP = "pe", "act", "dve", "pool", "sp"
ENGS = (PE, ACT, DVE, POOL, SP)

D = 1024
SEQ = 2048
L = 2
T = 512
NT = T // 128
DIN = 10768
DFF = 2816
C_RQ, C_RK, C_RV, C_RG = 0, 512, 1024, 2048
C_GQ, C_GK, C_GV, C_GG, C_GA = 3072, 3584, 4096, 5120, 6144
C_SQ, C_SK, C_SV, C_GATE = 6160, 7184, 7440, 7696
NPAR = 64
WPL = (D * DIN + 3 * D * D + D * D + 3 * D * DFF) // 128
P_MIXPRE, P_MIXPOST, P_FFNPRE, P_FFNPOST, P_RETW, P_RETB, P_GLAW, P_SINK = 0, 8, 16, 24, 32, 40, 48, 56


class Buf:
    __slots__ = ("name", "w", "r")

    def __init__(self, name=""):
        self.name = name
        self.w = None
        self.r = {}


class PsH:
    __slots__ = ("bank", "gen", "t", "buf")

    def __init__(self, bank, gen, t, buf):
        self.bank, self.gen, self.t, self.buf = bank, gen, t, buf


class Sched:
    def __init__(self, nc):
        self.nc = nc
        self.q = {e: [] for e in ENGS}
        self.cnt = {e: 0 for e in ENGS}
        self.seen = {e: {} for e in ENGS}
        self.dma_sems = []
        self.nops = 0
        self.nwaits = 0
        self.psgen = {}
        self.phase = ''
        self.pe_phase = []

    def new_dma_sem(self, name):
        self.dma_sems.append(name)
        self.cnt[name] = 0
        return name

    def _buf(self, b):
        if isinstance(b, PsH):
            assert self.psgen[b.bank] == b.gen, "stale PSUM handle bank %d" % b.bank
            return b.buf
        return b

    def op(self, eng, fn, reads=(), writes=(), dma_sem=None):
        need = {}

        def req(dep, skip_own):
            if dep is None:
                return
            k, c = dep
            if skip_own and k == eng and dma_sem is None and eng == PE:
                return
            if c > need.get(k, 0):
                need[k] = c

        ps_reads = [self._buf(b) for b in reads if isinstance(b, PsH)]
        reads = [self._buf(b) for b in reads]
        writes = [self._buf(b) for b in writes]
        for b in reads:
            req(b.w, False)
        for b in ps_reads:
            for k, c in b.r.items():
                if k != eng:
                    req((k, c), False)
        for b in writes:
            req(b.w, True)
            for k, c in b.r.items():
                req((k, c), True)
        waits = []
        seen = self.seen[eng]
        for k, c in need.items():
            if seen.get(k, 0) < c:
                seen[k] = c
                waits.append((k, c))
        if dma_sem is None:
            key = eng
            self.cnt[eng] += 1
            c = self.cnt[eng]
        else:
            key = dma_sem
            self.cnt[dma_sem] += 16
            c = self.cnt[dma_sem]
        for b in writes:
            b.w = (key, c)
            b.r = {}
        for b in reads:
            if b.r.get(key, 0) < c:
                b.r[key] = c
        self.q[eng].append((waits, fn, key))
        if eng == PE:
            self.pe_phase.append(self.phase)
        self.nops += 1
        self.nwaits += len(waits)
        return (key, c)

    def final_wait(self, eng, deps):
        waits = []
        for k, c in deps:
            if self.seen[eng].get(k, 0) < c:
                self.seen[eng][k] = c
                waits.append((k, c))
        self.q[eng].append((waits, None, None))

    def emit(self):
        nc = self.nc
        with contextlib.ExitStack() as st:
            sems = {}
            for k in list(ENGS) + self.dma_sems:
                sems[k] = st.enter_context(nc.semaphore("s_" + k))
            block = st.enter_context(nc.Block())

            def run(engname):
                def body(e):
                    for waits, fn, key in self.q[engname]:
                        if fn is None:
                            for k, c in waits:
                                e.wait_ge(sems[k], c)
                            continue
                        for k, c in waits[1:]:
                            e.wait_ge(sems[k], c)
                        ins = fn(e)
                        if waits:
                            ins._wait_ge(sems[waits[0][0]], waits[0][1])
                        ins.then_inc(sems[key], 1 if key in ENGS else 16)
                return body

            block.tensor(run(PE))
            block.scalar(run(ACT))
            block.vector(run(DVE))
            block.gpsimd(run(POOL))
            block.sync(run(SP))


def make_consts():
    f = np.float32
    c = {}
    c["ident_f"] = np.eye(128, dtype=f)
    c["ident_b"] = np.eye(128, dtype=f).astype(ml_dtypes.bfloat16)
    c["ones_b"] = np.ones((128, 128), dtype=f).astype(ml_dtypes.bfloat16)
    H = 4
    logg = np.log1p(-np.exp2(-5.0 - np.arange(H, dtype=np.float64)))
    pos = np.arange(128, dtype=np.float64)
    scale = 128.0 ** -0.5
    rel = pos[None, :] - pos[:, None]
    DTm = np.zeros((128, H, 128), dtype=np.float64)
    for h in range(H):
        DTm[:, h, :] = np.where(rel >= 0, scale * np.exp(rel * logg[h]), 0.0)
    c["ret_dt"] = DTm.astype(f).reshape(128, H * 128)
    XI = np.zeros((128, H, 128))
    ZT = np.zeros((128, H, 128))
    for h in range(H):
        XI[:, h, :] = (scale * np.exp((pos + 1.0) * logg[h]))[None, :]
        ZT[:, h, :] = np.exp((127.0 - pos) * logg[h])[None, :]
    c["ret_xi"] = XI.astype(f).reshape(128, H * 128)
    c["ret_zt"] = ZT.astype(f).reshape(128, H * 128)
    blk = (np.arange(128) // 64)
    same = blk[:, None] == blk[None, :]
    jj = np.arange(128)[:, None]
    ii = np.arange(128)[None, :]
    c["gla_tri"] = (same & (jj <= ii)).astype(f)
    c["gla_sl"] = (same & (jj > ii)).astype(f)
    Hq = 16
    slopes = np.exp2(-8.0 * np.arange(1, Hq + 1, dtype=np.float64) / Hq)
    M = np.zeros((128, Hq, 2, 128))
    k = np.arange(128)[:, None].astype(np.float64)
    q = np.arange(128)[None, :].astype(np.float64)
    for h in range(Hq):
        d1 = q - k
        M[:, h, 1, :] = np.where(d1 >= 0, np.exp(-slopes[h] * d1), 0.0)
        d0 = q + 128.0 - k
        M[:, h, 0, :] = np.where(d0 < 128, np.exp(-slopes[h] * d0), 0.0)
    M2 = M.reshape(128, 8, 2, 2, 128).transpose(0, 2, 1, 3, 4)
    c["swa_mask"] = np.ascontiguousarray(M2).astype(f).astype(ml_dtypes.bfloat16).reshape(128, Hq * 2 * 128)
    return c


RET_G128 = [float(np.exp(128.0 * np.log1p(-np.exp2(-5.0 - h)))) for h in range(4)]


def col(v):
    return np.ascontiguousarray(np.asarray(v, dtype=np.float32).reshape(8, 128).T)


def build(n_seq=2, n_mt=4, n_layers=2, dbg=None):
    nc = bass.Bass("TRN2", target_bir_lowering=False)
    st = contextlib.ExitStack()

    def dram(name, shape, dt=F32, kind="ExternalInput"):
        return nc.dram_tensor(name, list(shape), dt, kind=kind).ap()

    x_d = dram("x", [n_seq, SEQ, D])
    out_d = dram("out", [n_seq, SEQ, D], kind="ExternalOutput")
    win_d = dram("w_in", [L, D, DIN])
    wbr_d = dram("w_branch", [L, 3, D, D])
    wout_d = dram("w_out", [L, D, D])
    wg_d = dram("ffn_w_gate", [L, D, DFF])
    wu_d = dram("ffn_w_up", [L, D, DFF])
    wd_d = dram("ffn_w_down", [L, DFF, D])
    par_d = dram("par", [L, 128, NPAR])
    w2_d = dram("w2aug", [L, 17, 512])
    wscr_d = dram("wscr", [128, L * WPL], BF16, kind="Internal")
    cd = {}
    for name, shp, dt in (("ident_f", [128, 128], F32), ("ident_b", [128, 128], BF16), ("ones_b", [128, 128], BF16),
                          ("ret_dt", [128, 512], F32), ("ret_xi", [128, 512], F32), ("ret_zt", [128, 512], F32),
                          ("gla_tri", [128, 128], F32), ("gla_sl", [128, 128], F32),
                          ("swa_mask", [128, 4096], BF16)):
        cd[name] = dram(name, shp, dt)
    if dbg:
        dbg_d = dram("dbg", [128, 8 * T], kind="ExternalOutput")

    def sb(name, shape, dt):
        return st.enter_context(nc.sbuf_tensor(name, list(shape), dt))

    S = Sched(nc)
    bufs = {}

    def B(*key):
        b = bufs.get(key)
        if b is None:
            b = bufs[key] = Buf(str(key))
        return b

    xT = sb("xT", [128, 8, T], F32)
    hT = sb("hT", [128, 8, T], BF16)
    sqr = sb("sqr", [128, 2, T], BF16)
    rstd = sb("rstd", [128, T], F32)
    tmpf = sb("tmpf", [128, T], F32)
    NST, NBF = 2, 3
    wst = [sb("wst%d" % i, [128, 2048], F32) for i in range(NST)]
    wbf = [sb("wbf%d" % i, [128, 2048], BF16) for i in range(NBF)]
    merged = sb("merged", [128, 8, T], BF16)
    qT = sb("qT", [128, 4, T], BF16)
    kT = sb("kT", [128, 4, T], BF16)
    kzT = sb("kzT", [128, 4, T], BF16)
    vtok = sb("vtok", [128, NT, 1024], BF16)
    sgr = sb("sgr", [128, 2, T], BF16)
    sgall = sb("sgall", [128, 8, T], BF16)
    osb = sb("osb", [128, 8, T], F32)
    obf = sb("obf", [128, 8, T], BF16)
    AT = [sb("AT%d" % i, [128, 512], BF16) for i in range(2)]
    ktok = [sb("ktok%d" % i, [128, 512], BF16) for i in range(2)]
    retS = [sb("retS%d" % l, [128, 4, 256], F32) for l in range(L)]
    retSb = [sb("retSb%d" % l, [128, 4, 256], BF16) for l in range(L)]
    glaS = [sb("glaS%d" % l, [128, 4, 256], F32) for l in range(L)]
    glaSb = [sb("glaSb%d" % l, [128, 4, 256], BF16) for l in range(L)]
    gE = [sb("gE%d" % i, [128, T], F32) for i in range(2)]
    gEi = [sb("gEi%d" % i, [128, T], F32) for i in range(2)]
    gEs = [sb("gEs%d" % i, [128, T], F32) for i in range(2)]
    gaT = sb("gaT", [32, T], BF16)
    w2f = [sb("w2f%d" % l, [17, 512], F32) for l in range(L)]
    w2b = [sb("w2b%d" % l, [17, 512], BF16) for l in range(L)]
    KK = sb("KK", [128, 4, 128 + T], BF16)
    svt = sb("svt", [128, NT + 1, 256], BF16)
    ETb = [sb("ET%d" % i, [128, 512], BF16) for i in range(2)]
    PTs = sb("PTs", [128, 2, 4, 2, 128], BF16)
    den = sb("den", [128, 4, 128], F32)
    kkprev = [sb("kkprev%d" % l, [128, 4, 128], BF16) for l in range(L)]
    svprev = [sb("svprev%d" % l, [128, 256], BF16) for l in range(L)]
    xin = sb("xin", [128, D], F32)
    par = [sb("par%d" % l, [128, NPAR], F32) for l in range(L)]
    es = [sb("es%d" % l, [128, 8], F32) for l in range(L)]
    ct = {}
    for name, ap in cd.items():
        ct[name] = sb("c_" + name, list(ap.shape), ap.dtype)
    onesb = sb("onesb32", [32, T], BF16)

    NPS = 7
    pst = [st.enter_context(nc.psum_tensor("ps%d" % i, [128, 512], F32)) for i in range(NPS)]
    ptr = st.enter_context(nc.psum_tensor("ptr", [128, 1024], BF16))
    psbuf = [Buf("ps%d" % i) for i in range(NPS)]
    for i in range(NPS):
        S.psgen[i] = 0
    psrr = [0]

    def ps_alloc():
        i = psrr[0] % NPS
        psrr[0] += 1
        S.psgen[i] += 1
        return PsH(i, S.psgen[i], pst[i], psbuf[i])

    def mm(out, lhsT, rhs, start, stop, reads, writes):
        S.op(PE, lambda e: e.matmul(out, lhsT=lhsT, rhs=rhs, start=start, stop=stop), reads=reads, writes=writes)

    def tr(out, in_, ident, reads, writes):
        S.op(PE, lambda e: e.transpose(out=out, in_=in_, identity=ident), reads=reads, writes=writes)

    def act(out, in_, func, reads, writes, **kw):
        S.op(ACT, lambda e: e.activation(out=out, in_=in_, func=func, **kw), reads=reads, writes=writes)

    def tt(out, in0, in1, op, reads, writes, eng=DVE):
        S.op(eng, lambda e: e.tensor_tensor(out=out, in0=in0, in1=in1, op=op), reads=reads, writes=writes)

    def ts(out, in0, s1, s2, op0, op1, reads, writes, eng=DVE):
        if s2 is None:
            S.op(eng, lambda e: e.tensor_scalar(out=out, in0=in0, scalar1=s1, scalar2=None, op0=op0),
                 reads=reads, writes=writes)
        else:
            S.op(eng, lambda e: e.tensor_scalar(out=out, in0=in0, scalar1=s1, scalar2=s2, op0=op0, op1=op1),
                 reads=reads, writes=writes)

    def stt(out, in0, scalar, in1, op0, op1, reads, writes):
        S.op(DVE, lambda e: e.scalar_tensor_tensor(out=out, in0=in0, scalar=scalar, in1=in1, op0=op0, op1=op1),
             reads=reads, writes=writes)

    def vcopy(out, in_, reads, writes):
        S.op(DVE, lambda e: e.tensor_copy(out=out, in_=in_), reads=reads, writes=writes)

    def recip(out, in_, reads, writes):
        S.op(DVE, lambda e: e.reciprocal(out=out, in_=in_), reads=reads, writes=writes)

    dsem_c = S.new_dma_sem("dc")
    dsem_x = S.new_dma_sem("dx")
    dsem_o = S.new_dma_sem("do")

    def dma(out, in_, reads, writes, sem, eng=SP):
        return S.op(eng, lambda e: e.dma_start(out=out, in_=in_), reads=reads, writes=writes, dma_sem=sem)

    for name in cd:
        dma(ct[name][:], cd[name], [], [B("c", name)], S.new_dma_sem("dc_" + name))
    for l in range(n_layers):
        dma(par[l][:], par_d[l], [], [B("par", l)], S.new_dma_sem("dc_par%d" % l))
        dma(w2f[l][:], w2_d[l], [], [B("w2f", l)], S.new_dma_sem("dc_w2%d" % l))
        vcopy(w2b[l][:], w2f[l][:], [B("w2f", l)], [B("w2b", l)])
        act(es[l][:], par[l][:, P_SINK:P_SINK + 8], AF.Exp, [B("par", l)], [B("es", l)])
    S.op(DVE, lambda e: e.memset(onesb[:], 1.0), writes=[B("onesb")])
    identf, identb, ones = ct["ident_f"], ct["ident_b"], ct["ones_b"]
    Bif, Bib, Bones = B("c", "ident_f"), B("c", "ident_b"), B("c", "ones_b")

    wsem = [S.new_dma_sem("dw%d" % i) for i in range(NST)]
    NRING = NBF + 2 * NST
    rsem = [S.new_dma_sem("dr%d" % i) for i in range(NRING)]
    dws = S.new_dma_sem("dws")
    wlist = []
    wstate = {"dma": 0, "cast": 0, "i0": None, "nconv": 0}
    wst_bf = [w_[:, :].bitcast(BF16) for w_ in wst]

    def ring_slot(r):
        if r < NBF:
            return wbf[r][:, :], [B("wbf", r)]
        s_, h_ = (r - NBF) // 2, (r - NBF) % 2
        return wst_bf[s_][:, h_ * 2048:(h_ + 1) * 2048], [B("wsth", s_, h_)]

    def wst_bufs(slot):
        return [B("wsth", slot, 0), B("wsth", slot, 1)]

    def w_issue_dma(i):
        view, KC, ncols, off, conv = wlist[i]
        n = KC * ncols
        if conv:
            slot = i % NST
            dst = wst[slot][:, 0:n].rearrange("p (k n) -> p k n", k=KC)
            dma(dst, view, [], wst_bufs(slot), wsem[slot])
        else:
            if wstate["i0"] is None:
                wstate["i0"] = i
            r = (i - wstate["i0"]) % NRING
            tl, bb = ring_slot(r)
            dma(tl[:, 0:n], wscr_d[:, off:off + n], [B("wscr_all")], bb, rsem[r])

    def w_issue_cast(i):
        view, KC, ncols, off, conv = wlist[i]
        if not conv:
            return
        slot, bslot = i % NST, i % NBF
        n = KC * ncols
        if i % 5 in (0, 2):
            vcopy(wbf[bslot][:, 0:n], wst[slot][:, 0:n], wst_bufs(slot), [B("wbf", bslot)])
        else:
            act(wbf[bslot][:, 0:n], wst[slot][:, 0:n], AF.Copy, wst_bufs(slot), [B("wbf", bslot)])
        dma(wscr_d[:, off:off + n], wbf[bslot][:, 0:n], [B("wbf", bslot)], [B("wscr_all")], dws, eng=POOL)

    def wget(i):
        assert i == wstate.get("last", -1) + 1, ("weight groups must be consumed in list order", i, wstate.get("last"))
        wstate["last"] = i
        nconv = wstate.get("n_conv")
        if nconv is None:
            nconv = wstate["n_conv"] = sum(1 for w_ in wlist if w_[4])

        def dma_ok(j):
            if wlist[j][4]:
                return j < i + NST
            return i >= nconv and j < i + NRING - 1
        while wstate["cast"] < min(len(wlist), i + 2):
            while wstate["dma"] <= wstate["cast"] and wlist[wstate["dma"]][4]:
                w_issue_dma(wstate["dma"])
                wstate["dma"] += 1
            w_issue_cast(wstate["cast"])
            wstate["cast"] += 1
        while wstate["dma"] < len(wlist) and dma_ok(wstate["dma"]):
            w_issue_dma(wstate["dma"])
            wstate["dma"] += 1
        assert wstate["dma"] > i
        view, KC, ncols, off, conv = wlist[i]
        n = KC * ncols
        if conv:
            return wbf[i % NBF][:, 0:n].rearrange("p (k n) -> p k n", k=KC), B("wbf", i % NBF)
        tl, bb = ring_slot((i - wstate["i0"]) % NRING)
        return tl[:, 0:n].rearrange("p (k n) -> p k n", k=KC), bb[0]

    scr_off = {}

    def layer_groups(l, conv):
        g = {}
        win = win_d[l].rearrange("(kc p) n -> p kc n", p=128)
        cursor = [l * WPL]

        def add(name, view, KC, ncols):
            g[name] = len(wlist)
            wlist.append((view, KC, ncols, cursor[0], conv))
            cursor[0] += KC * ncols
            assert cursor[0] <= (l + 1) * WPL

        def add_in(name, c0, n):
            for i in range(0, n, 256):
                w = min(256, n - i)
                add((name, i // 256), win[:, :, c0 + i:c0 + i + w], 8, w)
        add_in("rq", C_RQ, 512); add_in("rk", C_RK, 512); add_in("rv", C_RV, 1024); add_in("rg", C_RG, 1024)
        for n in range(3):
            if n == 1:
                add_in("ga", C_GA, 16); add_in("gv", C_GV, 1024)
                for i_ in range(2):
                    add(("gq", i_), win[:, :, C_GQ + i_ * 256:C_GQ + (i_ + 1) * 256], 8, 256)
                    add(("gk", i_), win[:, :, C_GK + i_ * 256:C_GK + (i_ + 1) * 256], 8, 256)
                add_in("gg", C_GG, 1024)
            if n == 2:
                add_in("sk", C_SK, 256); add_in("sv", C_SV, 256); add_in("sq", C_SQ, 1024)
            wb = wbr_d[l, n].rearrange("(kc p) n -> p kc n", p=128)
            for i in range(4):
                add(("gate", n, i), win[:, :, C_GATE + n * 1024 + i * 256:C_GATE + n * 1024 + (i + 1) * 256], 8, 256)
                add(("br", n, i), wb[:, :, i * 256:(i + 1) * 256], 8, 256)
        wo = wout_d[l].rearrange("(kc p) n -> p kc n", p=128)
        for i in range(4):
            add(("wo", i), wo[:, :, i * 256:(i + 1) * 256], 8, 256)
        wgv = wg_d[l].rearrange("(kc p) n -> p kc n", p=128)
        wuv = wu_d[l].rearrange("(kc p) n -> p kc n", p=128)
        wdv = wd_d[l].rearrange("(kc p) n -> p kc n", p=128)
        for half in range(2):
            for i in range(6):
                w = 256 if i < 5 else 128
                c0 = half * 1408 + i * 256
                add(("fg", half, i), wgv[:, :, c0:c0 + w], 8, w)
                add(("fu", half, i), wuv[:, :, c0:c0 + w], 8, w)
            for c in range(8):
                add(("fd", half, c), wdv[:, half * 11:(half + 1) * 11, c * 128:(c + 1) * 128], 11, 128)
        return g

    def proj_fm(gidx, ncols_chunks, evac, src=None, srcB=None):
        if src is None:
            src, srcB = hT, [B("hT", c) for c in range(8)]
        wv, wb = wget(gidx)
        KC = wv.shape[1]
        ncols = wv.shape[2]
        j = 0
        c0 = 0
        while c0 < ncols:
            w = min(128, ncols - c0)
            p = ps_alloc()
            for kc in range(KC):
                if os.environ.get("KDBG_NOMM"):
                    continue
                mm(p.t[0:w, :], wv[:, kc, c0:c0 + w], src[:, kc, :], kc == 0, kc == KC - 1,
                   [wb, srcB[kc]], [p])
            if not os.environ.get("KDBG_NOEVAC"):
                evac(j, p, w)
            j += 1
            c0 += w

    def proj_tm(gidx, evac):
        wv, wb = wget(gidx)
        ncols = wv.shape[2]
        for t in range(NT):
            p = ps_alloc()
            for kc in range(8):
                mm(p.t[:, 0:ncols], hT[:, kc, t * 128:(t + 1) * 128], wv[:, kc, :], kc == 0, kc == 7,
                   [wb, B("hT", kc)], [p])
            evac(t, p, ncols)

    sq_rr = [0]

    def rmsnorm_stats(srcT, srcB, width_scale, eps):
        p = ps_alloc()
        for c in range(8):
            s = sq_rr[0] % 2
            sq_rr[0] += 1
            act(sqr[:, s, :], srcT[:, c, :], AF.Square, [srcB[c]], [B("sqr", s)])
            mm(p.t[:, :], ones[:, :], sqr[:, s, :], c == 0, c == 7, [Bones, B("sqr", s)], [p])
        act(tmpf[:], p.t[:, :], AF.Sqrt, [p, Beps(eps)], [B("tmpf")], scale=width_scale, bias=eps_ap(eps))
        recip(rstd[:], tmpf[:], [B("tmpf")], [B("rstd")])

    eps_tiles = {}

    def eps_ap(v):
        if v not in eps_tiles:
            t_ = sb("eps%d" % len(eps_tiles), [128, 1], F32)
            S.op(DVE, lambda e: e.memset(t_[:], float(v)), writes=[B("eps", v)])
            eps_tiles[v] = t_
        return eps_tiles[v][:, 0:1]

    for v_ in (1e-6, 1e-5, 1.0):
        eps_ap(v_)

    def Beps(v):
        return B("eps", v)

    def pre_norm(l, poff):
        rmsnorm_stats(xT, [B("xT", c) for c in range(8)], 1.0 / D, 1e-6)
        for c in range(8):
            stt(hT[:, c, :], xT[:, c, :], par[l][:, poff + c:poff + c + 1], rstd[:], ALU.mult, ALU.mult,
                [B("xT", c), B("par", l), B("rstd")], [B("hT", c)])

    def post_norm_residual(l, poff):
        rmsnorm_stats(osb, [B("osb", c) for c in range(8)], 1.0 / D, 1e-6)
        for c in range(8):
            stt(osb[:, c, :], osb[:, c, :], par[l][:, poff + c:poff + c + 1], rstd[:], ALU.mult, ALU.mult,
                [B("osb", c), B("par", l), B("rstd")], [B("osb", c)])
            tt(xT[:, c, :], xT[:, c, :], osb[:, c, :], ALU.add, [B("xT", c), B("osb", c)], [B("xT", c)])

    _act = act

    def ret_branch(l, g, first_mt):
        DTt, XIt, ZTt = ct["ret_dt"], ct["ret_xi"], ct["ret_zt"]
        XIb = XIt[:, :].rearrange("p (h i) -> p h i", h=4)
        ZTb = ZTt[:, :].rearrange("p (h i) -> p h i", h=4)

        def ev_q(gi):
            def f(j, p, w):
                h = gi * 2 + j
                for t_ in range(NT):
                    tt(qT[:, h, t_ * 128:(t_ + 1) * 128], p.t[:, t_ * 128:(t_ + 1) * 128], XIt[:, h * 128:(h + 1) * 128], ALU.mult,
                       [p, B("c", "ret_xi")], [B("qT", h)])
            return f

        def ev_k(gi):
            def f(j, p, w):
                h = gi * 2 + j
                act(kT[:, h, :], p.t[:, :], AF.Copy, [p], [B("kT", h)])
                for t_ in range(NT):
                    tt(kzT[:, h, t_ * 128:(t_ + 1) * 128], p.t[:, t_ * 128:(t_ + 1) * 128], ZTt[:, h * 128:(h + 1) * 128], ALU.mult,
                       [p, B("c", "ret_zt")], [B("kzT", h)])
            return f

        def ev_q2(gi):
            def f(j, p, w):
                h = gi * 2 + j
                act(obf[:, h, :], p.t[:, :], AF.Copy, [p], [B("obf", h)])
                ev_q(gi)(j, p, w)
            return f
        for gi in range(2):
            proj_fm(g[("rq", gi)], 2, ev_q2(gi))
        for gi in range(2):
            proj_fm(g[("rk", gi)], 2, ev_k(gi))

        dbg_dump("r1", xT, [B("xT", c_) for c_ in range(8)])

        def ev_v(gi):
            def f(t, p, n):
                act(vtok[:, t, gi * 256:(gi + 1) * 256], p.t[:, 0:256], AF.Copy, [p], [B("vtok", t)])
            return f
        for gi in range(4):
            proj_tm(g[("rv", gi)], ev_v(gi))
        dbg_dump("r2", xT, [B("xT", c_) for c_ in range(8)])
        if first_mt:
            S.op(DVE, lambda e: e.memset(retS[l][:], 0.0), writes=[B("retS", l)])
            S.op(DVE, lambda e: e.memset(retSb[l][:], 0.0), writes=[B("retSb", l)])
        def ret_stage_a(t):
            tok = slice(t * 128, (t + 1) * 128)
            a = t % 2
            p = ps_alloc()
            for h in range(4):
                mm(p.t[:, h * 128:(h + 1) * 128], kT[:, h, tok], obf[:, h, tok], True, True,
                   [B("kT", h), B("obf", h)], [p])
            tt(AT[a][:], p.t[:, :], DTt[:, :], ALU.mult, [p, B("c", "ret_dt")], [B("AT", a)])
            dbg_dump("r3", xT, [B("xT", c_) for c_ in range(8)])
            for h in range(4):
                tr(ptr[:, h * 128:(h + 1) * 128], kzT[:, h, tok], identb[:], [B("kzT", h), Bib], [B("ptr", 0)])
            act(ktok[a][:], ptr[:, 0:512], AF.Copy, [B("ptr", 0)], [B("ktok", a)])
            dbg_dump("r4", xT, [B("xT", c_) for c_ in range(8)])

        def ret_stage_b(t):
            tok = slice(t * 128, (t + 1) * 128)
            a = t % 2
            for hh in range(2):
                p = ps_alloc()
                for h2 in range(2):
                    h = hh * 2 + h2
                    for ec in range(2):
                        o_ = p.t[:, (h2 * 2 + ec) * 128:(h2 * 2 + ec + 1) * 128]
                        mm(o_, vtok[:, t, h * 256 + ec * 128:h * 256 + (ec + 1) * 128], AT[a][:, h * 128:(h + 1) * 128],
                           True, False, [B("vtok", t), B("AT", a)], [p])
                        mm(o_, retSb[l][:, h, ec * 128:(ec + 1) * 128], qT[:, h, tok], False, True,
                           [B("retSb", l), B("qT", h)], [p])
                act(osb[:, hh * 4:(hh + 1) * 4, tok], p.t[:, :].rearrange("p (c i) -> p c i", c=4), AF.Copy,
                    [p], [B("osb", c) for c in range(hh * 4, hh * 4 + 4)])
            dbg_dump("r5", xT, [B("xT", c_) for c_ in range(8)])
            for hh in range(2):
                p = ps_alloc()
                for h2 in range(2):
                    h = hh * 2 + h2
                    mm(p.t[:, h2 * 256:(h2 + 1) * 256], ktok[a][:, h * 128:(h + 1) * 128], vtok[:, t, h * 256:(h + 1) * 256],
                       True, True, [B("ktok", a), B("vtok", t)], [p])
                for h2 in range(2):
                    h = hh * 2 + h2
                    stt(retS[l][:, h, :], retS[l][:, h, :], RET_G128[h], p.t[:, h2 * 256:(h2 + 1) * 256], ALU.mult, ALU.add,
                        [B("retS", l), p], [B("retS", l)])
                act(retSb[l][:, hh * 2:hh * 2 + 2, :], retS[l][:, hh * 2:hh * 2 + 2, :], AF.Copy,
                    [B("retS", l)], [B("retSb", l)])

        ret_stage_a(0)
        for t in range(NT):
            if t + 1 < NT:
                ret_stage_a(t + 1)
            ret_stage_b(t)
        dbg_dump("r6", xT, [B("xT", c_) for c_ in range(8)])
        def ev_gs(gi):
            def f(j, p, w):
                c = gi * 2 + j
                act(sgall[:, c, :], p.t[:, :], AF.Silu, [p], [B("sgall", c)])
            return f

        def gn_head(h):
            pm = ps_alloc()
            pq = ps_alloc()
            for ec in range(2):
                c = h * 2 + ec
                s = sq_rr[0] % 2
                sq_rr[0] += 1
                act(sqr[:, s, :], osb[:, c, :], AF.Copy, [B("osb", c)], [B("sqr", s)])
                mm(pm.t[:, :], ones[:, :], sqr[:, s, :], ec == 0, ec == 1, [Bones, B("sqr", s)], [pm])
                s = sq_rr[0] % 2
                sq_rr[0] += 1
                act(sqr[:, s, :], osb[:, c, :], AF.Square, [B("osb", c)], [B("sqr", s)])
                mm(pq.t[:, :], ones[:, :], sqr[:, s, :], ec == 0, ec == 1, [Bones, B("sqr", s)], [pq])
            act(tmpf[:], pm.t[:, :], AF.Square, [pm], [B("tmpf")], scale=1.0 / 256)
            stt(tmpf[:], pq.t[:, :], 1.0 / 256, tmpf[:], ALU.mult, ALU.subtract, [pq, B("tmpf")], [B("tmpf")])
            act(tmpf[:], tmpf[:], AF.Sqrt, [B("tmpf"), Beps(1e-5)], [B("tmpf")], bias=eps_ap(1e-5))
            recip(rstd[:], tmpf[:], [B("tmpf")], [B("rstd")])
            stt(tmpf[:], pm.t[:, :], -1.0 / 256, rstd[:], ALU.mult, ALU.mult, [pm, B("rstd")], [B("tmpf")])
            for ec in range(2):
                c = h * 2 + ec
                tt(osb[:, c, :], osb[:, c, :], rstd[:], ALU.mult, [B("osb", c), B("rstd")], [B("osb", c)])
                tt(osb[:, c, :], osb[:, c, :], tmpf[:], ALU.add, [B("osb", c), B("tmpf")], [B("osb", c)])
                act(osb[:, c, :], osb[:, c, :], AF.Identity, [B("osb", c), B("par", l)], [B("osb", c)],
                    scale=par[l][:, P_RETW + c:P_RETW + c + 1], bias=par[l][:, P_RETB + c:P_RETB + c + 1])
                tt(obf[:, c, :], osb[:, c, :], sgall[:, c, :], ALU.mult, [B("osb", c), B("sgall", c)], [B("obf", c)])

        proj_fm(g[("rg", 0)], 2, ev_gs(0))
        for h in range(4):
            if h + 1 < 4:
                proj_fm(g[("rg", h + 1)], 2, ev_gs(h + 1))
            gn_head(h)

    def gla_branch2(l, g, first_mt):
        TRI, SLm = ct["gla_tri"], ct["gla_sl"]
        vcopy(gaT[:, :], onesb[:, :], [B("onesb")], [B("gaT")])

        def ev_ga(j, p, w):
            act(gaT[0:16, :], p.t[0:16, :], AF.Copy, [p], [B("gaT")])
        proj_fm(g[("ga", 0)], 1, ev_ga)

        def ev_v(gi):
            def f(t, p, n):
                act(vtok[:, t, gi * 256:(gi + 1) * 256], p.t[:, 0:256], AF.Copy, [p], [B("vtok", t)])
            return f
        for gi in range(4):
            proj_tm(g[("gv", gi)], ev_v(gi))
        if first_mt:
            S.op(DVE, lambda e: e.memset(glaS[l][:], 0.0), writes=[B("glaS", l, h_) for h_ in range(4)])
            S.op(DVE, lambda e: e.memset(glaSb[l][:], 0.0), writes=[B("glaSb", l, h_) for h_ in range(4)])
        for t in range(NT):
            pz = ps_alloc()
            mm(pz.t[:, :], gaT[0:17, t * 128:(t + 1) * 128], w2b[l][0:17, :], True, True,
               [B("gaT"), B("w2b", l)], [pz])
            act(osb[:, t, :], pz.t[:, :], AF.Exp, [pz], [B("osb", t)], scale=-1.0)
            act(osb[:, t, :], osb[:, t, :], AF.Ln, [B("osb", t), Beps(1.0)], [B("osb", t)], bias=eps_ap(1.0))
            ts(osb[:, t, :], osb[:, t, :], -1.0 / 16, -1.0, ALU.mult, ALU.max, [B("osb", t)], [B("osb", t)])
        for gi in range(2):
            for j in range(2):
                h = gi * 2 + j
                pb_ = ps_alloc()
                pl_ = ps_alloc()
                for t in range(NT):
                    mm(pb_.t[:, t * 128:(t + 1) * 128], osb[:, t, h * 128:(h + 1) * 128], TRI[:, :], True, True,
                       [B("osb", t), B("c", "gla_tri")], [pb_])
                    mm(pl_.t[:, t * 128:(t + 1) * 128], osb[:, t, h * 128:(h + 1) * 128], SLm[:, :], True, True,
                       [B("osb", t), B("c", "gla_sl")], [pl_])
                act(gE[j][:], pb_.t[:, :], AF.Exp, [pb_], [B("gE", j)])
                act(gEi[j][:], pb_.t[:, :], AF.Exp, [pb_], [B("gEi", j)], scale=-1.0)
                act(gEs[j][:], pl_.t[:, :], AF.Exp, [pl_], [B("gEs", j)])
                vcopy(gdec[:, h, :], gE[j][:, :].rearrange("p (b i) -> p b i", i=64)[:, :, 63], [B("gE", j)], [B("gdec")])
            wv, wb = wget(g[("gq", gi)])
            for j in range(2):
                h = gi * 2 + j
                p = ps_alloc()
                for kc in range(8):
                    mm(p.t[:, :], wv[:, kc, j * 128:(j + 1) * 128], hT[:, kc, :], kc == 0, kc == 7, [wb, B("hT", kc)], [p])
                stt(qT[:, h, :], p.t[:, :], 128.0 ** -0.5, gE[j][:], ALU.mult, ALU.mult, [p, B("gE", j)], [B("qT", h)])
            wv, wb = wget(g[("gk", gi)])
            for j in range(2):
                h = gi * 2 + j
                p = ps_alloc()
                for kc in range(8):
                    mm(p.t[:, :], wv[:, kc, j * 128:(j + 1) * 128], hT[:, kc, :], kc == 0, kc == 7, [wb, B("hT", kc)], [p])
                tt(kT[:, h, :], p.t[:, :], gEi[j][:], ALU.mult, [p, B("gEi", j)], [B("kT", h)])
                tt(kzT[:, h, :], p.t[:, :], gEs[j][:], ALU.mult, [p, B("gEs", j)], [B("kzT", h)])
        TRIb = TRI[:, :].unsqueeze(1).to_broadcast([128, 4, 128])

        def gla_stage_a(t):
            tok = slice(t * 128, (t + 1) * 128)
            a = t % 2
            p = ps_alloc()
            for h in range(4):
                mm(p.t[:, h * 128:(h + 1) * 128], kT[:, h, tok], qT[:, h, tok], True, True,
                   [B("kT", h), B("qT", h)], [p])
            tt(AT[a][:].rearrange("p (h i) -> p h i", h=4), p.t[:, :].rearrange("p (h i) -> p h i", h=4), TRIb, ALU.mult,
               [p, B("c", "gla_tri")], [B("AT", a)])
            for h in range(4):
                tr(ptr[:, h * 128:(h + 1) * 128], kzT[:, h, tok], identb[:], [B("kzT", h), Bib], [B("ptr", 0)])
            act(ktok[a][:], ptr[:, 0:512], AF.Copy, [B("ptr", 0)], [B("ktok", a)])

        def gla_stage_b(t):
            tok = slice(t * 128, (t + 1) * 128)
            a = t % 2
            po = [ps_alloc(), ps_alloc()]
            for blk in range(2):
                bt = slice(t * 128 + blk * 64, t * 128 + blk * 64 + 64)
                for h in range(4):
                    for ec in range(2):
                        base = ((h % 2) * 2 + ec) * 128 + blk * 64
                        o_ = po[h // 2].t[:, base:base + 64]
                        mm(o_, vtok[:, t, h * 256 + ec * 128:h * 256 + (ec + 1) * 128],
                           AT[a][:, h * 128 + blk * 64:h * 128 + blk * 64 + 64],
                           True, False, [B("vtok", t), B("AT", a)], [po[h // 2]])
                        mm(o_, glaSb[l][:, h, ec * 128:(ec + 1) * 128], qT[:, h, bt],
                           False, True, [B("glaSb", l, h), B("qT", h)], [po[h // 2]])
                for hh in range(2):
                    pk = ps_alloc()
                    for h2 in range(2):
                        h = hh * 2 + h2
                        mm(pk.t[:, h2 * 256:(h2 + 1) * 256], ktok[a][blk * 64:(blk + 1) * 64, h * 128:(h + 1) * 128],
                           vtok[blk * 64:(blk + 1) * 64, t, h * 256:(h + 1) * 256], True, True,
                           [B("ktok", a), B("vtok", t)], [pk])
                    for h2 in range(2):
                        h = hh * 2 + h2
                        stt(glaS[l][:, h, :], glaS[l][:, h, :], gdec[:, h, t * 2 + blk:t * 2 + blk + 1],
                            pk.t[:, h2 * 256:(h2 + 1) * 256], ALU.mult, ALU.add,
                            [B("glaS", l, h), B("gdec"), pk], [B("glaS", l, h)])
                        act(glaSb[l][:, h, :], glaS[l][:, h, :], AF.Copy, [B("glaS", l, h)], [B("glaSb", l, h)])
            for hh in range(2):
                act(obf_raw(hh, tok), po[hh].t[:, :].rearrange("p (c i) -> p c i", c=4), AF.Copy,
                    [po[hh]], [B("osb", c) for c in range(hh * 4, hh * 4 + 4)])

        gla_stage_a(0)
        for t in range(NT):
            if t + 1 < NT:
                gla_stage_a(t + 1)
            gla_stage_b(t)
        def ev_gs(gi):
            def f(j, p, w):
                c = gi * 2 + j
                act(sgall[:, c, :], p.t[:, :], AF.Silu, [p], [B("sgall", c)])
            return f

        def rms_head(h):
            pq = ps_alloc()
            for ec in range(2):
                c = h * 2 + ec
                s = sq_rr[0] % 2
                sq_rr[0] += 1
                act(sqr[:, s, :], osb[:, c, :], AF.Square, [B("osb", c)], [B("sqr", s)])
                mm(pq.t[:, :], ones[:, :], sqr[:, s, :], ec == 0, ec == 1, [Bones, B("sqr", s)], [pq])
            act(tmpf[:], pq.t[:, :], AF.Sqrt, [pq, Beps(1e-6)], [B("tmpf")], scale=1.0 / 256, bias=eps_ap(1e-6))
            recip(rstd[:], tmpf[:], [B("tmpf")], [B("rstd")])
            for ec in range(2):
                c = h * 2 + ec
                stt(osb[:, c, :], osb[:, c, :], par[l][:, P_GLAW + c:P_GLAW + c + 1], rstd[:], ALU.mult, ALU.mult,
                    [B("osb", c), B("par", l), B("rstd")], [B("osb", c)])
                tt(obf[:, c, :], osb[:, c, :], sgall[:, c, :], ALU.mult, [B("osb", c), B("sgall", c)], [B("obf", c)])

        proj_fm(g[("gg", 0)], 2, ev_gs(0))
        for h in range(4):
            if h + 1 < 4:
                proj_fm(g[("gg", h + 1)], 2, ev_gs(h + 1))
            rms_head(h)

    gdec = sb("gdec", [128, 4, 2 * NT], F32)

    def obf_raw(hh, tok):
        return osb[:, hh * 4:(hh + 1) * 4, tok]

    def swa_branch(l, g, first_mt):
        MASK = ct["swa_mask"][:, :].rearrange("p (s c k q) -> p s c k q", s=2, c=8, k=2)
        wv, wb = wget(g[("sk", 0)])
        if first_mt:
            S.op(DVE, lambda e: e.memset(KK[:, :, 0:128], 0.0), writes=[B("KK", gg) for gg in range(4)])
            S.op(DVE, lambda e: e.memset(svt[:, 0, :], 0.0), writes=[B("svt", 0)])
        else:
            vcopy(KK[:, :, 0:128], kkprev[l][:, :, :], [B("kkprev", l)], [B("KK", gg) for gg in range(4)])
            vcopy(svt[:, 0, :], svprev[l][:, :], [B("svprev", l)], [B("svt", 0)])
        for gg in range(4):
            p = ps_alloc()
            for half in range(2):
                for kc in range(8):
                    mm(p.t[half * 64:(half + 1) * 64, :], wv[:, kc, gg * 64:(gg + 1) * 64], hT[:, kc, :], kc == 0, kc == 7,
                       [wb, B("hT", kc)], [p])
            act(KK[:, gg, 128:128 + T], p.t[:, :], AF.Copy, [p], [B("KK", gg)])
        wv, wb = wget(g[("sv", 0)])
        for t in range(NT):
            p = ps_alloc()
            for kc in range(8):
                mm(p.t[:, 0:256], hT[:, kc, t * 128:(t + 1) * 128], wv[:, kc, :], kc == 0, kc == 7, [wb, B("hT", kc)], [p])
            act(svt[:, t + 1, :], p.t[:, 0:256], AF.Copy, [p], [B("svt", t + 1)])
        def qdst(c):
            return (qT, "qT", c) if c < 4 else (kT, "kT", c - 4)

        def ev_q(gi):
            def f(j, p, w):
                c = gi * 2 + j
                tl, nm, ci = qdst(c)
                act(tl[:, ci, :], p.t[:, :], AF.Copy, [p], [B(nm, ci)])
            return f
        for gi in range(4):
            proj_fm(g[("sq", gi)], 2, ev_q(gi))
        vcopy(kkprev[l][:, :, :], KK[:, :, T:T + 128], [B("KK", gg) for gg in range(4)], [B("kkprev", l)])
        vcopy(svprev[l][:, :], svt[:, NT, :], [B("svt", NT)], [B("svprev", l)])
        esb = es[l]
        for t in range(NT):
            tok = slice(t * 128, (t + 1) * 128)
            kts = [1] if (first_mt and t == 0) else [0, 1]
            for half in range(2):
                for s in range(2):
                    for pair in range(2):
                        p = ps_alloc()
                        for ci2 in range(2):
                            cc = pair * 2 + ci2
                            c = half * 4 + cc
                            tl, nm, ci = qdst(c)
                            gkv = c // 2
                            for kt in kts:
                                kcols = slice(t * 128 + kt * 128, t * 128 + kt * 128 + 128)
                                mm(p.t[:, (ci2 * 2 + kt) * 128:(ci2 * 2 + kt + 1) * 128], KK[s * 64:(s + 1) * 64, gkv, kcols],
                                   tl[s * 64:(s + 1) * 64, ci, tok], True, True, [B("KK", gkv), B(nm, ci)], [p])
                        e_ = pair
                        c0 = half * 4 + pair * 2
                        if len(kts) == 2:
                            act(ETb[e_][:], p.t[:, :], AF.Exp, [p], [B("ET", e_)], scale=0.125)
                            tt(PTs[:, s, pair * 2:pair * 2 + 2, :, :], ETb[e_][:].rearrange("p (h k q) -> p h k q", h=2, k=2),
                               MASK[:, s, c0:c0 + 2, :, :], ALU.mult, [B("ET", e_), B("c", "swa_mask")], [B("PTs", s, pair)])
                        else:
                            for ci2 in range(2):
                                act(ETb[e_][:, ci2 * 256 + 128:ci2 * 256 + 256], p.t[:, ci2 * 256 + 128:ci2 * 256 + 256], AF.Exp,
                                    [p], [B("ET", e_)], scale=0.125)
                            tt(PTs[:, s, pair * 2:pair * 2 + 2, 1, :],
                               ETb[e_][:].rearrange("p (h k q) -> p h k q", h=2, k=2)[:, :, 1, :],
                               MASK[:, s, c0:c0 + 2, 1, :], ALU.mult, [B("ET", e_), B("c", "swa_mask")], [B("PTs", s, pair)])
                po = ps_alloc()
                pd = ps_alloc()
                for cc in range(4):
                    c = half * 4 + cc
                    gkv = c // 2
                    for s in range(2):
                        for i, kt in enumerate(kts):
                            mm(po.t[s * 64:(s + 1) * 64, cc * 128:(cc + 1) * 128], svt[:, t + kt, gkv * 64:(gkv + 1) * 64],
                               PTs[:, s, cc, kt, :], i == 0, i == len(kts) - 1, [B("svt", t + kt), B("PTs", s, cc // 2)], [po])
                        for i, kt in enumerate(kts):
                            mm(pd.t[s * 64:(s + 1) * 64, cc * 128:(cc + 1) * 128], ones[:, 0:64],
                               PTs[:, s, cc, kt, :], i == 0, i == len(kts) - 1, [Bones, B("PTs", s, cc // 2)], [pd])
                tt(den[:, :, :], pd.t[:, :].rearrange("p (c q) -> p c q", c=4),
                   esb[:, half * 4:(half + 1) * 4].unsqueeze(2).to_broadcast([128, 4, 128]), ALU.add,
                   [pd, B("es", l)], [B("den")])
                recip(den[:, :, :], den[:, :, :], [B("den")], [B("den")])
                tt(obf[:, half * 4:(half + 1) * 4, tok], po.t[:, :].rearrange("p (c q) -> p c q", c=4), den[:, :, :], ALU.mult,
                   [po, B("den")], [B("obf", c) for c in range(half * 4, half * 4 + 4)])

    def branch_merge(l, g, n):
        for i in range(4):
            wvg, wbg = wget(g[("gate", n, i)])
            gates = []
            for j in range(2):
                c = i * 2 + j
                p = ps_alloc()
                for kc in range(8):
                    mm(p.t[:, :], wvg[:, kc, j * 128:(j + 1) * 128], hT[:, kc, :], kc == 0, kc == 7, [wbg, B("hT", kc)], [p])
                s = c % 2
                act(sgr[:, s, :], p.t[:, :], AF.Sigmoid, [p], [B("sgr", s)])
            wvb, wbb = wget(g[("br", n, i)])
            for j in range(2):
                c = i * 2 + j
                s = c % 2
                p = ps_alloc()
                for kc in range(8):
                    mm(p.t[:, :], wvb[:, kc, j * 128:(j + 1) * 128], obf[:, kc, :], kc == 0, kc == 7, [wbb, B("obf", kc)], [p])
                if n == 0:
                    tt(merged[:, c, :], p.t[:, :], sgr[:, s, :], ALU.mult, [p, B("sgr", s)], [B("merged", c)])
                else:
                    tt(tmpf[:], p.t[:, :], sgr[:, s, :], ALU.mult, [p, B("sgr", s)], [B("tmpf")])
                    tt(merged[:, c, :], merged[:, c, :], tmpf[:], ALU.add, [B("merged", c), B("tmpf")], [B("merged", c)])

    def out_proj(l, g):
        for i in range(4):
            wv, wb = wget(g[("wo", i)])
            for j in range(2):
                c = i * 2 + j
                p = ps_alloc()
                for kc in range(8):
                    mm(p.t[:, :], wv[:, kc, j * 128:(j + 1) * 128], merged[:, kc, :], kc == 0, kc == 7,
                       [wb, B("merged", kc)], [p])
                act(osb[:, c, :], p.t[:, :], AF.Copy, [p], [B("osb", c)])

    def fdst(j):
        return (obf, "obf", j) if j < 8 else (kzT, "kzT", j - 8)

    def ffn(l, g):
        for half in range(2):
            for i in range(6):
                wvg, wbg = wget(g[("fg", half, i)])
                nj = wvg.shape[2] // 128
                for jj in range(nj):
                    j = i * 2 + jj
                    p = ps_alloc()
                    for kc in range(8):
                        mm(p.t[:, :], wvg[:, kc, jj * 128:(jj + 1) * 128], hT[:, kc, :], kc == 0, kc == 7, [wbg, B("hT", kc)], [p])
                    s = j % 2
                    act(sgr[:, s, :], p.t[:, :], AF.Silu, [p], [B("sgr", s)])
                wvu, wbu = wget(g[("fu", half, i)])
                for jj in range(nj):
                    j = i * 2 + jj
                    s = j % 2
                    tl, nm, ci = fdst(j)
                    p = ps_alloc()
                    for kc in range(8):
                        mm(p.t[:, :], wvu[:, kc, jj * 128:(jj + 1) * 128], hT[:, kc, :], kc == 0, kc == 7, [wbu, B("hT", kc)], [p])
                    tt(tl[:, ci, :], p.t[:, :], sgr[:, s, :], ALU.mult, [p, B("sgr", s)], [B(nm, ci)])
            for c in range(8):
                wv, wb = wget(g[("fd", half, c)])
                p = ps_alloc()
                for j in range(11):
                    tl, nm, ci = fdst(j)
                    mm(p.t[:, :], wv[:, j, :], tl[:, ci, :], j == 0, j == 10, [wb, B(nm, ci)], [p])
                if half == 0:
                    act(osb[:, c, :], p.t[:, :], AF.Copy, [p], [B("osb", c)])
                else:
                    tt(osb[:, c, :], osb[:, c, :], p.t[:, :], ALU.add, [B("osb", c), p], [B("osb", c)])

    def load_x(sq, mt):
        for t in range(NT):
            r0 = mt * T + t * 128
            dma(xin[:], x_d[sq, r0:r0 + 128, :], [], [B("xin")], dsem_x, eng=POOL)
            for hh in range(2):
                p = ps_alloc()
                for c4 in range(4):
                    c = hh * 4 + c4
                    tr(p.t[:, c4 * 128:(c4 + 1) * 128], xin[:, c * 128:(c + 1) * 128], identf[:], [B("xin"), Bif], [p])
                act(xT[:, hh * 4:(hh + 1) * 4, t * 128:(t + 1) * 128], p.t[:, :].rearrange("p (c i) -> p c i", c=4), AF.Copy,
                    [p], [B("xT", c) for c in range(hh * 4, hh * 4 + 4)])

    out_deps = []

    def store_x(sq, mt):
        for t in range(NT):
            r0 = mt * T + t * 128
            for hh in range(2):
                p = ps_alloc()
                for c4 in range(4):
                    c = hh * 4 + c4
                    tr(p.t[:, c4 * 128:(c4 + 1) * 128], xT[:, c, t * 128:(t + 1) * 128], identf[:], [B("xT", c), Bif], [p])
                act(xin[:, hh * 512:(hh + 1) * 512], p.t[:, :], AF.Copy, [p], [B("xin")])
            out_deps.append(dma(out_d[sq, r0:r0 + 128, :], xin[:], [B("xin")], [], dsem_o, eng=POOL))

    class StopBuild(Exception):
        pass

    def dbg_dump(stage, tile, names):
        if not dbg or dbg.get("stage") != stage:
            return
        dsem_d = S.new_dma_sem("ddbg")
        if tile.dtype == BF16:
            dbgt = sb("dbgt", [128, 4, T], F32)
            for hf in range(2):
                vcopy(dbgt[:], tile[:, hf * 4:(hf + 1) * 4, :], names, [B("dbgt")])
                dep = dma(dbg_d[:, hf * 4 * T:(hf + 1) * 4 * T], dbgt[:].rearrange("p c t -> p (c t)"), [B("dbgt")], [], dsem_d)
        else:
            dep = dma(dbg_d, tile[:].rearrange("p c t -> p (c t)"), names, [], dsem_d)
        out_deps.append(dep)
        raise StopBuild()

    groups = {}
    plan = []
    for sq in range(n_seq):
        for mt in range(n_mt):
            for l in range(n_layers):
                groups[(sq, mt, l)] = layer_groups(l, conv=(sq == 0 and mt == 0))
    only = (dbg or {}).get("only")
    c8 = list(range(8))
    try:
      for sq in range(n_seq):
        for mt in range(n_mt):
            S.phase = 'load_x'
            load_x(sq, mt)
            dbg_dump("x", xT, [B("xT", c) for c in c8])
            for l in range(n_layers):
                g = groups[(sq, mt, l)]
                first = (mt == 0)
                S.phase = 'pre_norm_P_MIXPRE'
                pre_norm(l, P_MIXPRE)
                dbg_dump("h", hT, [B("hT", c) for c in c8])
                S.phase = 'ret_branch'
                ret_branch(l, g, first)
                dbg_dump("o_ret", obf, [B("obf", c) for c in c8])
                S.phase = 'branch_merge_0'
                branch_merge(l, g, 0)
                S.phase = 'gla_branch2'
                gla_branch2(l, g, first)
                dbg_dump("o_gla", obf, [B("obf", c) for c in c8])
                S.phase = 'branch_merge_1'
                branch_merge(l, g, 1)
                S.phase = 'swa_branch'
                swa_branch(l, g, first)
                dbg_dump("o_swa", obf, [B("obf", c) for c in c8])
                S.phase = 'branch_merge_2'
                branch_merge(l, g, 2)
                dbg_dump("merged", merged, [B("merged", c) for c in c8])
                S.phase = 'out_proj'
                out_proj(l, g)
                S.phase = 'post_norm_residual_P_MIXPOST'
                post_norm_residual(l, P_MIXPOST)
                dbg_dump("x1", xT, [B("xT", c) for c in c8])
                S.phase = 'pre_norm_P_FFNPRE'
                pre_norm(l, P_FFNPRE)
                S.phase = 'ffn'
                ffn(l, g)
                S.phase = 'post_norm_residual_P_FFNPOST'
                post_norm_residual(l, P_FFNPOST)
            S.phase = 'store_x'
            store_x(sq, mt)
    except StopBuild:
        pass
    S.final_wait(POOL, out_deps)
    S.emit()
    st.close()
    return nc, S


def host_inputs(inputs):
    f = np.float32
    shared = dict(make_consts())
    for k in ("w_in", "w_branch", "w_out", "ffn_w_gate", "ffn_w_up", "ffn_w_down"):
        shared[k] = np.ascontiguousarray(np.asarray(inputs[k], dtype=f))
    par = np.zeros((L, 128, NPAR), dtype=f)
    for l in range(L):
        par[l, :, P_MIXPRE:P_MIXPRE + 8] = col(inputs["norm_mix_pre"][l])
        par[l, :, P_MIXPOST:P_MIXPOST + 8] = col(inputs["norm_mix_post"][l])
        par[l, :, P_FFNPRE:P_FFNPRE + 8] = col(inputs["norm_ffn_pre"][l])
        par[l, :, P_FFNPOST:P_FFNPOST + 8] = col(inputs["norm_ffn_post"][l])
        par[l, :, P_RETW:P_RETW + 8] = col(inputs["ret_norm_w"][l])
        par[l, :, P_RETB:P_RETB + 8] = col(inputs["ret_norm_b"][l])
        par[l, :, P_GLAW:P_GLAW + 8] = col(inputs["gla_norm_w"][l])
        sk = np.asarray(inputs["attn_sinks"][l], dtype=f)
        par[l, :, P_SINK:P_SINK + 8] = sk.reshape(8, 2).T[np.arange(128) // 64, :]
    shared["par"] = par
    w2 = np.concatenate([np.asarray(inputs["gla_w_alpha2"], dtype=f),
                         np.asarray(inputs["gla_b_alpha"], dtype=f)[:, None, :]], axis=1)
    shared["w2aug"] = np.ascontiguousarray(w2)
    return shared


_CACHE = {}


def kernel(**inputs):
    x = np.asarray(inputs["x"], dtype=np.float32)
    n = 8
    shared = host_inputs(inputs)
    if "nc" not in _CACHE:
        _CACHE["nc"] = build()[0]
    nc = _CACHE["nc"]
    in_maps = []
    for i in range(n):
        m = dict(shared)
        m["x"] = np.ascontiguousarray(x[2 * i:2 * i + 2])
        in_maps.append(m)
    res = run_bass_kernel_spmd(nc, in_maps, core_ids=list(range(n)))
    out = np.concatenate([np.asarray(r["out"], dtype=np.float32) for r in res.results], axis=0)
    return out
```

```python
import contextlib
import os
import numpy as np
import ml_dtypes
import concourse.bass as bass
import concourse.mybir as mybir
from concourse.bass_utils import run_bass_kernel_spmd

F32 = mybir.dt.float32
BF16 = mybir.dt.bfloat16
AF = mybir.ActivationFunctionType
ALU = mybir.AluOpType

PE, ACT, DVE, POOL, SP = "pe", "act", "dve", "pool", "sp"
ENGS = (PE, ACT, DVE, POOL, SP)

D = 1024
SEQ = 2048
L = 2
T = 512
NT = T // 128
DIN = 10768
DFF = 2816
C_RQ, C_RK, C_RV, C_RG = 0, 512, 1024, 2048
C_GQ, C_GK, C_GV, C_GG, C_GA = 3072, 3584, 4096, 5120, 6144
C_SQ, C_SK, C_SV, C_GATE = 6160, 7184, 7440, 7696
NPAR = 64
WPL = (D * DIN + 3 * D * D + D * D + 3 * D * DFF) // 128
P_MIXPRE, P_MIXPOST, P_FFNPRE, P_FFNPOST, P_RETW, P_RETB, P_GLAW, P_SINK = 0, 8, 16, 24, 32, 40, 48, 56


class Buf:
    __slots__ = ("name", "w", "r")

    def __init__(self, name=""):
        self.name = name
        self.w = None
        self.r = {}


class PsH:
    __slots__ = ("bank", "gen", "t", "buf")

    def __init__(self, bank, gen, t, buf):
        self.bank, self.gen, self.t, self.buf = bank, gen, t, buf


class Sched:
    def __init__(self, nc):
        self.nc = nc
        self.q = {e: [] for e in ENGS}
        self.cnt = {e: 0 for e in ENGS}
        self.seen = {e: {} for e in ENGS}
        self.dma_sems = []
        self.nops = 0
        self.nwaits = 0
        self.psgen = {}
        self.phase = ''
        self.pe_phase = []

    def new_dma_sem(self, name):
        self.dma_sems.append(name)
        self.cnt[name] = 0
        return name

    def _buf(self, b):
        if isinstance(b, PsH):
            assert self.psgen[b.bank] == b.gen, "stale PSUM handle bank %d" % b.bank
            return b.buf
        return b

    def op(self, eng, fn, reads=(), writes=(), dma_sem=None):
        need = {}

        def req(dep, skip_own):
            if dep is None:
                return
            k, c = dep
            if skip_own and k == eng and dma_sem is None and eng == PE:
                return
            if c > need.get(k, 0):
                need[k] = c

        ps_reads = [self._buf(b) for b in reads if isinstance(b, PsH)]
        reads = [self._buf(b) for b in reads]
        writes = [self._buf(b) for b in writes]
        for b in reads:
            req(b.w, False)
        for b in ps_reads:
            for k, c in b.r.items():
                if k != eng:
                    req((k, c), False)
        for b in writes:
            req(b.w, True)
            for k, c in b.r.items():
                req((k, c), True)
        waits = []
        seen = self.seen[eng]
        for k, c in need.items():
            if seen.get(k, 0) < c:
                seen[k] = c
                waits.append((k, c))
        if dma_sem is None:
            key = eng
            self.cnt[eng] += 1
            c = self.cnt[eng]
        else:
            key = dma_sem
            self.cnt[dma_sem] += 16
            c = self.cnt[dma_sem]
        for b in writes:
            b.w = (key, c)
            b.r = {}
        for b in reads:
            if b.r.get(key, 0) < c:
                b.r[key] = c
        self.q[eng].append((waits, fn, key))
        if eng == PE:
            self.pe_phase.append(self.phase)
        self.nops += 1
        self.nwaits += len(waits)
        return (key, c)

    def final_wait(self, eng, deps):
        waits = []
        for k, c in deps:
            if self.seen[eng].get(k, 0) < c:
                self.seen[eng][k] = c
                waits.append((k, c))
        self.q[eng].append((waits, None, None))

    def emit(self):
        nc = self.nc
        with contextlib.ExitStack() as st:
            sems = {}
            for k in list(ENGS) + self.dma_sems:
                sems[k] = st.enter_context(nc.semaphore("s_" + k))
            block = st.enter_context(nc.Block())

            def run(engname):
                def body(e):
                    for waits, fn, key in self.q[engname]:
                        if fn is None:
                            for k, c in waits:
                                e.wait_ge(sems[k], c)
                            continue
                        for k, c in waits[1:]:
                            e.wait_ge(sems[k], c)
                        ins = fn(e)
                        if waits:
                            ins._wait_ge(sems[waits[0][0]], waits[0][1])
                        ins.then_inc(sems[key], 1 if key in ENGS else 16)
                return body

            block.tensor(run(PE))
            block.scalar(run(ACT))
            block.vector(run(DVE))
            block.gpsimd(run(POOL))
            block.sync(run(SP))


def make_consts():
    f = np.float32
    c = {}
    c["ident_f"] = np.eye(128, dtype=f)
    c["ident_b"] = np.eye(128, dtype=f).astype(ml_dtypes.bfloat16)
    c["ones_b"] = np.ones((128, 128), dtype=f).astype(ml_dtypes.bfloat16)
    H = 4
    logg = np.log1p(-np.exp2(-5.0 - np.arange(H, dtype=np.float64)))
    pos = np.arange(128, dtype=np.float64)
    scale = 128.0 ** -0.5
    rel = pos[None, :] - pos[:, None]
    DTm = np.zeros((128, H, 128), dtype=np.float64)
    for h in range(H):
        DTm[:, h, :] = np.where(rel >= 0, scale * np.exp(rel * logg[h]), 0.0)
    c["ret_dt"] = DTm.astype(f).reshape(128, H * 128)
    XI = np.zeros((128, H, 128))
    ZT = np.zeros((128, H, 128))
    for h in range(H):
        XI[:, h, :] = (scale * np.exp((pos + 1.0) * logg[h]))[None, :]
        ZT[:, h, :] = np.exp((127.0 - pos) * logg[h])[None, :]
    c["ret_xi"] = XI.astype(f).reshape(128, H * 128)
    c["ret_zt"] = ZT.astype(f).reshape(128, H * 128)
    blk = (np.arange(128) // 64)
    same = blk[:, None] == blk[None, :]
    jj = np.arange(128)[:, None]
    ii = np.arange(128)[None, :]
    c["gla_tri"] = (same & (jj <= ii)).astype(f)
    c["gla_sl"] = (same & (jj > ii)).astype(f)
    Hq = 16
    slopes = np.exp2(-8.0 * np.arange(1, Hq + 1, dtype=np.float64) / Hq)
    M = np.zeros((128, Hq, 2, 128))
    k = np.arange(128)[:, None].astype(np.float64)
    q = np.arange(128)[None, :].astype(np.float64)
    for h in range(Hq):
        d1 = q - k
        M[:, h, 1, :] = np.where(d1 >= 0, np.exp(-slopes[h] * d1), 0.0)
        d0 = q + 128.0 - k
        M[:, h, 0, :] = np.where(d0 < 128, np.exp(-slopes[h] * d0), 0.0)
    M2 = M.reshape(128, 8, 2, 2, 128).transpose(0, 2, 1, 3, 4)
    c["swa_mask"] = np.ascontiguousarray(M2).astype(f).astype(ml_dtypes.bfloat16).reshape(128, Hq * 2 * 128)
    return c


RET_G128 = [float(np.exp(128.0 * np.log1p(-np.exp2(-5.0 - h)))) for h in range(4)]


def col(v):
    return np.ascontiguousarray(np.asarray(v, dtype=np.float32).reshape(8, 128).T)


def build(n_seq=2, n_mt=4, n_layers=2, dbg=None):
    nc = bass.Bass("TRN2", target_bir_lowering=False)
    st = contextlib.ExitStack()

    def dram(name, shape, dt=F32, kind="ExternalInput"):
        return nc.dram_tensor(name, list(shape), dt, kind=kind).ap()

    x_d = dram("x", [n_seq, SEQ, D])
    out_d = dram("out", [n_seq, SEQ, D], kind="ExternalOutput")
    win_d = dram("w_in", [L, D, DIN])
    wbr_d = dram("w_branch", [L, 3, D, D])
    wout_d = dram("w_out", [L, D, D])
    wg_d = dram("ffn_w_gate", [L, D, DFF])
    wu_d = dram("ffn_w_up", [L, D, DFF])
    wd_d = dram("ffn_w_down", [L, DFF, D])
    par_d = dram("par", [L, 128, NPAR])
    w2_d = dram("w2aug", [L, 17, 512])
    wscr_d = dram("wscr", [128, L * WPL], BF16, kind="Internal")
    cd = {}
    for name, shp, dt in (("ident_f", [128, 128], F32), ("ident_b", [128, 128], BF16), ("ones_b", [128, 128], BF16),
                          ("ret_dt", [128, 512], F32), ("ret_xi", [128, 512], F32), ("ret_zt", [128, 512], F32),
                          ("gla_tri", [128, 128], F32), ("gla_sl", [128, 128], F32),
                          ("swa_mask", [128, 4096], BF16)):
        cd[name] = dram(name, shp, dt)
    if dbg:
        dbg_d = dram("dbg", [128, 8 * T], kind="ExternalOutput")

    def sb(name, shape, dt):
        return st.enter_context(nc.sbuf_tensor(name, list(shape), dt))

    S = Sched(nc)
    bufs = {}

    def B(*key):
        b = bufs.get(key)
        if b is None:
            b = bufs[key] = Buf(str(key))
        return b

    xT = sb("xT", [128, 8, T], F32)
    hT = sb("hT", [128, 8, T], BF16)
    sqr = sb("sqr", [128, 2, T], BF16)
    rstd2 = sb("rstd2", [128, 2, T], F32)
    tmp2 = sb("tmp2", [128, 2, T], F32)
    rstd = rstd2[:, 0, :]
    tmpf = tmp2[:, 0, :]
    NST, NBF = 2, 3
    wst = [sb("wst%d" % i, [128, 2048], F32) for i in range(NST)]
    wbf = [sb("wbf%d" % i, [128, 2048], BF16) for i in range(NBF)]
    merged = sb("merged", [128, 8, T], BF16)
    qT = sb("qT", [128, 4, T], BF16)
    kT = sb("kT", [128, 4, T], BF16)
    kzT = sb("kzT", [128, 4, T], BF16)
    vtok = sb("vtok", [128, NT, 1024], BF16)
    sgr = sb("sgr", [128, 2, T], BF16)
    sgall = sb("sgall", [128, 8, T], BF16)
    osb = sb("osb", [128, 8, T], F32)
    obf = sb("obf", [128, 8, T], BF16)
    AT = [sb("AT%d" % i, [128, 512], BF16) for i in range(2)]
    ktok = [sb("ktok%d" % i, [128, 512], BF16) for i in range(2)]
    retS = [sb("retS%d" % l, [128, 4, 256], F32) for l in range(L)]
    retSb = [sb("retSb%d" % l, [128, 4, 256], BF16) for l in range(L)]
    glaS = [sb("glaS%d" % l, [128, 4, 256], F32) for l in range(L)]
    glaSb = [sb("glaSb%d" % l, [128, 4, 256], BF16) for l in range(L)]
    gE = [sb("gE%d" % i, [128, T], F32) for i in range(2)]
    gEi = [sb("gEi%d" % i, [128, T], F32) for i in range(2)]
    gEs = [sb("gEs%d" % i, [128, T], F32) for i in range(2)]
    gaT = sb("gaT", [32, T], BF16)
    w2f = [sb("w2f%d" % l, [17, 512], F32) for l in range(L)]
    w2b = [sb("w2b%d" % l, [17, 512], BF16) for l in range(L)]
    KK = sb("KK", [128, 4, 128 + T], BF16)
    svt = sb("svt", [128, NT + 1, 256], BF16)
    ETb = [sb("ET%d" % i, [128, 512], BF16) for i in range(2)]
    PTs = sb("PTs", [128, 2, 4, 2, 128], BF16)
    den = sb("den", [128, 4, 128], F32)
    kkprev = [sb("kkprev%d" % l, [128, 4, 128], BF16) for l in range(L)]
    svprev = [sb("svprev%d" % l, [128, 256], BF16) for l in range(L)]
    par = [sb("par%d" % l, [128, NPAR], F32) for l in range(L)]
    es = [sb("es%d" % l, [128, 8], F32) for l in range(L)]
    ct = {}
    for name, ap in cd.items():
        ct[name] = sb("c_" + name, list(ap.shape), ap.dtype)
    onesb = sb("onesb32", [32, T], BF16)

    NPS = 7
    pst = [st.enter_context(nc.psum_tensor("ps%d" % i, [128, 512], F32)) for i in range(NPS)]
    ptr = st.enter_context(nc.psum_tensor("ptr", [128, 1024], BF16))
    psbuf = [Buf("ps%d" % i) for i in range(NPS)]
    for i in range(NPS):
        S.psgen[i] = 0
    psrr = [0]

    def ps_alloc():
        i = psrr[0] % NPS
        psrr[0] += 1
        S.psgen[i] += 1
        return PsH(i, S.psgen[i], pst[i], psbuf[i])

    def mm(out, lhsT, rhs, start, stop, reads, writes):
        S.op(PE, lambda e: e.matmul(out, lhsT=lhsT, rhs=rhs, start=start, stop=stop), reads=reads, writes=writes)

    def tr(out, in_, ident, reads, writes):
        S.op(PE, lambda e: e.transpose(out=out, in_=in_, identity=ident), reads=reads, writes=writes)

    def act(out, in_, func, reads, writes, **kw):
        S.op(ACT, lambda e: e.activation(out=out, in_=in_, func=func, **kw), reads=reads, writes=writes)

    def tt(out, in0, in1, op, reads, writes, eng=DVE):
        S.op(eng, lambda e: e.tensor_tensor(out=out, in0=in0, in1=in1, op=op), reads=reads, writes=writes)

    def ts(out, in0, s1, s2, op0, op1, reads, writes, eng=DVE):
        if s2 is None:
            S.op(eng, lambda e: e.tensor_scalar(out=out, in0=in0, scalar1=s1, scalar2=None, op0=op0),
                 reads=reads, writes=writes)
        else:
            S.op(eng, lambda e: e.tensor_scalar(out=out, in0=in0, scalar1=s1, scalar2=s2, op0=op0, op1=op1),
                 reads=reads, writes=writes)

    def stt(out, in0, scalar, in1, op0, op1, reads, writes):
        S.op(DVE, lambda e: e.scalar_tensor_tensor(out=out, in0=in0, scalar=scalar, in1=in1, op0=op0, op1=op1),
             reads=reads, writes=writes)

    def vcopy(out, in_, reads, writes):
        S.op(DVE, lambda e: e.tensor_copy(out=out, in_=in_), reads=reads, writes=writes)

    def recip(out, in_, reads, writes):
        S.op(DVE, lambda e: e.reciprocal(out=out, in_=in_), reads=reads, writes=writes)

    dsem_c = S.new_dma_sem("dc")
    dsem_x = S.new_dma_sem("dx")
    dsem_xs = [S.new_dma_sem("dx0"), S.new_dma_sem("dx1")]
    dsem_o = S.new_dma_sem("do")

    def dma(out, in_, reads, writes, sem, eng=SP):
        return S.op(eng, lambda e: e.dma_start(out=out, in_=in_), reads=reads, writes=writes, dma_sem=sem)

    for name in cd:
        dma(ct[name][:], cd[name], [], [B("c", name)], S.new_dma_sem("dc_" + name))
    for l in range(n_layers):
        dma(par[l][:], par_d[l], [], [B("par", l)], S.new_dma_sem("dc_par%d" % l))
        dma(w2f[l][:], w2_d[l], [], [B("w2f", l)], S.new_dma_sem("dc_w2%d" % l))
        vcopy(w2b[l][:], w2f[l][:], [B("w2f", l)], [B("w2b", l)])
        act(es[l][:], par[l][:, P_SINK:P_SINK + 8], AF.Exp, [B("par", l)], [B("es", l)])
    S.op(DVE, lambda e: e.memset(onesb[:], 1.0), writes=[B("onesb")])
    identf, identb, ones = ct["ident_f"], ct["ident_b"], ct["ones_b"]
    Bif, Bib, Bones = B("c", "ident_f"), B("c", "ident_b"), B("c", "ones_b")

    wsem = [S.new_dma_sem("dw%d" % i) for i in range(NST)]
    NRING = NBF + 2 * NST
    rsem = [S.new_dma_sem("dr%d" % i) for i in range(NRING)]
    dws = S.new_dma_sem("dws")
    wlist = []
    wstate = {"dma": 0, "cast": 0, "i0": None, "nconv": 0}
    wst_bf = [w_[:, :].bitcast(BF16) for w_ in wst]

    def ring_slot(r):
        if r < NBF:
            return wbf[r][:, :], [B("wbf", r)]
        s_, h_ = (r - NBF) // 2, (r - NBF) % 2
        return wst_bf[s_][:, h_ * 2048:(h_ + 1) * 2048], [B("wsth", s_, h_)]

    def wst_bufs(slot):
        return [B("wsth", slot, 0), B("wsth", slot, 1)]

    def w_issue_dma(i):
        view, KC, ncols, off, conv = wlist[i]
        n = KC * ncols
        if conv:
            slot = i % NST
            dst = wst[slot][:, 0:n].rearrange("p (k n) -> p k n", k=KC)
            dma(dst, view, [], wst_bufs(slot), wsem[slot])
        else:
            if wstate["i0"] is None:
                wstate["i0"] = i
            r = (i - wstate["i0"]) % NRING
            tl, bb = ring_slot(r)
            dma(tl[:, 0:n], wscr_d[:, off:off + n], [B("wscr_all")], bb, rsem[r])

    def w_issue_cast(i):
        view, KC, ncols, off, conv = wlist[i]
        if not conv:
            return
        slot, bslot = i % NST, i % NBF
        n = KC * ncols
        if i % 5 in (0, 2):
            vcopy(wbf[bslot][:, 0:n], wst[slot][:, 0:n], wst_bufs(slot), [B("wbf", bslot)])
        else:
            act(wbf[bslot][:, 0:n], wst[slot][:, 0:n], AF.Copy, wst_bufs(slot), [B("wbf", bslot)])
        dma(wscr_d[:, off:off + n], wbf[bslot][:, 0:n], [B("wbf", bslot)], [B("wscr_all")], dws, eng=POOL)

    def wget(i):
        assert i == wstate.get("last", -1) + 1, ("weight groups must be consumed in list order", i, wstate.get("last"))
        wstate["last"] = i
        nconv = wstate.get("n_conv")
        if nconv is None:
            nconv = wstate["n_conv"] = sum(1 for w_ in wlist if w_[4])

        def dma_ok(j):
            if wlist[j][4]:
                return j < i + NST
            return i >= nconv and j < i + NRING - 1
        while wstate["cast"] < min(len(wlist), i + 2):
            while wstate["dma"] <= wstate["cast"] and wlist[wstate["dma"]][4]:
                w_issue_dma(wstate["dma"])
                wstate["dma"] += 1
            w_issue_cast(wstate["cast"])
            wstate["cast"] += 1
        while wstate["dma"] < len(wlist) and dma_ok(wstate["dma"]):
            w_issue_dma(wstate["dma"])
            wstate["dma"] += 1
        assert wstate["dma"] > i
        view, KC, ncols, off, conv = wlist[i]
        n = KC * ncols
        if conv:
            return wbf[i % NBF][:, 0:n].rearrange("p (k n) -> p k n", k=KC), B("wbf", i % NBF)
        tl, bb = ring_slot((i - wstate["i0"]) % NRING)
        return tl[:, 0:n].rearrange("p (k n) -> p k n", k=KC), bb[0]

    scr_off = {}

    def layer_groups(l, conv):
        g = {}
        win = win_d[l].rearrange("(kc p) n -> p kc n", p=128)
        cursor = [l * WPL]

        def add(name, view, KC, ncols):
            g[name] = len(wlist)
            wlist.append((view, KC, ncols, cursor[0], conv))
            cursor[0] += KC * ncols
            assert cursor[0] <= (l + 1) * WPL

        def add_in(name, c0, n):
            for i in range(0, n, 256):
                w = min(256, n - i)
                add((name, i // 256), win[:, :, c0 + i:c0 + i + w], 8, w)
        add_in("rq", C_RQ, 512); add_in("rk", C_RK, 512); add_in("rv", C_RV, 1024); add_in("rg", C_RG, 1024)
        for n in range(3):
            if n == 1:
                add_in("ga", C_GA, 16); add_in("gv", C_GV, 1024)
                for i_ in range(2):
                    add(("gq", i_), win[:, :, C_GQ + i_ * 256:C_GQ + (i_ + 1) * 256], 8, 256)
                    add(("gk", i_), win[:, :, C_GK + i_ * 256:C_GK + (i_ + 1) * 256], 8, 256)
                add_in("gg", C_GG, 1024)
            if n == 2:
                add_in("sk", C_SK, 256); add_in("sv", C_SV, 256); add_in("sq", C_SQ, 1024)
            wb = wbr_d[l, n].rearrange("(kc p) n -> p kc n", p=128)
            for i in range(4):
                add(("gate", n, i), win[:, :, C_GATE + n * 1024 + i * 256:C_GATE + n * 1024 + (i + 1) * 256], 8, 256)
                add(("br", n, i), wb[:, :, i * 256:(i + 1) * 256], 8, 256)
        wo = wout_d[l].rearrange("(kc p) n -> p kc n", p=128)
        for i in range(4):
            add(("wo", i), wo[:, :, i * 256:(i + 1) * 256], 8, 256)
        wgv = wg_d[l].rearrange("(kc p) n -> p kc n", p=128)
        wuv = wu_d[l].rearrange("(kc p) n -> p kc n", p=128)
        wdv = wd_d[l].rearrange("(kc p) n -> p kc n", p=128)
        for half in range(2):
            for i in range(6):
                w = 256 if i < 5 else 128
                c0 = half * 1408 + i * 256
                add(("fg", half, i), wgv[:, :, c0:c0 + w], 8, w)
                add(("fu", half, i), wuv[:, :, c0:c0 + w], 8, w)
            for c in range(8):
                add(("fd", half, c), wdv[:, half * 11:(half + 1) * 11, c * 128:(c + 1) * 128], 11, 128)
        return g

    def proj_fm(gidx, ncols_chunks, evac, src=None, srcB=None):
        if src is None:
            src, srcB = hT, [B("hT", c) for c in range(8)]
        wv, wb = wget(gidx)
        KC = wv.shape[1]
        ncols = wv.shape[2]
        j = 0
        c0 = 0
        while c0 < ncols:
            w = min(128, ncols - c0)
            p = ps_alloc()
            for kc in range(KC):
                if os.environ.get("KDBG_NOMM"):
                    continue
                mm(p.t[0:w, :], wv[:, kc, c0:c0 + w], src[:, kc, :], kc == 0, kc == KC - 1,
                   [wb, srcB[kc]], [p])
            if not os.environ.get("KDBG_NOEVAC"):
                evac(j, p, w)
            j += 1
            c0 += w

    def proj_tm(gidx, evac):
        wv, wb = wget(gidx)
        ncols = wv.shape[2]
        for t in range(NT):
            p = ps_alloc()
            for kc in range(8):
                mm(p.t[:, 0:ncols], hT[:, kc, t * 128:(t + 1) * 128], wv[:, kc, :], kc == 0, kc == 7,
                   [wb, B("hT", kc)], [p])
            evac(t, p, ncols)

    sq_rr = [0]

    def rmsnorm_stats(srcT, srcB, width_scale, eps):
        p = ps_alloc()
        for c in range(8):
            s = sq_rr[0] % 2
            sq_rr[0] += 1
            act(sqr[:, s, :], srcT[:, c, :], AF.Square, [srcB[c]], [B("sqr", s)])
            mm(p.t[:, :], ones[:, :], sqr[:, s, :], c == 0, c == 7, [Bones, B("sqr", s)], [p])
        act(tmpf[:], p.t[:, :], AF.Sqrt, [p, Beps(eps)], [B("tmpf")], scale=width_scale, bias=eps_ap(eps))
        recip(rstd[:], tmpf[:], [B("tmpf")], [B("rstd")])

    eps_tiles = {}

    def eps_ap(v):
        if v not in eps_tiles:
            t_ = sb("eps%d" % len(eps_tiles), [128, 1], F32)
            S.op(DVE, lambda e: e.memset(t_[:], float(v)), writes=[B("eps", v)])
            eps_tiles[v] = t_
        return eps_tiles[v][:, 0:1]

    for v_ in (1e-6, 1e-5, 1.0):
        eps_ap(v_)

    def Beps(v):
        return B("eps", v)

    def pre_norm(l, poff):
        rmsnorm_stats(xT, [B("xT", c) for c in range(8)], 1.0 / D, 1e-6)
        for c in range(8):
            stt(hT[:, c, :], xT[:, c, :], par[l][:, poff + c:poff + c + 1], rstd[:], ALU.mult, ALU.mult,
                [B("xT", c), B("par", l), B("rstd")], [B("hT", c)])

    def post_norm_residual(l, poff):
        rmsnorm_stats(osb, [B("osb", c) for c in range(8)], 1.0 / D, 1e-6)
        for c in range(8):
            stt(osb[:, c, :], osb[:, c, :], par[l][:, poff + c:poff + c + 1], rstd[:], ALU.mult, ALU.mult,
                [B("osb", c), B("par", l), B("rstd")], [B("osb", c)])
            tt(xT[:, c, :], xT[:, c, :], osb[:, c, :], ALU.add, [B("xT", c), B("osb", c)], [B("xT", c)])

    _act = act

    def ret_branch(l, g, first_mt):
        DTt, XIt, ZTt = ct["ret_dt"], ct["ret_xi"], ct["ret_zt"]
        XIb = XIt[:, :].rearrange("p (h i) -> p h i", h=4)
        ZTb = ZTt[:, :].rearrange("p (h i) -> p h i", h=4)

        def ev_q(gi):
            def f(j, p, w):
                h = gi * 2 + j
                for t_ in range(NT):
                    tt(qT[:, h, t_ * 128:(t_ + 1) * 128], p.t[:, t_ * 128:(t_ + 1) * 128], XIt[:, h * 128:(h + 1) * 128], ALU.mult,
                       [p, B("c", "ret_xi")], [B("qT", h)])
            return f

        def ev_k(gi):
            def f(j, p, w):
                h = gi * 2 + j
                act(kT[:, h, :], p.t[:, :], AF.Copy, [p], [B("kT", h)])
                for t_ in range(NT):
                    tt(kzT[:, h, t_ * 128:(t_ + 1) * 128], p.t[:, t_ * 128:(t_ + 1) * 128], ZTt[:, h * 128:(h + 1) * 128], ALU.mult,
                       [p, B("c", "ret_zt")], [B("kzT", h)])
            return f

        def ev_q2(gi):
            def f(j, p, w):
                h = gi * 2 + j
                act(obf[:, h, :], p.t[:, :], AF.Copy, [p], [B("obf", h)])
                ev_q(gi)(j, p, w)
            return f
        for gi in range(2):
            proj_fm(g[("rq", gi)], 2, ev_q2(gi))
        for gi in range(2):
            proj_fm(g[("rk", gi)], 2, ev_k(gi))

        dbg_dump("r1", xT, [B("xT", c_) for c_ in range(8)])

        def ev_v(gi):
            def f(t, p, n):
                act(vtok[:, t, gi * 256:(gi + 1) * 256], p.t[:, 0:256], AF.Copy, [p], [B("vtok", t)])
            return f
        for gi in range(4):
            proj_tm(g[("rv", gi)], ev_v(gi))
        dbg_dump("r2", xT, [B("xT", c_) for c_ in range(8)])
        if first_mt:
            S.op(DVE, lambda e: e.memset(retS[l][:], 0.0), writes=[B("retS", l)])
            S.op(DVE, lambda e: e.memset(retSb[l][:], 0.0), writes=[B("retSb", l)])
        def ret_stage_a(t):
            S.phase = 'ret_a'
            tok = slice(t * 128, (t + 1) * 128)
            a = t % 2
            p = ps_alloc()
            for h in range(4):
                mm(p.t[:, h * 128:(h + 1) * 128], kT[:, h, tok], obf[:, h, tok], True, True,
                   [B("kT", h), B("obf", h)], [p])
            tt(AT[a][:], p.t[:, :], DTt[:, :], ALU.mult, [p, B("c", "ret_dt")], [B("AT", a)])
            dbg_dump("r3", xT, [B("xT", c_) for c_ in range(8)])
            for h in range(4):
                tr(ptr[:, h * 128:(h + 1) * 128], kzT[:, h, tok], identb[:], [B("kzT", h), Bib], [B("ptr", 0)])
            act(ktok[a][:], ptr[:, 0:512], AF.Copy, [B("ptr", 0)], [B("ktok", a)])
            dbg_dump("r4", xT, [B("xT", c_) for c_ in range(8)])

        def ret_stage_b(t):
            S.phase = 'ret_b'
            tok = slice(t * 128, (t + 1) * 128)
            a = t % 2
            for hh in range(2):
                p = ps_alloc()
                for h2 in range(2):
                    h = hh * 2 + h2
                    for ec in range(2):
                        o_ = p.t[:, (h2 * 2 + ec) * 128:(h2 * 2 + ec + 1) * 128]
                        mm(o_, vtok[:, t, h * 256 + ec * 128:h * 256 + (ec + 1) * 128], AT[a][:, h * 128:(h + 1) * 128],
                           True, False, [B("vtok", t), B("AT", a)], [p])
                        mm(o_, retSb[l][:, h, ec * 128:(ec + 1) * 128], qT[:, h, tok], False, True,
                           [B("retSb", l), B("qT", h)], [p])
                act(osb[:, hh * 4:(hh + 1) * 4, tok], p.t[:, :].rearrange("p (c i) -> p c i", c=4), AF.Copy,
                    [p], [B("osb", c) for c in range(hh * 4, hh * 4 + 4)])
            dbg_dump("r5", xT, [B("xT", c_) for c_ in range(8)])
            for hh in range(2):
                p = ps_alloc()
                for h2 in range(2):
                    h = hh * 2 + h2
                    mm(p.t[:, h2 * 256:(h2 + 1) * 256], ktok[a][:, h * 128:(h + 1) * 128], vtok[:, t, h * 256:(h + 1) * 256],
                       True, True, [B("ktok", a), B("vtok", t)], [p])
                for h2 in range(2):
                    h = hh * 2 + h2
                    stt(retS[l][:, h, :], retS[l][:, h, :], RET_G128[h], p.t[:, h2 * 256:(h2 + 1) * 256], ALU.mult, ALU.add,
                        [B("retS", l), p], [B("retS", l)])
                act(retSb[l][:, hh * 2:hh * 2 + 2, :], retS[l][:, hh * 2:hh * 2 + 2, :], AF.Copy,
                    [B("retS", l)], [B("retSb", l)])

        ret_stage_a(0)
        for t in range(NT):
            if t + 1 < NT:
                ret_stage_a(t + 1)
            ret_stage_b(t)
        dbg_dump("r6", xT, [B("xT", c_) for c_ in range(8)])
        def ev_gs(gi):
            def f(j, p, w):
                c = gi * 2 + j
                act(sgall[:, c, :], p.t[:, :], AF.Silu, [p], [B("sgall", c)])
            return f

        def gn_pair(hp):
            S.phase = 'ret_gn'
            pms, pqs = [], []
            for h2 in range(2):
                h = hp * 2 + h2
                pm = ps_alloc()
                pq = ps_alloc()
                for ec in range(2):
                    c = h * 2 + ec
                    s = sq_rr[0] % 2
                    sq_rr[0] += 1
                    act(sqr[:, s, :], osb[:, c, :], AF.Copy, [B("osb", c)], [B("sqr", s)])
                    mm(pm.t[:, :], ones[:, :], sqr[:, s, :], ec == 0, ec == 1, [Bones, B("sqr", s)], [pm])
                    s = sq_rr[0] % 2
                    sq_rr[0] += 1
                    act(sqr[:, s, :], osb[:, c, :], AF.Square, [B("osb", c)], [B("sqr", s)])
                    mm(pq.t[:, :], ones[:, :], sqr[:, s, :], ec == 0, ec == 1, [Bones, B("sqr", s)], [pq])
                pms.append(pm)
                pqs.append(pq)
            TB = [B("tmpf"), B("tmpfB")]
            RB = [B("rstd"), B("rstdB")]
            for h2 in range(2):
                act(tmp2[:, h2, :], pms[h2].t[:, :], AF.Square, [pms[h2]], [TB[h2]], scale=1.0 / 256)
                stt(tmp2[:, h2, :], pqs[h2].t[:, :], 1.0 / 256, tmp2[:, h2, :], ALU.mult, ALU.subtract,
                    [pqs[h2], TB[h2]], [TB[h2]])
            act(tmp2[:, :, :], tmp2[:, :, :], AF.Sqrt, TB + [Beps(1e-5)], TB, bias=eps_ap(1e-5))
            recip(rstd2[:, :, :], tmp2[:, :, :], TB, RB)
            for h2 in range(2):
                stt(tmp2[:, h2, :], pms[h2].t[:, :], -1.0 / 256, rstd2[:, h2, :], ALU.mult, ALU.mult,
                    [pms[h2], RB[h2]], [TB[h2]])
            cs = list(range(hp * 4, hp * 4 + 4))
            OB = [B("osb", c) for c in cs]
            osb4 = osb[:, hp * 4:hp * 4 + 4, :].rearrange("p (h e) t -> p h e t", h=2)
            tt(osb4, osb4, rstd2[:, :, :].unsqueeze(2).to_broadcast([128, 2, 2, T]), ALU.mult, OB + RB, OB)
            tt(osb4, osb4, tmp2[:, :, :].unsqueeze(2).to_broadcast([128, 2, 2, T]), ALU.add, OB + TB, OB)
            for c in cs:
                act(osb[:, c, :], osb[:, c, :], AF.Identity, [B("osb", c), B("par", l)], [B("osb", c)],
                    scale=par[l][:, P_RETW + c:P_RETW + c + 1], bias=par[l][:, P_RETB + c:P_RETB + c + 1])
            tt(obf[:, hp * 4:hp * 4 + 4, :], osb[:, hp * 4:hp * 4 + 4, :], sgall[:, hp * 4:hp * 4 + 4, :], ALU.mult,
               OB + [B("sgall", c) for c in cs], [B("obf", c) for c in cs])

        S.phase = 'ret_rg'
        for gi in range(4):
            proj_fm(g[("rg", gi)], 2, ev_gs(gi))
        gn_pair(0)
        gn_pair(1)

    def gla_branch2(l, g, first_mt):
        TRI, SLm = ct["gla_tri"], ct["gla_sl"]
        vcopy(gaT[:, :], onesb[:, :], [B("onesb")], [B("gaT")])

        def ev_ga(j, p, w):
            act(gaT[0:16, :], p.t[0:16, :], AF.Copy, [p], [B("gaT")])
        proj_fm(g[("ga", 0)], 1, ev_ga)

        def ev_v(gi):
            def f(t, p, n):
                act(vtok[:, t, gi * 256:(gi + 1) * 256], p.t[:, 0:256], AF.Copy, [p], [B("vtok", t)])
            return f
        for gi in range(4):
            proj_tm(g[("gv", gi)], ev_v(gi))
        if first_mt:
            S.op(DVE, lambda e: e.memset(glaS[l][:], 0.0), writes=[B("glaS", l, h_) for h_ in range(4)])
            S.op(DVE, lambda e: e.memset(glaSb[l][:], 0.0), writes=[B("glaSb", l, h_) for h_ in range(4)])
        for t in range(NT):
            pz = ps_alloc()
            mm(pz.t[:, :], gaT[0:17, t * 128:(t + 1) * 128], w2b[l][0:17, :], True, True,
               [B("gaT"), B("w2b", l)], [pz])
            act(osb[:, t, :], pz.t[:, :], AF.Exp, [pz], [B("osb", t)], scale=-1.0)
            act(osb[:, t, :], osb[:, t, :], AF.Ln, [B("osb", t), Beps(1.0)], [B("osb", t)], bias=eps_ap(1.0))
            ts(osb[:, t, :], osb[:, t, :], -1.0 / 16, -1.0, ALU.mult, ALU.max, [B("osb", t)], [B("osb", t)])
        for gi in range(2):
            for j in range(2):
                h = gi * 2 + j
                pb_ = ps_alloc()
                pl_ = ps_alloc()
                for t in range(NT):
                    mm(pb_.t[:, t * 128:(t + 1) * 128], osb[:, t, h * 128:(h + 1) * 128], TRI[:, :], True, True,
                       [B("osb", t), B("c", "gla_tri")], [pb_])
                    mm(pl_.t[:, t * 128:(t + 1) * 128], osb[:, t, h * 128:(h + 1) * 128], SLm[:, :], True, True,
                       [B("osb", t), B("c", "gla_sl")], [pl_])
                act(gE[j][:], pb_.t[:, :], AF.Exp, [pb_], [B("gE", j)])
                act(gEi[j][:], pb_.t[:, :], AF.Exp, [pb_], [B("gEi", j)], scale=-1.0)
                act(gEs[j][:], pl_.t[:, :], AF.Exp, [pl_], [B("gEs", j)])
                vcopy(gdec[:, h, :], gE[j][:, :].rearrange("p (b i) -> p b i", i=64)[:, :, 63], [B("gE", j)], [B("gdec")])
            wv, wb = wget(g[("gq", gi)])
            for j in range(2):
                h = gi * 2 + j
                p = ps_alloc()
                for kc in range(8):
                    mm(p.t[:, :], wv[:, kc, j * 128:(j + 1) * 128], hT[:, kc, :], kc == 0, kc == 7, [wb, B("hT", kc)], [p])
                stt(qT[:, h, :], p.t[:, :], 128.0 ** -0.5, gE[j][:], ALU.mult, ALU.mult, [p, B("gE", j)], [B("qT", h)])
            wv, wb = wget(g[("gk", gi)])
            for j in range(2):
                h = gi * 2 + j
                p = ps_alloc()
                for kc in range(8):
                    mm(p.t[:, :], wv[:, kc, j * 128:(j + 1) * 128], hT[:, kc, :], kc == 0, kc == 7, [wb, B("hT", kc)], [p])
                tt(kT[:, h, :], p.t[:, :], gEi[j][:], ALU.mult, [p, B("gEi", j)], [B("kT", h)])
                tt(kzT[:, h, :], p.t[:, :], gEs[j][:], ALU.mult, [p, B("gEs", j)], [B("kzT", h)])
        TRIb = TRI[:, :].unsqueeze(1).to_broadcast([128, 4, 128])

        def gla_stage_a(t):
            S.phase = 'gla_a'
            tok = slice(t * 128, (t + 1) * 128)
            a = t % 2
            p = ps_alloc()
            for h in range(4):
                mm(p.t[:, h * 128:(h + 1) * 128], kT[:, h, tok], qT[:, h, tok], True, True,
                   [B("kT", h), B("qT", h)], [p])
            tt(AT[a][:].rearrange("p (h i) -> p h i", h=4), p.t[:, :].rearrange("p (h i) -> p h i", h=4), TRIb, ALU.mult,
               [p, B("c", "gla_tri")], [B("AT", a)])
            for h in range(4):
                tr(ptr[:, h * 128:(h + 1) * 128], kzT[:, h, tok], identb[:], [B("kzT", h), Bib], [B("ptr", 0)])
            act(ktok[a][:], ptr[:, 0:512], AF.Copy, [B("ptr", 0)], [B("ktok", a)])

        def gla_stage_b(t):
            S.phase = 'gla_b'
            tok = slice(t * 128, (t + 1) * 128)
            a = t % 2
            po = [ps_alloc(), ps_alloc()]
            for blk in range(2):
                bt = slice(t * 128 + blk * 64, t * 128 + blk * 64 + 64)
                for h in range(4):
                    for ec in range(2):
                        base = ((h % 2) * 2 + ec) * 128 + blk * 64
                        o_ = po[h // 2].t[:, base:base + 64]
                        mm(o_, vtok[:, t, h * 256 + ec * 128:h * 256 + (ec + 1) * 128],
                           AT[a][:, h * 128 + blk * 64:h * 128 + blk * 64 + 64],
                           True, False, [B("vtok", t), B("AT", a)], [po[h // 2]])
                        mm(o_, glaSb[l][:, h, ec * 128:(ec + 1) * 128], qT[:, h, bt],
                           False, True, [B("glaSb", l, h), B("qT", h)], [po[h // 2]])
                for hh in range(2):
                    pk = ps_alloc()
                    for h2 in range(2):
                        h = hh * 2 + h2
                        mm(pk.t[:, h2 * 256:(h2 + 1) * 256], ktok[a][blk * 64:(blk + 1) * 64, h * 128:(h + 1) * 128],
                           vtok[blk * 64:(blk + 1) * 64, t, h * 256:(h + 1) * 256], True, True,
                           [B("ktok", a), B("vtok", t)], [pk])
                    for h2 in range(2):
                        h = hh * 2 + h2
                        stt(glaS[l][:, h, :], glaS[l][:, h, :], gdec[:, h, t * 2 + blk:t * 2 + blk + 1],
                            pk.t[:, h2 * 256:(h2 + 1) * 256], ALU.mult, ALU.add,
                            [B("glaS", l, h), B("gdec"), pk], [B("glaS", l, h)])
                        act(glaSb[l][:, h, :], glaS[l][:, h, :], AF.Copy, [B("glaS", l, h)], [B("glaSb", l, h)])
            for hh in range(2):
                act(obf_raw(hh, tok), po[hh].t[:, :].rearrange("p (c i) -> p c i", c=4), AF.Copy,
                    [po[hh]], [B("osb", c) for c in range(hh * 4, hh * 4 + 4)])

        gla_stage_a(0)
        for t in range(NT):
            if t + 1 < NT:
                gla_stage_a(t + 1)
            gla_stage_b(t)
        def ev_gs(gi):
            def f(j, p, w):
                c = gi * 2 + j
                act(sgall[:, c, :], p.t[:, :], AF.Silu, [p], [B("sgall", c)])
            return f

        def rms_head(h):
            S.phase = 'gla_rms'
            pq = ps_alloc()
            for ec in range(2):
                c = h * 2 + ec
                s = sq_rr[0] % 2
                sq_rr[0] += 1
                act(sqr[:, s, :], osb[:, c, :], AF.Square, [B("osb", c)], [B("sqr", s)])
                mm(pq.t[:, :], ones[:, :], sqr[:, s, :], ec == 0, ec == 1, [Bones, B("sqr", s)], [pq])
            act(tmpf[:], pq.t[:, :], AF.Sqrt, [pq, Beps(1e-6)], [B("tmpf")], scale=1.0 / 256, bias=eps_ap(1e-6))
            recip(rstd[:], tmpf[:], [B("tmpf")], [B("rstd")])
            for ec in range(2):
                c = h * 2 + ec
                stt(osb[:, c, :], osb[:, c, :], par[l][:, P_GLAW + c:P_GLAW + c + 1], rstd[:], ALU.mult, ALU.mult,
                    [B("osb", c), B("par", l), B("rstd")], [B("osb", c)])
                tt(obf[:, c, :], osb[:, c, :], sgall[:, c, :], ALU.mult, [B("osb", c), B("sgall", c)], [B("obf", c)])

        proj_fm(g[("gg", 0)], 2, ev_gs(0))
        for h in range(4):
            if h + 1 < 4:
                proj_fm(g[("gg", h + 1)], 2, ev_gs(h + 1))
            rms_head(h)

    gdec = sb("gdec", [128, 4, 2 * NT], F32)

    def obf_raw(hh, tok):
        return osb[:, hh * 4:(hh + 1) * 4, tok]

    def swa_branch(l, g, first_mt):
        MASK = ct["swa_mask"][:, :].rearrange("p (s c k q) -> p s c k q", s=2, c=8, k=2)
        wv, wb = wget(g[("sk", 0)])
        if first_mt:
            S.op(DVE, lambda e: e.memset(KK[:, :, 0:128], 0.0), writes=[B("KK", gg) for gg in range(4)])
            S.op(DVE, lambda e: e.memset(svt[:, 0, :], 0.0), writes=[B("svt", 0)])
        else:
            vcopy(KK[:, :, 0:128], kkprev[l][:, :, :], [B("kkprev", l)], [B("KK", gg) for gg in range(4)])
            vcopy(svt[:, 0, :], svprev[l][:, :], [B("svprev", l)], [B("svt", 0)])
        for gg in range(4):
            p = ps_alloc()
            for half in range(2):
                for kc in range(8):
                    mm(p.t[half * 64:(half + 1) * 64, :], wv[:, kc, gg * 64:(gg + 1) * 64], hT[:, kc, :], kc == 0, kc == 7,
                       [wb, B("hT", kc)], [p])
            act(KK[:, gg, 128:128 + T], p.t[:, :], AF.Copy, [p], [B("KK", gg)])
        wv, wb = wget(g[("sv", 0)])
        for t in range(NT):
            p = ps_alloc()
            for kc in range(8):
                mm(p.t[:, 0:256], hT[:, kc, t * 128:(t + 1) * 128], wv[:, kc, :], kc == 0, kc == 7, [wb, B("hT", kc)], [p])
            act(svt[:, t + 1, :], p.t[:, 0:256], AF.Copy, [p], [B("svt", t + 1)])
        def qdst(c):
            return (qT, "qT", c) if c < 4 else (kT, "kT", c - 4)

        def ev_q(gi):
            def f(j, p, w):
                c = gi * 2 + j
                tl, nm, ci = qdst(c)
                act(tl[:, ci, :], p.t[:, :], AF.Copy, [p], [B(nm, ci)])
            return f
        for gi in range(4):
            proj_fm(g[("sq", gi)], 2, ev_q(gi))
        vcopy(kkprev[l][:, :, :], KK[:, :, T:T + 128], [B("KK", gg) for gg in range(4)], [B("kkprev", l)])
        vcopy(svprev[l][:, :], svt[:, NT, :], [B("svt", NT)], [B("svprev", l)])
        esb = es[l]
        for t in range(NT):
            tok = slice(t * 128, (t + 1) * 128)
            kts = [1] if (first_mt and t == 0) else [0, 1]
            for half in range(2):
                for s in range(2):
                    for pair in range(2):
                        p = ps_alloc()
                        for ci2 in range(2):
                            cc = pair * 2 + ci2
                            c = half * 4 + cc
                            tl, nm, ci = qdst(c)
                            gkv = c // 2
                            for kt in kts:
                                kcols = slice(t * 128 + kt * 128, t * 128 + kt * 128 + 128)
                                mm(p.t[:, (ci2 * 2 + kt) * 128:(ci2 * 2 + kt + 1) * 128], KK[s * 64:(s + 1) * 64, gkv, kcols],
                                   tl[s * 64:(s + 1) * 64, ci, tok], True, True, [B("KK", gkv), B(nm, ci)], [p])
                        e_ = pair
                        c0 = half * 4 + pair * 2
                        if len(kts) == 2:
                            act(ETb[e_][:], p.t[:, :], AF.Exp, [p], [B("ET", e_)], scale=0.125)
                            tt(PTs[:, s, pair * 2:pair * 2 + 2, :, :], ETb[e_][:].rearrange("p (h k q) -> p h k q", h=2, k=2),
                               MASK[:, s, c0:c0 + 2, :, :], ALU.mult, [B("ET", e_), B("c", "swa_mask")], [B("PTs", s, pair)])
                        else:
                            for ci2 in range(2):
                                act(ETb[e_][:, ci2 * 256 + 128:ci2 * 256 + 256], p.t[:, ci2 * 256 + 128:ci2 * 256 + 256], AF.Exp,
                                    [p], [B("ET", e_)], scale=0.125)
                            tt(PTs[:, s, pair * 2:pair * 2 + 2, 1, :],
                               ETb[e_][:].rearrange("p (h k q) -> p h k q", h=2, k=2)[:, :, 1, :],
                               MASK[:, s, c0:c0 + 2, 1, :], ALU.mult, [B("ET", e_), B("c", "swa_mask")], [B("PTs", s, pair)])
                po = ps_alloc()
                pd = ps_alloc()
                for cc in range(4):
                    c = half * 4 + cc
                    gkv = c // 2
                    for s in range(2):
                        for i, kt in enumerate(kts):
                            mm(po.t[s * 64:(s + 1) * 64, cc * 128:(cc + 1) * 128], svt[:, t + kt, gkv * 64:(gkv + 1) * 64],
                               PTs[:, s, cc, kt, :], i == 0, i == len(kts) - 1, [B("svt", t + kt), B("PTs", s, cc // 2)], [po])
                        for i, kt in enumerate(kts):
                            mm(pd.t[s * 64:(s + 1) * 64, cc * 128:(cc + 1) * 128], ones[:, 0:64],
                               PTs[:, s, cc, kt, :], i == 0, i == len(kts) - 1, [Bones, B("PTs", s, cc // 2)], [pd])
                tt(den[:, :, :], pd.t[:, :].rearrange("p (c q) -> p c q", c=4),
                   esb[:, half * 4:(half + 1) * 4].unsqueeze(2).to_broadcast([128, 4, 128]), ALU.add,
                   [pd, B("es", l)], [B("den")])
                recip(den[:, :, :], den[:, :, :], [B("den")], [B("den")])
                tt(obf[:, half * 4:(half + 1) * 4, tok], po.t[:, :].rearrange("p (c q) -> p c q", c=4), den[:, :, :], ALU.mult,
                   [po, B("den")], [B("obf", c) for c in range(half * 4, half * 4 + 4)])

    def branch_merge(l, g, n):
        for i in range(4):
            wvg, wbg = wget(g[("gate", n, i)])
            gates = []
            for j in range(2):
                c = i * 2 + j
                p = ps_alloc()
                for kc in range(8):
                    mm(p.t[:, :], wvg[:, kc, j * 128:(j + 1) * 128], hT[:, kc, :], kc == 0, kc == 7, [wbg, B("hT", kc)], [p])
                s = c % 2
                act(sgr[:, s, :], p.t[:, :], AF.Sigmoid, [p], [B("sgr", s)])
            wvb, wbb = wget(g[("br", n, i)])
            for j in range(2):
                c = i * 2 + j
                s = c % 2
                p = ps_alloc()
                for kc in range(8):
                    mm(p.t[:, :], wvb[:, kc, j * 128:(j + 1) * 128], obf[:, kc, :], kc == 0, kc == 7, [wbb, B("obf", kc)], [p])
                if n == 0:
                    tt(merged[:, c, :], p.t[:, :], sgr[:, s, :], ALU.mult, [p, B("sgr", s)], [B("merged", c)])
                else:
                    tt(tmpf[:], p.t[:, :], sgr[:, s, :], ALU.mult, [p, B("sgr", s)], [B("tmpf")])
                    tt(merged[:, c, :], merged[:, c, :], tmpf[:], ALU.add, [B("merged", c), B("tmpf")], [B("merged", c)])

    def out_proj(l, g):
        for i in range(4):
            wv, wb = wget(g[("wo", i)])
            for j in range(2):
                c = i * 2 + j
                p = ps_alloc()
                for kc in range(8):
                    mm(p.t[:, :], wv[:, kc, j * 128:(j + 1) * 128], merged[:, kc, :], kc == 0, kc == 7,
                       [wb, B("merged", kc)], [p])
                act(osb[:, c, :], p.t[:, :], AF.Copy, [p], [B("osb", c)])

    def fdst(j):
        return (obf, "obf", j) if j < 8 else (kzT, "kzT", j - 8)

    def ffn(l, g):
        for half in range(2):
            for i in range(6):
                wvg, wbg = wget(g[("fg", half, i)])
                nj = wvg.shape[2] // 128
                for jj in range(nj):
                    j = i * 2 + jj
                    p = ps_alloc()
                    for kc in range(8):
                        mm(p.t[:, :], wvg[:, kc, jj * 128:(jj + 1) * 128], hT[:, kc, :], kc == 0, kc == 7, [wbg, B("hT", kc)], [p])
                    s = j % 2
                    act(sgr[:, s, :], p.t[:, :], AF.Silu, [p], [B("sgr", s)])
                wvu, wbu = wget(g[("fu", half, i)])
                for jj in range(nj):
                    j = i * 2 + jj
                    s = j % 2
                    tl, nm, ci = fdst(j)
                    p = ps_alloc()
                    for kc in range(8):
                        mm(p.t[:, :], wvu[:, kc, jj * 128:(jj + 1) * 128], hT[:, kc, :], kc == 0, kc == 7, [wbu, B("hT", kc)], [p])
                    tt(tl[:, ci, :], p.t[:, :], sgr[:, s, :], ALU.mult, [p, B("sgr", s)], [B(nm, ci)])
            for c in range(8):
                wv, wb = wget(g[("fd", half, c)])
                p = ps_alloc()
                for j in range(11):
                    tl, nm, ci = fdst(j)
                    mm(p.t[:, :], wv[:, j, :], tl[:, ci, :], j == 0, j == 10, [wb, B(nm, ci)], [p])
                if half == 0:
                    act(osb[:, c, :], p.t[:, :], AF.Copy, [p], [B("osb", c)])
                else:
                    tt(osb[:, c, :], osb[:, c, :], p.t[:, :], ALU.add, [B("osb", c), p], [B("osb", c)])

    def f32_stage(tile_bf16):
        return tile_bf16[:, :, :].rearrange("p c t -> p (c t)").bitcast(F32).rearrange("p (t d) -> p t d", t=2)

    ld_stage = [(f32_stage(sgall), "sgall"), (f32_stage(merged), "merged")]
    st_stage = osb[:, :, :].rearrange("p c t -> p (c t)").rearrange("p (t d) -> p t d", t=4)

    def issue_x_load(sq, mt):
        for half, (stg, nm) in enumerate(ld_stage):
            r0 = mt * T + half * 256
            dma(stg, x_d[sq, r0:r0 + 256, :].rearrange("(t p) d -> p t d", p=128), [], [B(nm, c) for c in range(8)],
                dsem_xs[half], eng=SP)

    def load_x(sq, mt):
        for t in range(NT):
            stg, nm = ld_stage[t // 2]
            for hh in range(2):
                p = ps_alloc()
                for c4 in range(4):
                    c = hh * 4 + c4
                    tr(p.t[:, c4 * 128:(c4 + 1) * 128], stg[:, t % 2, c * 128:(c + 1) * 128], identf[:],
                       [B(nm, c_) for c_ in range(8)] + [Bif], [p])
                act(xT[:, hh * 4:(hh + 1) * 4, t * 128:(t + 1) * 128], p.t[:, :].rearrange("p (c i) -> p c i", c=4), AF.Copy,
                    [p], [B("xT", c) for c in range(hh * 4, hh * 4 + 4)])

    out_deps = []

    def store_x(sq, mt):
        for t in range(NT):
            for hh in range(2):
                p = ps_alloc()
                for c4 in range(4):
                    c = hh * 4 + c4
                    tr(p.t[:, c4 * 128:(c4 + 1) * 128], xT[:, c, t * 128:(t + 1) * 128], identf[:], [B("xT", c), Bif], [p])
                act(st_stage[:, t, hh * 512:(hh + 1) * 512], p.t[:, :], AF.Copy, [p], [B("osb", c) for c in range(8)])
        r0 = mt * T
        out_deps.append(dma(out_d[sq, r0:r0 + T, :].rearrange("(t p) d -> p t d", p=128), st_stage,
                            [B("osb", c) for c in range(8)], [], dsem_o, eng=POOL))

    class StopBuild(Exception):
        pass

    def dbg_dump(stage, tile, names):
        if not dbg or dbg.get("stage") != stage:
            return
        dsem_d = S.new_dma_sem("ddbg")
        if tile.dtype == BF16:
            dbgt = sb("dbgt", [128, 4, T], F32)
            for hf in range(2):
                vcopy(dbgt[:], tile[:, hf * 4:(hf + 1) * 4, :], names, [B("dbgt")])
                dep = dma(dbg_d[:, hf * 4 * T:(hf + 1) * 4 * T], dbgt[:].rearrange("p c t -> p (c t)"), [B("dbgt")], [], dsem_d)
        else:
            dep = dma(dbg_d, tile[:].rearrange("p c t -> p (c t)"), names, [], dsem_d)
        out_deps.append(dep)
        raise StopBuild()

    groups = {}
    plan = []
    for sq in range(n_seq):
        for mt in range(n_mt):
            for l in range(n_layers):
                groups[(sq, mt, l)] = layer_groups(l, conv=(sq == 0 and mt == 0))
    only = (dbg or {}).get("only")
    c8 = list(range(8))
    try:
      order = [(sq_, mt_) for sq_ in range(n_seq) for mt_ in range(n_mt)]
      issue_x_load(*order[0])
      for oi, (sq, mt) in enumerate(order):
        if True:
            S.phase = 'load_x'
            load_x(sq, mt)
            dbg_dump("x", xT, [B("xT", c) for c in c8])
            for l in range(n_layers):
                g = groups[(sq, mt, l)]
                first = (mt == 0)
                S.phase = 'pre_norm_P_MIXPRE'
                pre_norm(l, P_MIXPRE)
                dbg_dump("h", hT, [B("hT", c) for c in c8])
                S.phase = 'ret_branch'
                ret_branch(l, g, first)
                dbg_dump("o_ret", obf, [B("obf", c) for c in c8])
                S.phase = 'branch_merge_0'
                branch_merge(l, g, 0)
                S.phase = 'gla_branch2'
                gla_branch2(l, g, first)
                dbg_dump("o_gla", obf, [B("obf", c) for c in c8])
                S.phase = 'branch_merge_1'
                branch_merge(l, g, 1)
                S.phase = 'swa_branch'
                swa_branch(l, g, first)
                dbg_dump("o_swa", obf, [B("obf", c) for c in c8])
                S.phase = 'branch_merge_2'
                branch_merge(l, g, 2)
                dbg_dump("merged", merged, [B("merged", c) for c in c8])
                S.phase = 'out_proj'
                out_proj(l, g)
                S.phase = 'post_norm_residual_P_MIXPOST'
                post_norm_residual(l, P_MIXPOST)
                dbg_dump("x1", xT, [B("xT", c) for c in c8])
                S.phase = 'pre_norm_P_FFNPRE'
                pre_norm(l, P_FFNPRE)
                if l == n_layers - 1 and oi + 1 < len(order):
                    issue_x_load(*order[oi + 1])
                S.phase = 'ffn'
                ffn(l, g)
                S.phase = 'post_norm_residual_P_FFNPOST'
                post_norm_residual(l, P_FFNPOST)
            S.phase = 'store_x'
            store_x(sq, mt)
    except StopBuild:
        pass
    S.final_wait(POOL, out_deps)
    S.emit()
    st.close()
    return nc, S


def host_inputs(inputs):
    f = np.float32
    shared = dict(make_consts())
    for k in ("w_in", "w_branch", "w_out", "ffn_w_gate", "ffn_w_up", "ffn_w_down"):
        shared[k] = np.ascontiguousarray(np.asarray(inputs[k], dtype=f))
    par = np.zeros((L, 128, NPAR), dtype=f)
    for l in range(L):
        par[l, :, P_MIXPRE:P_MIXPRE + 8] = col(inputs["norm_mix_pre"][l])
        par[l, :, P_MIXPOST:P_MIXPOST + 8] = col(inputs["norm_mix_post"][l])
        par[l, :, P_FFNPRE:P_FFNPRE + 8] = col(inputs["norm_ffn_pre"][l])
        par[l, :, P_FFNPOST:P_FFNPOST + 8] = col(inputs["norm_ffn_post"][l])
        par[l, :, P_RETW:P_RETW + 8] = col(inputs["ret_norm_w"][l])
        par[l, :, P_RETB:P_RETB + 8] = col(inputs["ret_norm_b"][l])
        par[l, :, P_GLAW:P_GLAW + 8] = col(inputs["gla_norm_w"][l])
        sk = np.asarray(inputs["attn_sinks"][l], dtype=f)
        par[l, :, P_SINK:P_SINK + 8] = sk.reshape(8, 2).T[np.arange(128) // 64, :]
    shared["par"] = par
    w2 = np.concatenate([np.asarray(inputs["gla_w_alpha2"], dtype=f),
                         np.asarray(inputs["gla_b_alpha"], dtype=f)[:, None, :]], axis=1)
    shared["w2aug"] = np.ascontiguousarray(w2)
    return shared


_CACHE = {}


def kernel(**inputs):
    x = np.asarray(inputs["x"], dtype=np.float32)
    n = 8
    shared = host_inputs(inputs)
    if "nc" not in _CACHE:
        _CACHE["nc"] = build()[0]
    nc = _CACHE["nc"]
    in_maps = []
    for i in range(n):
        m = dict(shared)
        m["x"] = np.ascontiguousarray(x[2 * i:2 * i + 2])
        in_maps.append(m)
    res = run_bass_kernel_spmd(nc, in_maps, core_ids=list(range(n)))
    out = np.concatenate([np.asarray(r["out"], dtype=np.float32) for r in res.results], axis=0)
    return out
```

```python
import contextlib
import os
import numpy as np
import ml_dtypes
import concourse.bass as bass
import concourse.mybir as mybir
from concourse.bass_utils import run_bass_kernel_spmd

F32 = mybir.dt.float32
BF16 = mybir.dt.bfloat16
AF = mybir.ActivationFunctionType
ALU = mybir.AluOpType

PE, ACT, DVE, POOL, SP = "pe", "act", "dve", "pool", "sp"
ENGS = (PE, ACT, DVE, POOL, SP)

D = 1024
SEQ = 2048
L = 2
T = 512
NT = T // 128
DIN = 10768
DFF = 2816
C_RQ, C_RK, C_RV, C_RG = 0, 512, 1024, 2048
C_GQ, C_GK, C_GV, C_GG, C_GA = 3072, 3584, 4096, 5120, 6144
C_SQ, C_SK, C_SV, C_GATE = 6160, 7184, 7440, 7696
NPAR = 64
WPL = (D * DIN + 3 * D * D + D * D + 3 * D * DFF) // 128
P_MIXPRE, P_MIXPOST, P_FFNPRE, P_FFNPOST, P_RETW, P_RETB, P_GLAW, P_SINK = 0, 8, 16, 24, 32, 40, 48, 56


class Buf:
    __slots__ = ("name", "w", "r")

    def __init__(self, name=""):
        self.name = name
        self.w = None
        self.r = {}


class PsH:
    __slots__ = ("bank", "gen", "t", "buf")

    def __init__(self, bank, gen, t, buf):
        self.bank, self.gen, self.t, self.buf = bank, gen, t, buf


class Sched:
    def __init__(self, nc):
        self.nc = nc
        self.q = {e: [] for e in ENGS}
        self.cnt = {e: 0 for e in ENGS}
        self.seen = {e: {} for e in ENGS}
        self.dma_sems = []
        self.nops = 0
        self.nwaits = 0
        self.psgen = {}
        self.phase = ''
        self.pe_phase = []

    def new_dma_sem(self, name):
        self.dma_sems.append(name)
        self.cnt[name] = 0
        return name

    def _buf(self, b):
        if isinstance(b, PsH):
            assert self.psgen[b.bank] == b.gen, "stale PSUM handle bank %d" % b.bank
            return b.buf
        return b

    def op(self, eng, fn, reads=(), writes=(), dma_sem=None):
        need = {}

        def req(dep, skip_own):
            if dep is None:
                return
            k, c = dep
            if skip_own and k == eng and dma_sem is None and eng == PE:
                return
            if c > need.get(k, 0):
                need[k] = c

        ps_reads = [self._buf(b) for b in reads if isinstance(b, PsH)]
        reads = [self._buf(b) for b in reads]
        writes = [self._buf(b) for b in writes]
        for b in reads:
            req(b.w, False)
        for b in ps_reads:
            for k, c in b.r.items():
                if k != eng:
                    req((k, c), False)
        for b in writes:
            req(b.w, True)
            for k, c in b.r.items():
                req((k, c), True)
        waits = []
        seen = self.seen[eng]
        for k, c in need.items():
            if seen.get(k, 0) < c:
                seen[k] = c
                waits.append((k, c))
        if dma_sem is None:
            key = eng
            self.cnt[eng] += 1
            c = self.cnt[eng]
        else:
            key = dma_sem
            self.cnt[dma_sem] += 16
            c = self.cnt[dma_sem]
        for b in writes:
            b.w = (key, c)
            b.r = {}
        for b in reads:
            if b.r.get(key, 0) < c:
                b.r[key] = c
        self.q[eng].append((waits, fn, key))
        if eng == PE:
            self.pe_phase.append(self.phase)
        self.nops += 1
        self.nwaits += len(waits)
        return (key, c)

    def final_wait(self, eng, deps):
        waits = []
        for k, c in deps:
            if self.seen[eng].get(k, 0) < c:
                self.seen[eng][k] = c
                waits.append((k, c))
        self.q[eng].append((waits, None, None))

    def emit(self):
        nc = self.nc
        with contextlib.ExitStack() as st:
            sems = {}
            for k in list(ENGS) + self.dma_sems:
                sems[k] = st.enter_context(nc.semaphore("s_" + k))
            block = st.enter_context(nc.Block())

            def run(engname):
                def body(e):
                    for waits, fn, key in self.q[engname]:
                        if fn is None:
                            for k, c in waits:
                                e.wait_ge(sems[k], c)
                            continue
                        for k, c in waits[1:]:
                            e.wait_ge(sems[k], c)
                        ins = fn(e)
                        if waits:
                            ins._wait_ge(sems[waits[0][0]], waits[0][1])
                        ins.then_inc(sems[key], 1 if key in ENGS else 16)
                return body

            block.tensor(run(PE))
            block.scalar(run(ACT))
            block.vector(run(DVE))
            block.gpsimd(run(POOL))
            block.sync(run(SP))


def make_consts():
    f = np.float32
    c = {}
    c["ident_f"] = np.eye(128, dtype=f)
    c["ident_b"] = np.eye(128, dtype=f).astype(ml_dtypes.bfloat16)
    c["ones_b"] = np.ones((128, 128), dtype=f).astype(ml_dtypes.bfloat16)
    H = 4
    logg = np.log1p(-np.exp2(-5.0 - np.arange(H, dtype=np.float64)))
    pos = np.arange(128, dtype=np.float64)
    scale = 128.0 ** -0.5
    rel = pos[None, :] - pos[:, None]
    DTm = np.zeros((128, H, 128), dtype=np.float64)
    for h in range(H):
        DTm[:, h, :] = np.where(rel >= 0, scale * np.exp(rel * logg[h]), 0.0)
    c["ret_dt"] = DTm.astype(f).reshape(128, H * 128)
    XI = np.zeros((128, H, 128))
    ZT = np.zeros((128, H, 128))
    for h in range(H):
        XI[:, h, :] = (scale * np.exp((pos + 1.0) * logg[h]))[None, :]
        ZT[:, h, :] = np.exp((127.0 - pos) * logg[h])[None, :]
    c["ret_xi"] = XI.astype(f).reshape(128, H * 128)
    c["ret_zt"] = ZT.astype(f).reshape(128, H * 128)
    blk = (np.arange(128) // 64)
    same = blk[:, None] == blk[None, :]
    jj = np.arange(128)[:, None]
    ii = np.arange(128)[None, :]
    c["gla_tri"] = (same & (jj <= ii)).astype(f)
    c["gla_sl"] = (same & (jj > ii)).astype(f)
    Hq = 16
    slopes = np.exp2(-8.0 * np.arange(1, Hq + 1, dtype=np.float64) / Hq)
    M = np.zeros((128, Hq, 2, 128))
    k = np.arange(128)[:, None].astype(np.float64)
    q = np.arange(128)[None, :].astype(np.float64)
    for h in range(Hq):
        d1 = q - k
        M[:, h, 1, :] = np.where(d1 >= 0, np.exp(-slopes[h] * d1), 0.0)
        d0 = q + 128.0 - k
        M[:, h, 0, :] = np.where(d0 < 128, np.exp(-slopes[h] * d0), 0.0)
    M2 = M.reshape(128, 8, 2, 2, 128).transpose(0, 2, 1, 3, 4)
    c["swa_mask"] = np.ascontiguousarray(M2).astype(f).astype(ml_dtypes.bfloat16).reshape(128, Hq * 2 * 128)
    return c


RET_G128 = [float(np.exp(128.0 * np.log1p(-np.exp2(-5.0 - h)))) for h in range(4)]


def col(v):
    return np.ascontiguousarray(np.asarray(v, dtype=np.float32).reshape(8, 128).T)


def build(n_seq=2, n_mt=4, n_layers=2, dbg=None):
    nc = bass.Bass("TRN2", target_bir_lowering=False)
    st = contextlib.ExitStack()

    def dram(name, shape, dt=F32, kind="ExternalInput"):
        return nc.dram_tensor(name, list(shape), dt, kind=kind).ap()

    x_d = dram("x", [n_seq, SEQ, D])
    out_d = dram("out", [n_seq, SEQ, D], kind="ExternalOutput")
    win_d = dram("w_in", [L, D, DIN])
    wbr_d = dram("w_branch", [L, 3, D, D])
    wout_d = dram("w_out", [L, D, D])
    wg_d = dram("ffn_w_gate", [L, D, DFF])
    wu_d = dram("ffn_w_up", [L, D, DFF])
    wd_d = dram("ffn_w_down", [L, DFF, D])
    par_d = dram("par", [L, 128, NPAR])
    w2_d = dram("w2aug", [L, 17, 512])
    wscr_d = dram("wscr", [128, L * WPL], BF16, kind="Internal")
    cd = {}
    for name, shp, dt in (("ident_f", [128, 128], F32), ("ident_b", [128, 128], BF16), ("ones_b", [128, 128], BF16),
                          ("ret_dt", [128, 512], F32), ("ret_xi", [128, 512], F32), ("ret_zt", [128, 512], F32),
                          ("gla_tri", [128, 128], F32), ("gla_sl", [128, 128], F32),
                          ("swa_mask", [128, 4096], BF16)):
        cd[name] = dram(name, shp, dt)
    if dbg:
        dbg_d = dram("dbg", [128, 8 * T], kind="ExternalOutput")

    def sb(name, shape, dt):
        return st.enter_context(nc.sbuf_tensor(name, list(shape), dt))

    S = Sched(nc)
    bufs = {}

    def B(*key):
        b = bufs.get(key)
        if b is None:
            b = bufs[key] = Buf(str(key))
        return b

    xT = sb("xT", [128, 8, T], F32)
    hT = sb("hT", [128, 8, T], BF16)
    sqr = sb("sqr", [128, 2, T], BF16)
    rstd2 = sb("rstd2", [128, 2, T], F32)
    tmp2 = sb("tmp2", [128, 2, T], F32)
    rstd = rstd2[:, 0, :]
    tmpf = tmp2[:, 0, :]
    NST, NBF = 2, 3
    wst = [sb("wst%d" % i, [128, 2048], F32) for i in range(NST)]
    wbf = [sb("wbf%d" % i, [128, 2048], BF16) for i in range(NBF)]
    merged = sb("merged", [128, 8, T], BF16)
    qT = sb("qT", [128, 4, T], BF16)
    kT = sb("kT", [128, 4, T], BF16)
    kzT = sb("kzT", [128, 4, T], BF16)
    vtok = sb("vtok", [128, NT, 1024], BF16)
    sgr = sb("sgr", [128, 2, T], BF16)
    sgall = sb("sgall", [128, 8, T], BF16)
    osb = sb("osb", [128, 8, T], F32)
    obf = sb("obf", [128, 8, T], BF16)
    AT = [sb("AT%d" % i, [128, 512], BF16) for i in range(2)]
    ktok = [sb("ktok%d" % i, [128, 512], BF16) for i in range(2)]
    retS = [sb("retS%d" % l, [128, 4, 256], F32) for l in range(L)]
    retSb = [sb("retSb%d" % l, [128, 4, 256], BF16) for l in range(L)]
    glaS = [sb("glaS%d" % l, [128, 4, 256], F32) for l in range(L)]
    glaSb = [sb("glaSb%d" % l, [128, 4, 256], BF16) for l in range(L)]
    gE = [sb("gE%d" % i, [128, T], F32) for i in range(2)]
    gEi = [sb("gEi%d" % i, [128, T], F32) for i in range(2)]
    gEs = [sb("gEs%d" % i, [128, T], F32) for i in range(2)]
    gaT = sb("gaT", [32, T], BF16)
    w2f = [sb("w2f%d" % l, [17, 512], F32) for l in range(L)]
    w2b = [sb("w2b%d" % l, [17, 512], BF16) for l in range(L)]
    KK = sb("KK", [128, 4, 128 + T], BF16)
    svt = sb("svt", [128, NT + 1, 256], BF16)
    ETb = [sb("ET%d" % i, [128, 512], BF16) for i in range(2)]
    PTs = sb("PTs", [128, 2, 4, 2, 128], BF16)
    den = sb("den", [128, 4, 128], F32)
    kkprev = [sb("kkprev%d" % l, [128, 4, 128], BF16) for l in range(L)]
    svprev = [sb("svprev%d" % l, [128, 256], BF16) for l in range(L)]
    par = [sb("par%d" % l, [128, NPAR], F32) for l in range(L)]
    es = [sb("es%d" % l, [128, 8], F32) for l in range(L)]
    ct = {}
    for name, ap in cd.items():
        ct[name] = sb("c_" + name, list(ap.shape), ap.dtype)
    onesb = sb("onesb32", [32, T], BF16)

    NPS = 7
    pst = [st.enter_context(nc.psum_tensor("ps%d" % i, [128, 512], F32)) for i in range(NPS)]
    ptr = st.enter_context(nc.psum_tensor("ptr", [128, 1024], BF16))
    psbuf = [Buf("ps%d" % i) for i in range(NPS)]
    for i in range(NPS):
        S.psgen[i] = 0
    psrr = [0]

    def ps_alloc():
        i = psrr[0] % NPS
        psrr[0] += 1
        S.psgen[i] += 1
        return PsH(i, S.psgen[i], pst[i], psbuf[i])

    def mm(out, lhsT, rhs, start, stop, reads, writes):
        S.op(PE, lambda e: e.matmul(out, lhsT=lhsT, rhs=rhs, start=start, stop=stop), reads=reads, writes=writes)

    def tr(out, in_, ident, reads, writes):
        S.op(PE, lambda e: e.transpose(out=out, in_=in_, identity=ident), reads=reads, writes=writes)

    def act(out, in_, func, reads, writes, **kw):
        S.op(ACT, lambda e: e.activation(out=out, in_=in_, func=func, **kw), reads=reads, writes=writes)

    def tt(out, in0, in1, op, reads, writes, eng=DVE):
        S.op(eng, lambda e: e.tensor_tensor(out=out, in0=in0, in1=in1, op=op), reads=reads, writes=writes)

    def ts(out, in0, s1, s2, op0, op1, reads, writes, eng=DVE):
        if s2 is None:
            S.op(eng, lambda e: e.tensor_scalar(out=out, in0=in0, scalar1=s1, scalar2=None, op0=op0),
                 reads=reads, writes=writes)
        else:
            S.op(eng, lambda e: e.tensor_scalar(out=out, in0=in0, scalar1=s1, scalar2=s2, op0=op0, op1=op1),
                 reads=reads, writes=writes)

    def stt(out, in0, scalar, in1, op0, op1, reads, writes):
        S.op(DVE, lambda e: e.scalar_tensor_tensor(out=out, in0=in0, scalar=scalar, in1=in1, op0=op0, op1=op1),
             reads=reads, writes=writes)

    def vcopy(out, in_, reads, writes):
        S.op(DVE, lambda e: e.tensor_copy(out=out, in_=in_), reads=reads, writes=writes)

    def recip(out, in_, reads, writes):
        S.op(DVE, lambda e: e.reciprocal(out=out, in_=in_), reads=reads, writes=writes)

    dsem_c = S.new_dma_sem("dc")
    dsem_x = S.new_dma_sem("dx")
    dsem_xs = [S.new_dma_sem("dx0"), S.new_dma_sem("dx1")]
    dsem_o = S.new_dma_sem("do")

    def dma(out, in_, reads, writes, sem, eng=SP):
        return S.op(eng, lambda e: e.dma_start(out=out, in_=in_), reads=reads, writes=writes, dma_sem=sem)

    for name in cd:
        dma(ct[name][:], cd[name], [], [B("c", name)], S.new_dma_sem("dc_" + name))
    for l in range(n_layers):
        dma(par[l][:], par_d[l], [], [B("par", l)], S.new_dma_sem("dc_par%d" % l))
        dma(w2f[l][:], w2_d[l], [], [B("w2f", l)], S.new_dma_sem("dc_w2%d" % l))
        vcopy(w2b[l][:], w2f[l][:], [B("w2f", l)], [B("w2b", l)])
        act(es[l][:], par[l][:, P_SINK:P_SINK + 8], AF.Exp, [B("par", l)], [B("es", l)])
    S.op(DVE, lambda e: e.memset(onesb[:], 1.0), writes=[B("onesb")])
    identf, identb, ones = ct["ident_f"], ct["ident_b"], ct["ones_b"]
    Bif, Bib, Bones = B("c", "ident_f"), B("c", "ident_b"), B("c", "ones_b")

    wsem = [S.new_dma_sem("dw%d" % i) for i in range(NST)]
    NRING = NBF + 2 * NST
    rsem = [S.new_dma_sem("dr%d" % i) for i in range(NRING)]
    dws = S.new_dma_sem("dws")
    wlist = []
    wstate = {"dma": 0, "cast": 0, "i0": None, "nconv": 0}
    wst_bf = [w_[:, :].bitcast(BF16) for w_ in wst]

    def ring_slot(r):
        if r < NBF:
            return wbf[r][:, :], [B("wbf", r)]
        s_, h_ = (r - NBF) // 2, (r - NBF) % 2
        return wst_bf[s_][:, h_ * 2048:(h_ + 1) * 2048], [B("wsth", s_, h_)]

    def wst_bufs(slot):
        return [B("wsth", slot, 0), B("wsth", slot, 1)]

    def w_issue_dma(i):
        view, KC, ncols, off, conv = wlist[i]
        n = KC * ncols
        if conv:
            slot = i % NST
            dst = wst[slot][:, 0:n].rearrange("p (k n) -> p k n", k=KC)
            dma(dst, view, [], wst_bufs(slot), wsem[slot])
        else:
            if wstate["i0"] is None:
                wstate["i0"] = i
            r = (i - wstate["i0"]) % NRING
            tl, bb = ring_slot(r)
            dma(tl[:, 0:n], wscr_d[:, off:off + n], [B("wscr_all")], bb, rsem[r])

    def w_issue_cast(i):
        view, KC, ncols, off, conv = wlist[i]
        if not conv:
            return
        slot, bslot = i % NST, i % NBF
        n = KC * ncols
        if i % 5 in (0, 2):
            vcopy(wbf[bslot][:, 0:n], wst[slot][:, 0:n], wst_bufs(slot), [B("wbf", bslot)])
        else:
            act(wbf[bslot][:, 0:n], wst[slot][:, 0:n], AF.Copy, wst_bufs(slot), [B("wbf", bslot)])
        dma(wscr_d[:, off:off + n], wbf[bslot][:, 0:n], [B("wbf", bslot)], [B("wscr_all")], dws, eng=POOL)

    def wget(i):
        assert i == wstate.get("last", -1) + 1, ("weight groups must be consumed in list order", i, wstate.get("last"))
        wstate["last"] = i
        nconv = wstate.get("n_conv")
        if nconv is None:
            nconv = wstate["n_conv"] = sum(1 for w_ in wlist if w_[4])

        def dma_ok(j):
            if wlist[j][4]:
                return j < i + NST
            return i >= nconv and j < i + NRING - 1
        while wstate["cast"] < min(len(wlist), i + 2):
            while wstate["dma"] <= wstate["cast"] and wlist[wstate["dma"]][4]:
                w_issue_dma(wstate["dma"])
                wstate["dma"] += 1
            w_issue_cast(wstate["cast"])
            wstate["cast"] += 1
        while wstate["dma"] < len(wlist) and dma_ok(wstate["dma"]):
            w_issue_dma(wstate["dma"])
            wstate["dma"] += 1
        assert wstate["dma"] > i
        view, KC, ncols, off, conv = wlist[i]
        n = KC * ncols
        if conv:
            return wbf[i % NBF][:, 0:n].rearrange("p (k n) -> p k n", k=KC), B("wbf", i % NBF)
        tl, bb = ring_slot((i - wstate["i0"]) % NRING)
        return tl[:, 0:n].rearrange("p (k n) -> p k n", k=KC), bb[0]

    scr_off = {}

    def layer_groups(l, conv):
        g = {}
        win = win_d[l].rearrange("(kc p) n -> p kc n", p=128)
        cursor = [l * WPL]

        def add(name, view, KC, ncols):
            g[name] = len(wlist)
            wlist.append((view, KC, ncols, cursor[0], conv))
            cursor[0] += KC * ncols
            assert cursor[0] <= (l + 1) * WPL

        def add_in(name, c0, n):
            for i in range(0, n, 256):
                w = min(256, n - i)
                add((name, i // 256), win[:, :, c0 + i:c0 + i + w], 8, w)
        add_in("rq", C_RQ, 512); add_in("rk", C_RK, 512); add_in("rv", C_RV, 1024); add_in("rg", C_RG, 1024)
        for n in range(3):
            if n == 1:
                add_in("ga", C_GA, 16); add_in("gv", C_GV, 1024)
                for i_ in range(2):
                    add(("gq", i_), win[:, :, C_GQ + i_ * 256:C_GQ + (i_ + 1) * 256], 8, 256)
                    add(("gk", i_), win[:, :, C_GK + i_ * 256:C_GK + (i_ + 1) * 256], 8, 256)
                add_in("gg", C_GG, 1024)
            if n == 2:
                add_in("sk", C_SK, 256); add_in("sv", C_SV, 256); add_in("sq", C_SQ, 1024)
            wb = wbr_d[l, n].rearrange("(kc p) n -> p kc n", p=128)
            for i in range(4):
                add(("gate", n, i), win[:, :, C_GATE + n * 1024 + i * 256:C_GATE + n * 1024 + (i + 1) * 256], 8, 256)
            for i in range(4):
                add(("br", n, i), wb[:, :, i * 256:(i + 1) * 256], 8, 256)
        wo = wout_d[l].rearrange("(kc p) n -> p kc n", p=128)
        for i in range(4):
            add(("wo", i), wo[:, :, i * 256:(i + 1) * 256], 8, 256)
        wgv = wg_d[l].rearrange("(kc p) n -> p kc n", p=128)
        wuv = wu_d[l].rearrange("(kc p) n -> p kc n", p=128)
        wdv = wd_d[l].rearrange("(kc p) n -> p kc n", p=128)
        for half in range(2):
            for i in range(6):
                w = 256 if i < 5 else 128
                c0 = half * 1408 + i * 256
                add(("fg", half, i), wgv[:, :, c0:c0 + w], 8, w)
                add(("fu", half, i), wuv[:, :, c0:c0 + w], 8, w)
            for c in range(8):
                add(("fd", half, c), wdv[:, half * 11:(half + 1) * 11, c * 128:(c + 1) * 128], 11, 128)
        return g

    def proj_fm(gidx, ncols_chunks, evac, src=None, srcB=None):
        if src is None:
            src, srcB = hT, [B("hT", c) for c in range(8)]
        wv, wb = wget(gidx)
        KC = wv.shape[1]
        ncols = wv.shape[2]
        j = 0
        c0 = 0
        while c0 < ncols:
            w = min(128, ncols - c0)
            p = ps_alloc()
            for kc in range(KC):
                if os.environ.get("KDBG_NOMM"):
                    continue
                mm(p.t[0:w, :], wv[:, kc, c0:c0 + w], src[:, kc, :], kc == 0, kc == KC - 1,
                   [wb, srcB[kc]], [p])
            if not os.environ.get("KDBG_NOEVAC"):
                evac(j, p, w)
            j += 1
            c0 += w

    def proj_tm(gidx, evac):
        wv, wb = wget(gidx)
        ncols = wv.shape[2]
        for t in range(NT):
            p = ps_alloc()
            for kc in range(8):
                mm(p.t[:, 0:ncols], hT[:, kc, t * 128:(t + 1) * 128], wv[:, kc, :], kc == 0, kc == 7,
                   [wb, B("hT", kc)], [p])
            evac(t, p, ncols)

    sq_rr = [0]

    def rmsnorm_stats(srcT, srcB, width_scale, eps):
        p = ps_alloc()
        for c in range(8):
            s = sq_rr[0] % 2
            sq_rr[0] += 1
            act(sqr[:, s, :], srcT[:, c, :], AF.Square, [srcB[c]], [B("sqr", s)])
            mm(p.t[:, :], ones[:, :], sqr[:, s, :], c == 0, c == 7, [Bones, B("sqr", s)], [p])
        act(tmpf[:], p.t[:, :], AF.Sqrt, [p, Beps(eps)], [B("tmpf")], scale=width_scale, bias=eps_ap(eps))
        recip(rstd[:], tmpf[:], [B("tmpf")], [B("rstd")])

    eps_tiles = {}

    def eps_ap(v):
        if v not in eps_tiles:
            t_ = sb("eps%d" % len(eps_tiles), [128, 1], F32)
            S.op(DVE, lambda e: e.memset(t_[:], float(v)), writes=[B("eps", v)])
            eps_tiles[v] = t_
        return eps_tiles[v][:, 0:1]

    for v_ in (1e-6, 1e-5, 1.0):
        eps_ap(v_)

    def Beps(v):
        return B("eps", v)

    def pre_norm(l, poff):
        rmsnorm_stats(xT, [B("xT", c) for c in range(8)], 1.0 / D, 1e-6)
        for c in range(8):
            stt(hT[:, c, :], xT[:, c, :], par[l][:, poff + c:poff + c + 1], rstd[:], ALU.mult, ALU.mult,
                [B("xT", c), B("par", l), B("rstd")], [B("hT", c)])

    def post_norm_residual(l, poff):
        rmsnorm_stats(osb, [B("osb", c) for c in range(8)], 1.0 / D, 1e-6)
        for c in range(8):
            stt(osb[:, c, :], osb[:, c, :], par[l][:, poff + c:poff + c + 1], rstd[:], ALU.mult, ALU.mult,
                [B("osb", c), B("par", l), B("rstd")], [B("osb", c)])
            tt(xT[:, c, :], xT[:, c, :], osb[:, c, :], ALU.add, [B("xT", c), B("osb", c)], [B("xT", c)])

    _act = act

    def ret_branch(l, g, first_mt):
        DTt, XIt, ZTt = ct["ret_dt"], ct["ret_xi"], ct["ret_zt"]
        XIb = XIt[:, :].rearrange("p (h i) -> p h i", h=4)
        ZTb = ZTt[:, :].rearrange("p (h i) -> p h i", h=4)

        def ev_q(gi):
            def f(j, p, w):
                h = gi * 2 + j
                for t_ in range(NT):
                    tt(qT[:, h, t_ * 128:(t_ + 1) * 128], p.t[:, t_ * 128:(t_ + 1) * 128], XIt[:, h * 128:(h + 1) * 128], ALU.mult,
                       [p, B("c", "ret_xi")], [B("qT", h)])
            return f

        def ev_k(gi):
            def f(j, p, w):
                h = gi * 2 + j
                act(kT[:, h, :], p.t[:, :], AF.Copy, [p], [B("kT", h)])
                for t_ in range(NT):
                    tt(kzT[:, h, t_ * 128:(t_ + 1) * 128], p.t[:, t_ * 128:(t_ + 1) * 128], ZTt[:, h * 128:(h + 1) * 128], ALU.mult,
                       [p, B("c", "ret_zt")], [B("kzT", h)])
            return f

        def ev_q2(gi):
            def f(j, p, w):
                h = gi * 2 + j
                act(obf[:, h, :], p.t[:, :], AF.Copy, [p], [B("obf", h)])
                ev_q(gi)(j, p, w)
            return f
        for gi in range(2):
            proj_fm(g[("rq", gi)], 2, ev_q2(gi))
        for gi in range(2):
            proj_fm(g[("rk", gi)], 2, ev_k(gi))

        dbg_dump("r1", xT, [B("xT", c_) for c_ in range(8)])

        def ev_v(gi):
            def f(t, p, n):
                act(vtok[:, t, gi * 256:(gi + 1) * 256], p.t[:, 0:256], AF.Copy, [p], [B("vtok", t)])
            return f
        for gi in range(4):
            proj_tm(g[("rv", gi)], ev_v(gi))
        dbg_dump("r2", xT, [B("xT", c_) for c_ in range(8)])
        if first_mt:
            S.op(DVE, lambda e: e.memset(retS[l][:], 0.0), writes=[B("retS", l)])
            S.op(DVE, lambda e: e.memset(retSb[l][:], 0.0), writes=[B("retSb", l)])
        def ret_stage_a(t):
            S.phase = 'ret_a'
            tok = slice(t * 128, (t + 1) * 128)
            a = t % 2
            p = ps_alloc()
            for h in range(4):
                mm(p.t[:, h * 128:(h + 1) * 128], kT[:, h, tok], obf[:, h, tok], True, True,
                   [B("kT", h), B("obf", h)], [p])
            tt(AT[a][:], p.t[:, :], DTt[:, :], ALU.mult, [p, B("c", "ret_dt")], [B("AT", a)])
            dbg_dump("r3", xT, [B("xT", c_) for c_ in range(8)])
            for h in range(4):
                tr(ptr[:, h * 128:(h + 1) * 128], kzT[:, h, tok], identb[:], [B("kzT", h), Bib], [B("ptr", 0)])
            act(ktok[a][:], ptr[:, 0:512], AF.Copy, [B("ptr", 0)], [B("ktok", a)])
            dbg_dump("r4", xT, [B("xT", c_) for c_ in range(8)])

        def ret_stage_b(t):
            S.phase = 'ret_b'
            tok = slice(t * 128, (t + 1) * 128)
            a = t % 2
            for hh in range(2):
                p = ps_alloc()
                for h2 in range(2):
                    h = hh * 2 + h2
                    for ec in range(2):
                        o_ = p.t[:, (h2 * 2 + ec) * 128:(h2 * 2 + ec + 1) * 128]
                        mm(o_, vtok[:, t, h * 256 + ec * 128:h * 256 + (ec + 1) * 128], AT[a][:, h * 128:(h + 1) * 128],
                           True, False, [B("vtok", t), B("AT", a)], [p])
                        mm(o_, retSb[l][:, h, ec * 128:(ec + 1) * 128], qT[:, h, tok], False, True,
                           [B("retSb", l), B("qT", h)], [p])
                act(osb[:, hh * 4:(hh + 1) * 4, tok], p.t[:, :].rearrange("p (c i) -> p c i", c=4), AF.Copy,
                    [p], [B("osb", c) for c in range(hh * 4, hh * 4 + 4)])
            dbg_dump("r5", xT, [B("xT", c_) for c_ in range(8)])
            for hh in range(2):
                p = ps_alloc()
                for h2 in range(2):
                    h = hh * 2 + h2
                    mm(p.t[:, h2 * 256:(h2 + 1) * 256], ktok[a][:, h * 128:(h + 1) * 128], vtok[:, t, h * 256:(h + 1) * 256],
                       True, True, [B("ktok", a), B("vtok", t)], [p])
                for h2 in range(2):
                    h = hh * 2 + h2
                    stt(retS[l][:, h, :], retS[l][:, h, :], RET_G128[h], p.t[:, h2 * 256:(h2 + 1) * 256], ALU.mult, ALU.add,
                        [B("retS", l), p], [B("retS", l)])
                act(retSb[l][:, hh * 2:hh * 2 + 2, :], retS[l][:, hh * 2:hh * 2 + 2, :], AF.Copy,
                    [B("retS", l)], [B("retSb", l)])

        ret_stage_a(0)
        for t in range(NT):
            if t + 1 < NT:
                ret_stage_a(t + 1)
            ret_stage_b(t)
        dbg_dump("r6", xT, [B("xT", c_) for c_ in range(8)])
        def ev_gs(gi):
            def f(j, p, w):
                c = gi * 2 + j
                act(sgall[:, c, :], p.t[:, :], AF.Silu, [p], [B("sgall", c)])
            return f

        def gn_pair(hp):
            S.phase = 'ret_gn'
            pms, pqs = [], []
            for h2 in range(2):
                h = hp * 2 + h2
                pm = ps_alloc()
                pq = ps_alloc()
                for ec in range(2):
                    c = h * 2 + ec
                    s = sq_rr[0] % 2
                    sq_rr[0] += 1
                    act(sqr[:, s, :], osb[:, c, :], AF.Copy, [B("osb", c)], [B("sqr", s)])
                    mm(pm.t[:, :], ones[:, :], sqr[:, s, :], ec == 0, ec == 1, [Bones, B("sqr", s)], [pm])
                    s = sq_rr[0] % 2
                    sq_rr[0] += 1
                    act(sqr[:, s, :], osb[:, c, :], AF.Square, [B("osb", c)], [B("sqr", s)])
                    mm(pq.t[:, :], ones[:, :], sqr[:, s, :], ec == 0, ec == 1, [Bones, B("sqr", s)], [pq])
                pms.append(pm)
                pqs.append(pq)
            TB = [B("tmpf"), B("tmpfB")]
            RB = [B("rstd"), B("rstdB")]
            for h2 in range(2):
                act(tmp2[:, h2, :], pms[h2].t[:, :], AF.Square, [pms[h2]], [TB[h2]], scale=1.0 / 256)
                stt(tmp2[:, h2, :], pqs[h2].t[:, :], 1.0 / 256, tmp2[:, h2, :], ALU.mult, ALU.subtract,
                    [pqs[h2], TB[h2]], [TB[h2]])
            act(tmp2[:, :, :], tmp2[:, :, :], AF.Sqrt, TB + [Beps(1e-5)], TB, bias=eps_ap(1e-5))
            recip(rstd2[:, :, :], tmp2[:, :, :], TB, RB)
            for h2 in range(2):
                stt(tmp2[:, h2, :], pms[h2].t[:, :], -1.0 / 256, rstd2[:, h2, :], ALU.mult, ALU.mult,
                    [pms[h2], RB[h2]], [TB[h2]])
            cs = list(range(hp * 4, hp * 4 + 4))
            OB = [B("osb", c) for c in cs]
            osb4 = osb[:, hp * 4:hp * 4 + 4, :].rearrange("p (h e) t -> p h e t", h=2)
            tt(osb4, osb4, rstd2[:, :, :].unsqueeze(2).to_broadcast([128, 2, 2, T]), ALU.mult, OB + RB, OB)
            tt(osb4, osb4, tmp2[:, :, :].unsqueeze(2).to_broadcast([128, 2, 2, T]), ALU.add, OB + TB, OB)
            for c in cs:
                act(osb[:, c, :], osb[:, c, :], AF.Identity, [B("osb", c), B("par", l)], [B("osb", c)],
                    scale=par[l][:, P_RETW + c:P_RETW + c + 1], bias=par[l][:, P_RETB + c:P_RETB + c + 1])
            tt(obf[:, hp * 4:hp * 4 + 4, :], osb[:, hp * 4:hp * 4 + 4, :], sgall[:, hp * 4:hp * 4 + 4, :], ALU.mult,
               OB + [B("sgall", c) for c in cs], [B("obf", c) for c in cs])

        S.phase = 'ret_rg'
        for gi in range(4):
            proj_fm(g[("rg", gi)], 2, ev_gs(gi))
        gn_pair(0)
        gn_pair(1)

    def gla_branch2(l, g, first_mt):
        TRI, SLm = ct["gla_tri"], ct["gla_sl"]
        vcopy(gaT[:, :], onesb[:, :], [B("onesb")], [B("gaT")])

        def ev_ga(j, p, w):
            act(gaT[0:16, :], p.t[0:16, :], AF.Copy, [p], [B("gaT")])
        proj_fm(g[("ga", 0)], 1, ev_ga)

        def ev_v(gi):
            def f(t, p, n):
                act(vtok[:, t, gi * 256:(gi + 1) * 256], p.t[:, 0:256], AF.Copy, [p], [B("vtok", t)])
            return f
        for gi in range(4):
            proj_tm(g[("gv", gi)], ev_v(gi))
        if first_mt:
            S.op(DVE, lambda e: e.memset(glaS[l][:], 0.0), writes=[B("glaS", l, h_) for h_ in range(4)])
            S.op(DVE, lambda e: e.memset(glaSb[l][:], 0.0), writes=[B("glaSb", l, h_) for h_ in range(4)])
        for t in range(NT):
            pz = ps_alloc()
            mm(pz.t[:, :], gaT[0:17, t * 128:(t + 1) * 128], w2b[l][0:17, :], True, True,
               [B("gaT"), B("w2b", l)], [pz])
            act(osb[:, t, :], pz.t[:, :], AF.Exp, [pz], [B("osb", t)], scale=-1.0)
            act(osb[:, t, :], osb[:, t, :], AF.Ln, [B("osb", t), Beps(1.0)], [B("osb", t)], bias=eps_ap(1.0))
            ts(osb[:, t, :], osb[:, t, :], -1.0 / 16, -1.0, ALU.mult, ALU.max, [B("osb", t)], [B("osb", t)])
        for gi in range(2):
            for j in range(2):
                h = gi * 2 + j
                pb_ = ps_alloc()
                pl_ = ps_alloc()
                for t in range(NT):
                    mm(pb_.t[:, t * 128:(t + 1) * 128], osb[:, t, h * 128:(h + 1) * 128], TRI[:, :], True, True,
                       [B("osb", t), B("c", "gla_tri")], [pb_])
                    mm(pl_.t[:, t * 128:(t + 1) * 128], osb[:, t, h * 128:(h + 1) * 128], SLm[:, :], True, True,
                       [B("osb", t), B("c", "gla_sl")], [pl_])
                act(gE[j][:], pb_.t[:, :], AF.Exp, [pb_], [B("gE", j)])
                act(gEi[j][:], pb_.t[:, :], AF.Exp, [pb_], [B("gEi", j)], scale=-1.0)
                act(gEs[j][:], pl_.t[:, :], AF.Exp, [pl_], [B("gEs", j)])
                vcopy(gdec[:, h, :], gE[j][:, :].rearrange("p (b i) -> p b i", i=64)[:, :, 63], [B("gE", j)], [B("gdec")])
            wv, wb = wget(g[("gq", gi)])
            for j in range(2):
                h = gi * 2 + j
                p = ps_alloc()
                for kc in range(8):
                    mm(p.t[:, :], wv[:, kc, j * 128:(j + 1) * 128], hT[:, kc, :], kc == 0, kc == 7, [wb, B("hT", kc)], [p])
                stt(qT[:, h, :], p.t[:, :], 128.0 ** -0.5, gE[j][:], ALU.mult, ALU.mult, [p, B("gE", j)], [B("qT", h)])
            wv, wb = wget(g[("gk", gi)])
            for j in range(2):
                h = gi * 2 + j
                p = ps_alloc()
                for kc in range(8):
                    mm(p.t[:, :], wv[:, kc, j * 128:(j + 1) * 128], hT[:, kc, :], kc == 0, kc == 7, [wb, B("hT", kc)], [p])
                tt(kT[:, h, :], p.t[:, :], gEi[j][:], ALU.mult, [p, B("gEi", j)], [B("kT", h)])
                tt(kzT[:, h, :], p.t[:, :], gEs[j][:], ALU.mult, [p, B("gEs", j)], [B("kzT", h)])
        TRIb = TRI[:, :].unsqueeze(1).to_broadcast([128, 4, 128])

        def gla_stage_a(t):
            S.phase = 'gla_a'
            tok = slice(t * 128, (t + 1) * 128)
            a = t % 2
            p = ps_alloc()
            for h in range(4):
                mm(p.t[:, h * 128:(h + 1) * 128], kT[:, h, tok], qT[:, h, tok], True, True,
                   [B("kT", h), B("qT", h)], [p])
            tt(AT[a][:].rearrange("p (h i) -> p h i", h=4), p.t[:, :].rearrange("p (h i) -> p h i", h=4), TRIb, ALU.mult,
               [p, B("c", "gla_tri")], [B("AT", a)])
            for h in range(4):
                tr(ptr[:, h * 128:(h + 1) * 128], kzT[:, h, tok], identb[:], [B("kzT", h), Bib], [B("ptr", 0)])
            act(ktok[a][:], ptr[:, 0:512], AF.Copy, [B("ptr", 0)], [B("ktok", a)])

        def gla_stage_b(t):
            S.phase = 'gla_b'
            tok = slice(t * 128, (t + 1) * 128)
            a = t % 2
            po = [ps_alloc(), ps_alloc()]
            for blk in range(2):
                bt = slice(t * 128 + blk * 64, t * 128 + blk * 64 + 64)
                for h in range(4):
                    for ec in range(2):
                        base = ((h % 2) * 2 + ec) * 128 + blk * 64
                        o_ = po[h // 2].t[:, base:base + 64]
                        mm(o_, vtok[:, t, h * 256 + ec * 128:h * 256 + (ec + 1) * 128],
                           AT[a][:, h * 128 + blk * 64:h * 128 + blk * 64 + 64],
                           True, False, [B("vtok", t), B("AT", a)], [po[h // 2]])
                        mm(o_, glaSb[l][:, h, ec * 128:(ec + 1) * 128], qT[:, h, bt],
                           False, True, [B("glaSb", l, h), B("qT", h)], [po[h // 2]])
                for hh in range(2):
                    pk = ps_alloc()
                    for h2 in range(2):
                        h = hh * 2 + h2
                        mm(pk.t[:, h2 * 256:(h2 + 1) * 256], ktok[a][blk * 64:(blk + 1) * 64, h * 128:(h + 1) * 128],
                           vtok[blk * 64:(blk + 1) * 64, t, h * 256:(h + 1) * 256], True, True,
                           [B("ktok", a), B("vtok", t)], [pk])
                    for h2 in range(2):
                        h = hh * 2 + h2
                        stt(glaS[l][:, h, :], glaS[l][:, h, :], gdec[:, h, t * 2 + blk:t * 2 + blk + 1],
                            pk.t[:, h2 * 256:(h2 + 1) * 256], ALU.mult, ALU.add,
                            [B("glaS", l, h), B("gdec"), pk], [B("glaS", l, h)])
                        act(glaSb[l][:, h, :], glaS[l][:, h, :], AF.Copy, [B("glaS", l, h)], [B("glaSb", l, h)])
            for hh in range(2):
                act(obf_raw(hh, tok), po[hh].t[:, :].rearrange("p (c i) -> p c i", c=4), AF.Copy,
                    [po[hh]], [B("osb", c) for c in range(hh * 4, hh * 4 + 4)])

        gla_stage_a(0)
        for t in range(NT):
            if t + 1 < NT:
                gla_stage_a(t + 1)
            gla_stage_b(t)
        def ev_gs(gi):
            def f(j, p, w):
                c = gi * 2 + j
                act(sgall[:, c, :], p.t[:, :], AF.Silu, [p], [B("sgall", c)])
            return f

        def rms_head(h):
            S.phase = 'gla_rms'
            pq = ps_alloc()
            for ec in range(2):
                c = h * 2 + ec
                s = sq_rr[0] % 2
                sq_rr[0] += 1
                act(sqr[:, s, :], osb[:, c, :], AF.Square, [B("osb", c)], [B("sqr", s)])
                mm(pq.t[:, :], ones[:, :], sqr[:, s, :], ec == 0, ec == 1, [Bones, B("sqr", s)], [pq])
            act(tmpf[:], pq.t[:, :], AF.Sqrt, [pq, Beps(1e-6)], [B("tmpf")], scale=1.0 / 256, bias=eps_ap(1e-6))
            recip(rstd[:], tmpf[:], [B("tmpf")], [B("rstd")])
            for ec in range(2):
                c = h * 2 + ec
                stt(osb[:, c, :], osb[:, c, :], par[l][:, P_GLAW + c:P_GLAW + c + 1], rstd[:], ALU.mult, ALU.mult,
                    [B("osb", c), B("par", l), B("rstd")], [B("osb", c)])
                tt(obf[:, c, :], osb[:, c, :], sgall[:, c, :], ALU.mult, [B("osb", c), B("sgall", c)], [B("obf", c)])

        proj_fm(g[("gg", 0)], 2, ev_gs(0))
        for h in range(4):
            if h + 1 < 4:
                proj_fm(g[("gg", h + 1)], 2, ev_gs(h + 1))
            rms_head(h)

    gdec = sb("gdec", [128, 4, 2 * NT], F32)

    def obf_raw(hh, tok):
        return osb[:, hh * 4:(hh + 1) * 4, tok]

    def swa_branch(l, g, first_mt):
        MASK = ct["swa_mask"][:, :].rearrange("p (s c k q) -> p s c k q", s=2, c=8, k=2)
        wv, wb = wget(g[("sk", 0)])
        if first_mt:
            S.op(DVE, lambda e: e.memset(KK[:, :, 0:128], 0.0), writes=[B("KK", gg) for gg in range(4)])
            S.op(DVE, lambda e: e.memset(svt[:, 0, :], 0.0), writes=[B("svt", 0)])
        else:
            vcopy(KK[:, :, 0:128], kkprev[l][:, :, :], [B("kkprev", l)], [B("KK", gg) for gg in range(4)])
            vcopy(svt[:, 0, :], svprev[l][:, :], [B("svprev", l)], [B("svt", 0)])
        for gg in range(4):
            p = ps_alloc()
            for half in range(2):
                for kc in range(8):
                    mm(p.t[half * 64:(half + 1) * 64, :], wv[:, kc, gg * 64:(gg + 1) * 64], hT[:, kc, :], kc == 0, kc == 7,
                       [wb, B("hT", kc)], [p])
            act(KK[:, gg, 128:128 + T], p.t[:, :], AF.Copy, [p], [B("KK", gg)])
        wv, wb = wget(g[("sv", 0)])
        for t in range(NT):
            p = ps_alloc()
            for kc in range(8):
                mm(p.t[:, 0:256], hT[:, kc, t * 128:(t + 1) * 128], wv[:, kc, :], kc == 0, kc == 7, [wb, B("hT", kc)], [p])
            act(svt[:, t + 1, :], p.t[:, 0:256], AF.Copy, [p], [B("svt", t + 1)])
        def qdst(c):
            return (qT, "qT", c) if c < 4 else (kT, "kT", c - 4)

        def ev_q(gi):
            def f(j, p, w):
                c = gi * 2 + j
                tl, nm, ci = qdst(c)
                act(tl[:, ci, :], p.t[:, :], AF.Copy, [p], [B(nm, ci)])
            return f
        for gi in range(4):
            proj_fm(g[("sq", gi)], 2, ev_q(gi))
        vcopy(kkprev[l][:, :, :], KK[:, :, T:T + 128], [B("KK", gg) for gg in range(4)], [B("kkprev", l)])
        vcopy(svprev[l][:, :], svt[:, NT, :], [B("svt", NT)], [B("svprev", l)])
        esb = es[l]
        for t in range(NT):
            tok = slice(t * 128, (t + 1) * 128)
            kts = [1] if (first_mt and t == 0) else [0, 1]
            for half in range(2):
                for s in range(2):
                    for pair in range(2):
                        p = ps_alloc()
                        for ci2 in range(2):
                            cc = pair * 2 + ci2
                            c = half * 4 + cc
                            tl, nm, ci = qdst(c)
                            gkv = c // 2
                            for kt in kts:
                                kcols = slice(t * 128 + kt * 128, t * 128 + kt * 128 + 128)
                                mm(p.t[:, (ci2 * 2 + kt) * 128:(ci2 * 2 + kt + 1) * 128], KK[s * 64:(s + 1) * 64, gkv, kcols],
                                   tl[s * 64:(s + 1) * 64, ci, tok], True, True, [B("KK", gkv), B(nm, ci)], [p])
                        e_ = pair
                        c0 = half * 4 + pair * 2
                        if len(kts) == 2:
                            act(ETb[e_][:], p.t[:, :], AF.Exp, [p], [B("ET", e_)], scale=0.125)
                            tt(PTs[:, s, pair * 2:pair * 2 + 2, :, :], ETb[e_][:].rearrange("p (h k q) -> p h k q", h=2, k=2),
                               MASK[:, s, c0:c0 + 2, :, :], ALU.mult, [B("ET", e_), B("c", "swa_mask")], [B("PTs", s, pair)])
                        else:
                            for ci2 in range(2):
                                act(ETb[e_][:, ci2 * 256 + 128:ci2 * 256 + 256], p.t[:, ci2 * 256 + 128:ci2 * 256 + 256], AF.Exp,
                                    [p], [B("ET", e_)], scale=0.125)
                            tt(PTs[:, s, pair * 2:pair * 2 + 2, 1, :],
                               ETb[e_][:].rearrange("p (h k q) -> p h k q", h=2, k=2)[:, :, 1, :],
                               MASK[:, s, c0:c0 + 2, 1, :], ALU.mult, [B("ET", e_), B("c", "swa_mask")], [B("PTs", s, pair)])
                po = ps_alloc()
                pd = ps_alloc()
                for cc in range(4):
                    c = half * 4 + cc
                    gkv = c // 2
                    for s in range(2):
                        for i, kt in enumerate(kts):
                            mm(po.t[s * 64:(s + 1) * 64, cc * 128:(cc + 1) * 128], svt[:, t + kt, gkv * 64:(gkv + 1) * 64],
                               PTs[:, s, cc, kt, :], i == 0, i == len(kts) - 1, [B("svt", t + kt), B("PTs", s, cc // 2)], [po])
                        for i, kt in enumerate(kts):
                            mm(pd.t[s * 64:(s + 1) * 64, cc * 128:(cc + 1) * 128], ones[:, 0:64],
                               PTs[:, s, cc, kt, :], i == 0, i == len(kts) - 1, [Bones, B("PTs", s, cc // 2)], [pd])
                tt(den[:, :, :], pd.t[:, :].rearrange("p (c q) -> p c q", c=4),
                   esb[:, half * 4:(half + 1) * 4].unsqueeze(2).to_broadcast([128, 4, 128]), ALU.add,
                   [pd, B("es", l)], [B("den")])
                recip(den[:, :, :], den[:, :, :], [B("den")], [B("den")])
                tt(obf[:, half * 4:(half + 1) * 4, tok], po.t[:, :].rearrange("p (c q) -> p c q", c=4), den[:, :, :], ALU.mult,
                   [po, B("den")], [B("obf", c) for c in range(half * 4, half * 4 + 4)])

    def branch_merge(l, g, n):
        for i in range(4):
            wvg, wbg = wget(g[("gate", n, i)])
            for j in range(2):
                c = i * 2 + j
                p = ps_alloc()
                for kc in range(8):
                    mm(p.t[:, :], wvg[:, kc, j * 128:(j + 1) * 128], hT[:, kc, :], kc == 0, kc == 7, [wbg, B("hT", kc)], [p])
                act(sgall[:, c, :], p.t[:, :], AF.Sigmoid, [p], [B("sgall", c)])
        for i in range(4):
            wvb, wbb = wget(g[("br", n, i)])
            for j in range(2):
                c = i * 2 + j
                p = ps_alloc()
                for kc in range(8):
                    mm(p.t[:, :], wvb[:, kc, j * 128:(j + 1) * 128], obf[:, kc, :], kc == 0, kc == 7, [wbb, B("obf", kc)], [p])
                if n == 0:
                    tt(merged[:, c, :], p.t[:, :], sgall[:, c, :], ALU.mult, [p, B("sgall", c)], [B("merged", c)])
                else:
                    tt(tmpf[:], p.t[:, :], sgall[:, c, :], ALU.mult, [p, B("sgall", c)], [B("tmpf")])
                    tt(merged[:, c, :], merged[:, c, :], tmpf[:], ALU.add, [B("merged", c), B("tmpf")], [B("merged", c)])

    def out_proj(l, g):
        for i in range(4):
            wv, wb = wget(g[("wo", i)])
            for j in range(2):
                c = i * 2 + j
                p = ps_alloc()
                for kc in range(8):
                    mm(p.t[:, :], wv[:, kc, j * 128:(j + 1) * 128], merged[:, kc, :], kc == 0, kc == 7,
                       [wb, B("merged", kc)], [p])
                act(osb[:, c, :], p.t[:, :], AF.Copy, [p], [B("osb", c)])

    def fdst(j):
        return (obf, "obf", j) if j < 8 else (kzT, "kzT", j - 8)

    def ffn(l, g):
        for half in range(2):
            for i in range(6):
                wvg, wbg = wget(g[("fg", half, i)])
                nj = wvg.shape[2] // 128
                for jj in range(nj):
                    j = i * 2 + jj
                    p = ps_alloc()
                    for kc in range(8):
                        mm(p.t[:, :], wvg[:, kc, jj * 128:(jj + 1) * 128], hT[:, kc, :], kc == 0, kc == 7, [wbg, B("hT", kc)], [p])
                    s = j % 2
                    act(sgr[:, s, :], p.t[:, :], AF.Silu, [p], [B("sgr", s)])
                wvu, wbu = wget(g[("fu", half, i)])
                for jj in range(nj):
                    j = i * 2 + jj
                    s = j % 2
                    tl, nm, ci = fdst(j)
                    p = ps_alloc()
                    for kc in range(8):
                        mm(p.t[:, :], wvu[:, kc, jj * 128:(jj + 1) * 128], hT[:, kc, :], kc == 0, kc == 7, [wbu, B("hT", kc)], [p])
                    tt(tl[:, ci, :], p.t[:, :], sgr[:, s, :], ALU.mult, [p, B("sgr", s)], [B(nm, ci)])
            for c in range(8):
                wv, wb = wget(g[("fd", half, c)])
                p = ps_alloc()
                for j in range(11):
                    tl, nm, ci = fdst(j)
                    mm(p.t[:, :], wv[:, j, :], tl[:, ci, :], j == 0, j == 10, [wb, B(nm, ci)], [p])
                if half == 0:
                    act(osb[:, c, :], p.t[:, :], AF.Copy, [p], [B("osb", c)])
                else:
                    tt(osb[:, c, :], osb[:, c, :], p.t[:, :], ALU.add, [B("osb", c), p], [B("osb", c)])

    def f32_stage(tile_bf16):
        return tile_bf16[:, :, :].rearrange("p c t -> p (c t)").bitcast(F32).rearrange("p (t d) -> p t d", t=2)

    ld_stage = [(f32_stage(sgall), "sgall"), (f32_stage(merged), "merged")]
    st_stage = osb[:, :, :].rearrange("p c t -> p (c t)").rearrange("p (t d) -> p t d", t=4)

    def issue_x_load(sq, mt):
        for half, (stg, nm) in enumerate(ld_stage):
            r0 = mt * T + half * 256
            dma(stg, x_d[sq, r0:r0 + 256, :].rearrange("(t p) d -> p t d", p=128), [], [B(nm, c) for c in range(8)],
                dsem_xs[half], eng=SP)

    def load_x(sq, mt):
        for t in range(NT):
            stg, nm = ld_stage[t // 2]
            for hh in range(2):
                p = ps_alloc()
                for c4 in range(4):
                    c = hh * 4 + c4
                    tr(p.t[:, c4 * 128:(c4 + 1) * 128], stg[:, t % 2, c * 128:(c + 1) * 128], identf[:],
                       [B(nm, c_) for c_ in range(8)] + [Bif], [p])
                act(xT[:, hh * 4:(hh + 1) * 4, t * 128:(t + 1) * 128], p.t[:, :].rearrange("p (c i) -> p c i", c=4), AF.Copy,
                    [p], [B("xT", c) for c in range(hh * 4, hh * 4 + 4)])

    out_deps = []

    def store_x(sq, mt):
        for t in range(NT):
            for hh in range(2):
                p = ps_alloc()
                for c4 in range(4):
                    c = hh * 4 + c4
                    tr(p.t[:, c4 * 128:(c4 + 1) * 128], xT[:, c, t * 128:(t + 1) * 128], identf[:], [B("xT", c), Bif], [p])
                act(st_stage[:, t, hh * 512:(hh + 1) * 512], p.t[:, :], AF.Copy, [p], [B("osb", c) for c in range(8)])
        r0 = mt * T
        out_deps.append(dma(out_d[sq, r0:r0 + T, :].rearrange("(t p) d -> p t d", p=128), st_stage,
                            [B("osb", c) for c in range(8)], [], dsem_o, eng=POOL))

    class StopBuild(Exception):
        pass

    def dbg_dump(stage, tile, names):
        if not dbg or dbg.get("stage") != stage:
            return
        dsem_d = S.new_dma_sem("ddbg")
        if tile.dtype == BF16:
            dbgt = sb("dbgt", [128, 4, T], F32)
            for hf in range(2):
                vcopy(dbgt[:], tile[:, hf * 4:(hf + 1) * 4, :], names, [B("dbgt")])
                dep = dma(dbg_d[:, hf * 4 * T:(hf + 1) * 4 * T], dbgt[:].rearrange("p c t -> p (c t)"), [B("dbgt")], [], dsem_d)
        else:
            dep = dma(dbg_d, tile[:].rearrange("p c t -> p (c t)"), names, [], dsem_d)
        out_deps.append(dep)
        raise StopBuild()

    groups = {}
    plan = []
    for sq in range(n_seq):
        for mt in range(n_mt):
            for l in range(n_layers):
                groups[(sq, mt, l)] = layer_groups(l, conv=(sq == 0 and mt == 0))
    only = (dbg or {}).get("only")
    c8 = list(range(8))
    try:
      order = [(sq_, mt_) for sq_ in range(n_seq) for mt_ in range(n_mt)]
      issue_x_load(*order[0])
      for oi, (sq, mt) in enumerate(order):
        if True:
            S.phase = 'load_x'
            load_x(sq, mt)
            dbg_dump("x", xT, [B("xT", c) for c in c8])
            for l in range(n_layers):
                g = groups[(sq, mt, l)]
                first = (mt == 0)
                S.phase = 'pre_norm_P_MIXPRE'
                pre_norm(l, P_MIXPRE)
                dbg_dump("h", hT, [B("hT", c) for c in c8])
                S.phase = 'ret_branch'
                ret_branch(l, g, first)
                dbg_dump("o_ret", obf, [B("obf", c) for c in c8])
                S.phase = 'branch_merge_0'
                branch_merge(l, g, 0)
                S.phase = 'gla_branch2'
                gla_branch2(l, g, first)
                dbg_dump("o_gla", obf, [B("obf", c) for c in c8])
                S.phase = 'branch_merge_1'
                branch_merge(l, g, 1)
                S.phase = 'swa_branch'
                swa_branch(l, g, first)
                dbg_dump("o_swa", obf, [B("obf", c) for c in c8])
                S.phase = 'branch_merge_2'
                branch_merge(l, g, 2)
                dbg_dump("merged", merged, [B("merged", c) for c in c8])
                S.phase = 'out_proj'
                out_proj(l, g)
                S.phase = 'post_norm_residual_P_MIXPOST'
                post_norm_residual(l, P_MIXPOST)
                dbg_dump("x1", xT, [B("xT", c) for c in c8])
                S.phase = 'pre_norm_P_FFNPRE'
                pre_norm(l, P_FFNPRE)
                if l == n_layers - 1 and oi + 1 < len(order):
                    issue_x_load(*order[oi + 1])
                S.phase = 'ffn'
                ffn(l, g)
                S.phase = 'post_norm_residual_P_FFNPOST'
                post_norm_residual(l, P_FFNPOST)
            S.phase = 'store_x'
            store_x(sq, mt)
    except StopBuild:
        pass
    S.final_wait(POOL, out_deps)
    S.emit()
    st.close()
    return nc, S


def host_inputs(inputs):
    f = np.float32
    shared = dict(make_consts())
    for k in ("w_in", "w_branch", "w_out", "ffn_w_gate", "ffn_w_up", "ffn_w_down"):
        shared[k] = np.ascontiguousarray(np.asarray(inputs[k], dtype=f))
    par = np.zeros((L, 128, NPAR), dtype=f)
    for l in range(L):
        par[l, :, P_MIXPRE:P_MIXPRE + 8] = col(inputs["norm_mix_pre"][l])
        par[l, :, P_MIXPOST:P_MIXPOST + 8] = col(inputs["norm_mix_post"][l])
        par[l, :, P_FFNPRE:P_FFNPRE + 8] = col(inputs["norm_ffn_pre"][l])
        par[l, :, P_FFNPOST:P_FFNPOST + 8] = col(inputs["norm_ffn_post"][l])
        par[l, :, P_RETW:P_RETW + 8] = col(inputs["ret_norm_w"][l])
        par[l, :, P_RETB:P_RETB + 8] = col(inputs["ret_norm_b"][l])
        par[l, :, P_GLAW:P_GLAW + 8] = col(inputs["gla_norm_w"][l])
        sk = np.asarray(inputs["attn_sinks"][l], dtype=f)
        par[l, :, P_SINK:P_SINK + 8] = sk.reshape(8, 2).T[np.arange(128) // 64, :]
    shared["par"] = par
    w2 = np.concatenate([np.asarray(inputs["gla_w_alpha2"], dtype=f),
                         np.asarray(inputs["gla_b_alpha"], dtype=f)[:, None, :]], axis=1)
    shared["w2aug"] = np.ascontiguousarray(w2)
    return shared


_CACHE = {}


def kernel(**inputs):
    x = np.asarray(inputs["x"], dtype=np.float32)
    n = 8
    shared = host_inputs(inputs)
    if "nc" not in _CACHE:
        _CACHE["nc"] = build()[0]
    nc = _CACHE["nc"]
    in_maps = []
    for i in range(n):
        m = dict(shared)
        m["x"] = np.ascontiguousarray(x[2 * i:2 * i + 2])
        in_maps.append(m)
    res = run_bass_kernel_spmd(nc, in_maps, core_ids=list(range(n)))
    out = np.concatenate([np.asarray(r["out"], dtype=np.float32) for r in res.results], axis=0)
    return out
```

```python
import contextlib
import os
import numpy as np
import ml_dtypes
import concourse.bass as bass
import concourse.mybir as mybir
from concourse.bass_utils import run_bass_kernel_spmd

F32 = mybir.dt.float32
BF16 = mybir.dt.bfloat16
AF = mybir.ActivationFunctionType
ALU = mybir.AluOpType

PE, ACT, DVE, POOL, SP = "pe", "act", "dve", "pool", "sp"
ENGS = (PE, ACT, DVE, POOL, SP)

D = 1024
SEQ = 2048
L = 2
T = 512
NT = T // 128
DIN = 10768
DFF = 2816
C_RQ, C_RK, C_RV, C_RG = 0, 512, 1024, 2048
C_GQ, C_GK, C_GV, C_GG, C_GA = 3072, 3584, 4096, 5120, 6144
C_SQ, C_SK, C_SV, C_GATE = 6160, 7184, 7440, 7696
NPAR = 64
WPL = (D * DIN + 3 * D * D + D * D + 3 * D * DFF) // 128
P_MIXPRE, P_MIXPOST, P_FFNPRE, P_FFNPOST, P_RETW, P_RETB, P_GLAW, P_SINK = 0, 8, 16, 24, 32, 40, 48, 56


class Buf:
    __slots__ = ("name", "w", "r")

    def __init__(self, name=""):
        self.name = name
        self.w = None
        self.r = {}


class PsH:
    __slots__ = ("bank", "gen", "t", "buf")

    def __init__(self, bank, gen, t, buf):
        self.bank, self.gen, self.t, self.buf = bank, gen, t, buf


class Sched:
    def __init__(self, nc):
        self.nc = nc
        self.q = {e: [] for e in ENGS}
        self.cnt = {e: 0 for e in ENGS}
        self.seen = {e: {} for e in ENGS}
        self.dma_sems = []
        self.nops = 0
        self.nwaits = 0
        self.psgen = {}
        self.phase = ''
        self.pe_phase = []

    def new_dma_sem(self, name):
        self.dma_sems.append(name)
        self.cnt[name] = 0
        return name

    def _buf(self, b):
        if isinstance(b, PsH):
            assert self.psgen[b.bank] == b.gen, "stale PSUM handle bank %d" % b.bank
            return b.buf
        return b

    def op(self, eng, fn, reads=(), writes=(), dma_sem=None):
        need = {}

        def req(dep, skip_own):
            if dep is None:
                return
            k, c = dep
            if skip_own and k == eng and dma_sem is None and eng == PE:
                return
            if c > need.get(k, 0):
                need[k] = c

        ps_reads = [self._buf(b) for b in reads if isinstance(b, PsH)]
        reads = [self._buf(b) for b in reads]
        writes = [self._buf(b) for b in writes]
        for b in reads:
            req(b.w, False)
        for b in ps_reads:
            for k, c in b.r.items():
                if k != eng:
                    req((k, c), False)
        for b in writes:
            req(b.w, True)
            for k, c in b.r.items():
                req((k, c), True)
        waits = []
        seen = self.seen[eng]
        for k, c in need.items():
            if seen.get(k, 0) < c:
                seen[k] = c
                waits.append((k, c))
        if dma_sem is None:
            key = eng
            self.cnt[eng] += 1
            c = self.cnt[eng]
        else:
            key = dma_sem
            self.cnt[dma_sem] += 16
            c = self.cnt[dma_sem]
        for b in writes:
            b.w = (key, c)
            b.r = {}
        for b in reads:
            if b.r.get(key, 0) < c:
                b.r[key] = c
        self.q[eng].append((waits, fn, key))
        if eng == PE:
            self.pe_phase.append(self.phase)
        self.nops += 1
        self.nwaits += len(waits)
        return (key, c)

    def final_wait(self, eng, deps):
        waits = []
        for k, c in deps:
            if self.seen[eng].get(k, 0) < c:
                self.seen[eng][k] = c
                waits.append((k, c))
        self.q[eng].append((waits, None, None))

    def emit(self):
        nc = self.nc
        with contextlib.ExitStack() as st:
            sems = {}
            for k in list(ENGS) + self.dma_sems:
                sems[k] = st.enter_context(nc.semaphore("s_" + k))
            block = st.enter_context(nc.Block())

            def run(engname):
                def body(e):
                    for waits, fn, key in self.q[engname]:
                        if fn is None:
                            for k, c in waits:
                                e.wait_ge(sems[k], c)
                            continue
                        for k, c in waits[1:]:
                            e.wait_ge(sems[k], c)
                        ins = fn(e)
                        if waits:
                            ins._wait_ge(sems[waits[0][0]], waits[0][1])
                        ins.then_inc(sems[key], 1 if key in ENGS else 16)
                return body

            block.tensor(run(PE))
            block.scalar(run(ACT))
            block.vector(run(DVE))
            block.gpsimd(run(POOL))
            block.sync(run(SP))


def make_consts():
    f = np.float32
    c = {}
    c["ident_f"] = np.eye(128, dtype=f)
    c["ident_b"] = np.eye(128, dtype=f).astype(ml_dtypes.bfloat16)
    c["ones_b"] = np.ones((128, 128), dtype=f).astype(ml_dtypes.bfloat16)
    H = 4
    logg = np.log1p(-np.exp2(-5.0 - np.arange(H, dtype=np.float64)))
    pos = np.arange(128, dtype=np.float64)
    scale = 128.0 ** -0.5
    rel = pos[None, :] - pos[:, None]
    DTm = np.zeros((128, H, 128), dtype=np.float64)
    for h in range(H):
        DTm[:, h, :] = np.where(rel >= 0, scale * np.exp(rel * logg[h]), 0.0)
    c["ret_dt"] = DTm.astype(f).reshape(128, H * 128)
    XI = np.zeros((128, H, 128))
    ZT = np.zeros((128, H, 128))
    for h in range(H):
        XI[:, h, :] = (scale * np.exp((pos + 1.0) * logg[h]))[None, :]
        ZT[:, h, :] = np.exp((127.0 - pos) * logg[h])[None, :]
    c["ret_xi"] = XI.astype(f).reshape(128, H * 128)
    c["ret_zt"] = ZT.astype(f).reshape(128, H * 128)
    blk = (np.arange(128) // 64)
    same = blk[:, None] == blk[None, :]
    jj = np.arange(128)[:, None]
    ii = np.arange(128)[None, :]
    c["gla_tri"] = (same & (jj <= ii)).astype(f)
    c["gla_sl"] = (same & (jj > ii)).astype(f)
    Hq = 16
    slopes = np.exp2(-8.0 * np.arange(1, Hq + 1, dtype=np.float64) / Hq)
    M = np.zeros((128, Hq, 2, 128))
    k = np.arange(128)[:, None].astype(np.float64)
    q = np.arange(128)[None, :].astype(np.float64)
    for h in range(Hq):
        d1 = q - k
        M[:, h, 1, :] = np.where(d1 >= 0, np.exp(-slopes[h] * d1), 0.0)
        d0 = q + 128.0 - k
        M[:, h, 0, :] = np.where(d0 < 128, np.exp(-slopes[h] * d0), 0.0)
    M2 = M.reshape(128, 8, 2, 2, 128).transpose(0, 2, 1, 3, 4)
    c["swa_mask"] = np.ascontiguousarray(M2).astype(f).astype(ml_dtypes.bfloat16).reshape(128, Hq * 2 * 128)
    return c


RET_G128 = [float(np.exp(128.0 * np.log1p(-np.exp2(-5.0 - h)))) for h in range(4)]


def col(v):
    return np.ascontiguousarray(np.asarray(v, dtype=np.float32).reshape(8, 128).T)


def build(n_seq=2, n_mt=4, n_layers=2, dbg=None):
    nc = bass.Bass("TRN2", target_bir_lowering=False)
    st = contextlib.ExitStack()

    def dram(name, shape, dt=F32, kind="ExternalInput"):
        return nc.dram_tensor(name, list(shape), dt, kind=kind).ap()

    x_d = dram("x", [n_seq, SEQ, D])
    out_d = dram("out", [n_seq, SEQ, D], kind="ExternalOutput")
    win_d = dram("w_in", [L, D, DIN])
    wbr_d = dram("w_branch", [L, 3, D, D])
    wout_d = dram("w_out", [L, D, D])
    wg_d = dram("ffn_w_gate", [L, D, DFF])
    wu_d = dram("ffn_w_up", [L, D, DFF])
    wd_d = dram("ffn_w_down", [L, DFF, D])
    par_d = dram("par", [L, 128, NPAR])
    w2_d = dram("w2aug", [L, 17, 512])
    wscr_d = dram("wscr", [128, L * WPL], BF16, kind="Internal")
    cd = {}
    for name, shp, dt in (("ident_f", [128, 128], F32), ("ident_b", [128, 128], BF16), ("ones_b", [128, 128], BF16),
                          ("ret_dt", [128, 512], F32), ("ret_xi", [128, 512], F32), ("ret_zt", [128, 512], F32),
                          ("gla_tri", [128, 128], F32), ("gla_sl", [128, 128], F32),
                          ("swa_mask", [128, 4096], BF16)):
        cd[name] = dram(name, shp, dt)
    if dbg:
        dbg_d = dram("dbg", [128, 8 * T], kind="ExternalOutput")

    def sb(name, shape, dt):
        return st.enter_context(nc.sbuf_tensor(name, list(shape), dt))

    S = Sched(nc)
    bufs = {}

    def B(*key):
        b = bufs.get(key)
        if b is None:
            b = bufs[key] = Buf(str(key))
        return b

    xT = sb("xT", [128, 8, T], F32)
    hT = sb("hT", [128, 8, T], BF16)
    sqr = sb("sqr", [128, 2, T], BF16)
    rstd2 = sb("rstd2", [128, 2, T], F32)
    tmp2 = sb("tmp2", [128, 2, T], F32)
    rstd = rstd2[:, 0, :]
    tmpf = tmp2[:, 0, :]
    NST, NBF = 2, 3
    wst = [sb("wst%d" % i, [128, 2048], F32) for i in range(NST)]
    wbf = [sb("wbf%d" % i, [128, 2048], BF16) for i in range(NBF)]
    merged = sb("merged", [128, 8, T], BF16)
    qT = sb("qT", [128, 4, T], BF16)
    kT = sb("kT", [128, 4, T], BF16)
    kzT = sb("kzT", [128, 4, T], BF16)
    vtok = sb("vtok", [128, NT, 1024], BF16)
    sgr = sb("sgr", [128, 2, T], BF16)
    sgall = sb("sgall", [128, 8, T], BF16)
    osb = sb("osb", [128, 8, T], F32)
    obf = sb("obf", [128, 8, T], BF16)
    AT = [sb("AT%d" % i, [128, 512], BF16) for i in range(2)]
    ktok = [sb("ktok%d" % i, [128, 512], BF16) for i in range(2)]
    retS = [sb("retS%d" % l, [128, 4, 256], F32) for l in range(L)]
    retSb = [sb("retSb%d" % l, [128, 4, 256], BF16) for l in range(L)]
    glaS = [sb("glaS%d" % l, [128, 4, 256], F32) for l in range(L)]
    glaSb = [sb("glaSb%d" % l, [128, 4, 256], BF16) for l in range(L)]
    gE = [sb("gE%d" % i, [128, T], F32) for i in range(2)]
    gEi = [sb("gEi%d" % i, [128, T], F32) for i in range(2)]
    gEs = [sb("gEs%d" % i, [128, T], F32) for i in range(2)]
    gaT = sb("gaT", [32, T], BF16)
    w2f = [sb("w2f%d" % l, [17, 512], F32) for l in range(L)]
    w2b = [sb("w2b%d" % l, [17, 512], BF16) for l in range(L)]
    KK = sb("KK", [128, 4, 128 + T], BF16)
    svt = sb("svt", [128, NT + 1, 256], BF16)
    ETb = [sb("ET%d" % i, [128, 512], BF16) for i in range(2)]
    PTs = sb("PTs", [128, 2, 4, 2, 128], BF16)
    den = sb("den", [128, 4, 128], F32)
    kkprev = [sb("kkprev%d" % l, [128, 4, 128], BF16) for l in range(L)]
    svprev = [sb("svprev%d" % l, [128, 256], BF16) for l in range(L)]
    par = [sb("par%d" % l, [128, NPAR], F32) for l in range(L)]
    es = [sb("es%d" % l, [128, 8], F32) for l in range(L)]
    ct = {}
    for name, ap in cd.items():
        ct[name] = sb("c_" + name, list(ap.shape), ap.dtype)
    onesb = sb("onesb32", [32, T], BF16)

    NPS = 7
    pst = [st.enter_context(nc.psum_tensor("ps%d" % i, [128, 512], F32)) for i in range(NPS)]
    ptr = st.enter_context(nc.psum_tensor("ptr", [128, 1024], BF16))
    psbuf = [Buf("ps%d" % i) for i in range(NPS)]
    for i in range(NPS):
        S.psgen[i] = 0
    psrr = [0]

    def ps_alloc():
        i = psrr[0] % NPS
        psrr[0] += 1
        S.psgen[i] += 1
        return PsH(i, S.psgen[i], pst[i], psbuf[i])

    def mm(out, lhsT, rhs, start, stop, reads, writes):
        S.op(PE, lambda e: e.matmul(out, lhsT=lhsT, rhs=rhs, start=start, stop=stop), reads=reads, writes=writes)

    def tr(out, in_, ident, reads, writes):
        S.op(PE, lambda e: e.transpose(out=out, in_=in_, identity=ident), reads=reads, writes=writes)

    def act(out, in_, func, reads, writes, **kw):
        S.op(ACT, lambda e: e.activation(out=out, in_=in_, func=func, **kw), reads=reads, writes=writes)

    def tt(out, in0, in1, op, reads, writes, eng=DVE):
        S.op(eng, lambda e: e.tensor_tensor(out=out, in0=in0, in1=in1, op=op), reads=reads, writes=writes)

    def ts(out, in0, s1, s2, op0, op1, reads, writes, eng=DVE):
        if s2 is None:
            S.op(eng, lambda e: e.tensor_scalar(out=out, in0=in0, scalar1=s1, scalar2=None, op0=op0),
                 reads=reads, writes=writes)
        else:
            S.op(eng, lambda e: e.tensor_scalar(out=out, in0=in0, scalar1=s1, scalar2=s2, op0=op0, op1=op1),
                 reads=reads, writes=writes)

    def stt(out, in0, scalar, in1, op0, op1, reads, writes):
        S.op(DVE, lambda e: e.scalar_tensor_tensor(out=out, in0=in0, scalar=scalar, in1=in1, op0=op0, op1=op1),
             reads=reads, writes=writes)

    def vcopy(out, in_, reads, writes):
        S.op(DVE, lambda e: e.tensor_copy(out=out, in_=in_), reads=reads, writes=writes)

    def recip(out, in_, reads, writes):
        S.op(DVE, lambda e: e.reciprocal(out=out, in_=in_), reads=reads, writes=writes)

    dsem_c = S.new_dma_sem("dc")
    dsem_x = S.new_dma_sem("dx")
    dsem_xs = [S.new_dma_sem("dx0"), S.new_dma_sem("dx1")]
    dsem_o = S.new_dma_sem("do")

    def dma(out, in_, reads, writes, sem, eng=SP):
        return S.op(eng, lambda e: e.dma_start(out=out, in_=in_), reads=reads, writes=writes, dma_sem=sem)

    for name in cd:
        dma(ct[name][:], cd[name], [], [B("c", name)], S.new_dma_sem("dc_" + name))
    for l in range(n_layers):
        dma(par[l][:], par_d[l], [], [B("par", l)], S.new_dma_sem("dc_par%d" % l))
        dma(w2f[l][:], w2_d[l], [], [B("w2f", l)], S.new_dma_sem("dc_w2%d" % l))
        vcopy(w2b[l][:], w2f[l][:], [B("w2f", l)], [B("w2b", l)])
        act(es[l][:], par[l][:, P_SINK:P_SINK + 8], AF.Exp, [B("par", l)], [B("es", l)])
    S.op(DVE, lambda e: e.memset(onesb[:], 1.0), writes=[B("onesb")])
    identf, identb, ones = ct["ident_f"], ct["ident_b"], ct["ones_b"]
    Bif, Bib, Bones = B("c", "ident_f"), B("c", "ident_b"), B("c", "ones_b")

    wsem = [S.new_dma_sem("dw%d" % i) for i in range(NST)]
    NRING = NBF + 2 * NST
    rsem = [S.new_dma_sem("dr%d" % i) for i in range(NRING)]
    dws = S.new_dma_sem("dws")
    wlist = []
    wstate = {"dma": 0, "cast": 0, "i0": None, "nconv": 0}
    wst_bf = [w_[:, :].bitcast(BF16) for w_ in wst]

    def ring_slot(r):
        if r < NBF:
            return wbf[r][:, :], [B("wbf", r)]
        s_, h_ = (r - NBF) // 2, (r - NBF) % 2
        return wst_bf[s_][:, h_ * 2048:(h_ + 1) * 2048], [B("wsth", s_, h_)]

    def wst_bufs(slot):
        return [B("wsth", slot, 0), B("wsth", slot, 1)]

    def w_issue_dma(i):
        view, KC, ncols, off, conv = wlist[i]
        n = KC * ncols
        if conv:
            slot = i % NST
            dst = wst[slot][:, 0:n].rearrange("p (k n) -> p k n", k=KC)
            dma(dst, view, [], wst_bufs(slot), wsem[slot])
        else:
            if wstate["i0"] is None:
                wstate["i0"] = i
            r = (i - wstate["i0"]) % NRING
            tl, bb = ring_slot(r)
            dma(tl[:, 0:n], wscr_d[:, off:off + n], [B("wscr_all")], bb, rsem[r])

    def w_issue_cast(i):
        view, KC, ncols, off, conv = wlist[i]
        if not conv:
            return
        slot, bslot = i % NST, i % NBF
        n = KC * ncols
        if i % 5 in (0, 2):
            vcopy(wbf[bslot][:, 0:n], wst[slot][:, 0:n], wst_bufs(slot), [B("wbf", bslot)])
        else:
            act(wbf[bslot][:, 0:n], wst[slot][:, 0:n], AF.Copy, wst_bufs(slot), [B("wbf", bslot)])
        dma(wscr_d[:, off:off + n], wbf[bslot][:, 0:n], [B("wbf", bslot)], [B("wscr_all")], dws, eng=POOL)

    def wget(i):
        assert i == wstate.get("last", -1) + 1, ("weight groups must be consumed in list order", i, wstate.get("last"))
        wstate["last"] = i
        nconv = wstate.get("n_conv")
        if nconv is None:
            nconv = wstate["n_conv"] = sum(1 for w_ in wlist if w_[4])

        def dma_ok(j):
            if wlist[j][4]:
                return j < i + NST
            return i >= nconv and j < i + NRING - 1
        while wstate["cast"] < min(len(wlist), i + 2):
            while wstate["dma"] <= wstate["cast"] and wlist[wstate["dma"]][4]:
                w_issue_dma(wstate["dma"])
                wstate["dma"] += 1
            w_issue_cast(wstate["cast"])
            wstate["cast"] += 1
        while wstate["dma"] < len(wlist) and dma_ok(wstate["dma"]):
            w_issue_dma(wstate["dma"])
            wstate["dma"] += 1
        assert wstate["dma"] > i
        view, KC, ncols, off, conv = wlist[i]
        n = KC * ncols
        if conv:
            return wbf[i % NBF][:, 0:n].rearrange("p (k n) -> p k n", k=KC), B("wbf", i % NBF)
        tl, bb = ring_slot((i - wstate["i0"]) % NRING)
        return tl[:, 0:n].rearrange("p (k n) -> p k n", k=KC), bb[0]

    scr_off = {}

    def layer_groups(l, conv):
        g = {}
        win = win_d[l].rearrange("(kc p) n -> p kc n", p=128)
        cursor = [l * WPL]

        def add(name, view, KC, ncols):
            g[name] = len(wlist)
            wlist.append((view, KC, ncols, cursor[0], conv))
            cursor[0] += KC * ncols
            assert cursor[0] <= (l + 1) * WPL

        def add_in(name, c0, n):
            for i in range(0, n, 256):
                w = min(256, n - i)
                add((name, i // 256), win[:, :, c0 + i:c0 + i + w], 8, w)
        add_in("rq", C_RQ, 512); add_in("rk", C_RK, 512); add_in("rv", C_RV, 1024); add_in("rg", C_RG, 1024)
        for n in range(3):
            if n == 1:
                add_in("ga", C_GA, 16); add_in("gv", C_GV, 1024)
                for i_ in range(2):
                    add(("gq", i_), win[:, :, C_GQ + i_ * 256:C_GQ + (i_ + 1) * 256], 8, 256)
                    add(("gk", i_), win[:, :, C_GK + i_ * 256:C_GK + (i_ + 1) * 256], 8, 256)
                add_in("gg", C_GG, 1024)
            if n == 2:
                add_in("sk", C_SK, 256); add_in("sv", C_SV, 256); add_in("sq", C_SQ, 1024)
            wb = wbr_d[l, n].rearrange("(kc p) n -> p kc n", p=128)
            for i in range(4):
                add(("gate", n, i), win[:, :, C_GATE + n * 1024 + i * 256:C_GATE + n * 1024 + (i + 1) * 256], 8, 256)
            for i in range(4):
                add(("br", n, i), wb[:, :, i * 256:(i + 1) * 256], 8, 256)
        wo = wout_d[l].rearrange("(kc p) n -> p kc n", p=128)
        for i in range(4):
            add(("wo", i), wo[:, :, i * 256:(i + 1) * 256], 8, 256)
        wgv = wg_d[l].rearrange("(kc p) n -> p kc n", p=128)
        wuv = wu_d[l].rearrange("(kc p) n -> p kc n", p=128)
        wdv = wd_d[l].rearrange("(kc p) n -> p kc n", p=128)
        for half in range(2):
            for i in range(6):
                w = 256 if i < 5 else 128
                c0 = half * 1408 + i * 256
                add(("fg", half, i), wgv[:, :, c0:c0 + w], 8, w)
                add(("fu", half, i), wuv[:, :, c0:c0 + w], 8, w)
            for c in range(8):
                add(("fd", half, c), wdv[:, half * 11:(half + 1) * 11, c * 128:(c + 1) * 128], 11, 128)
        return g

    def proj_fm(gidx, ncols_chunks, evac, src=None, srcB=None):
        if src is None:
            src, srcB = hT, [B("hT", c) for c in range(8)]
        wv, wb = wget(gidx)
        KC = wv.shape[1]
        ncols = wv.shape[2]
        j = 0
        c0 = 0
        while c0 < ncols:
            w = min(128, ncols - c0)
            p = ps_alloc()
            for kc in range(KC):
                if os.environ.get("KDBG_NOMM"):
                    continue
                mm(p.t[0:w, :], wv[:, kc, c0:c0 + w], src[:, kc, :], kc == 0, kc == KC - 1,
                   [wb, srcB[kc]], [p])
            if not os.environ.get("KDBG_NOEVAC"):
                evac(j, p, w)
            j += 1
            c0 += w

    def proj_tm(gidx, evac):
        wv, wb = wget(gidx)
        ncols = wv.shape[2]
        for t in range(NT):
            p = ps_alloc()
            for kc in range(8):
                mm(p.t[:, 0:ncols], hT[:, kc, t * 128:(t + 1) * 128], wv[:, kc, :], kc == 0, kc == 7,
                   [wb, B("hT", kc)], [p])
            evac(t, p, ncols)

    sq_rr = [0]

    def rmsnorm_stats(srcT, srcB, width_scale, eps):
        p = ps_alloc()
        for c in range(8):
            s = sq_rr[0] % 2
            sq_rr[0] += 1
            act(sqr[:, s, :], srcT[:, c, :], AF.Square, [srcB[c]], [B("sqr", s)])
            mm(p.t[:, :], ones[:, :], sqr[:, s, :], c == 0, c == 7, [Bones, B("sqr", s)], [p])
        act(tmpf[:], p.t[:, :], AF.Ln, [p, Beps(eps)], [B("tmpf")], scale=width_scale, bias=eps_ap(eps))
        act(rstd[:], tmpf[:], AF.Exp, [B("tmpf")], [B("rstd")], scale=-0.5)

    eps_tiles = {}

    def eps_ap(v):
        if v not in eps_tiles:
            t_ = sb("eps%d" % len(eps_tiles), [128, 1], F32)
            S.op(DVE, lambda e: e.memset(t_[:], float(v)), writes=[B("eps", v)])
            eps_tiles[v] = t_
        return eps_tiles[v][:, 0:1]

    for v_ in (1e-6, 1e-5, 1.0):
        eps_ap(v_)

    def Beps(v):
        return B("eps", v)

    def pre_norm(l, poff):
        rmsnorm_stats(xT, [B("xT", c) for c in range(8)], 1.0 / D, 1e-6)
        for c in range(8):
            stt(hT[:, c, :], xT[:, c, :], par[l][:, poff + c:poff + c + 1], rstd[:], ALU.mult, ALU.mult,
                [B("xT", c), B("par", l), B("rstd")], [B("hT", c)])

    def post_norm_residual(l, poff):
        rmsnorm_stats(osb, [B("osb", c) for c in range(8)], 1.0 / D, 1e-6)
        for c in range(8):
            stt(osb[:, c, :], osb[:, c, :], par[l][:, poff + c:poff + c + 1], rstd[:], ALU.mult, ALU.mult,
                [B("osb", c), B("par", l), B("rstd")], [B("osb", c)])
            tt(xT[:, c, :], xT[:, c, :], osb[:, c, :], ALU.add, [B("xT", c), B("osb", c)], [B("xT", c)])

    _act = act

    def ret_branch(l, g, first_mt):
        DTt, XIt, ZTt = ct["ret_dt"], ct["ret_xi"], ct["ret_zt"]
        XIb = XIt[:, :].rearrange("p (h i) -> p h i", h=4)
        ZTb = ZTt[:, :].rearrange("p (h i) -> p h i", h=4)

        def ev_q(gi):
            def f(j, p, w):
                h = gi * 2 + j
                for t_ in range(NT):
                    tt(qT[:, h, t_ * 128:(t_ + 1) * 128], p.t[:, t_ * 128:(t_ + 1) * 128], XIt[:, h * 128:(h + 1) * 128], ALU.mult,
                       [p, B("c", "ret_xi")], [B("qT", h)])
            return f

        def ev_k(gi):
            def f(j, p, w):
                h = gi * 2 + j
                act(kT[:, h, :], p.t[:, :], AF.Copy, [p], [B("kT", h)])
                for t_ in range(NT):
                    tt(kzT[:, h, t_ * 128:(t_ + 1) * 128], p.t[:, t_ * 128:(t_ + 1) * 128], ZTt[:, h * 128:(h + 1) * 128], ALU.mult,
                       [p, B("c", "ret_zt")], [B("kzT", h)])
            return f

        def ev_q2(gi):
            def f(j, p, w):
                h = gi * 2 + j
                act(obf[:, h, :], p.t[:, :], AF.Copy, [p], [B("obf", h)])
                ev_q(gi)(j, p, w)
            return f
        for gi in range(2):
            proj_fm(g[("rq", gi)], 2, ev_q2(gi))
        for gi in range(2):
            proj_fm(g[("rk", gi)], 2, ev_k(gi))

        dbg_dump("r1", xT, [B("xT", c_) for c_ in range(8)])

        def ev_v(gi):
            def f(t, p, n):
                act(vtok[:, t, gi * 256:(gi + 1) * 256], p.t[:, 0:256], AF.Copy, [p], [B("vtok", t)])
            return f
        for gi in range(4):
            proj_tm(g[("rv", gi)], ev_v(gi))
        dbg_dump("r2", xT, [B("xT", c_) for c_ in range(8)])
        if first_mt:
            S.op(DVE, lambda e: e.memset(retS[l][:], 0.0), writes=[B("retS", l)])
            S.op(DVE, lambda e: e.memset(retSb[l][:], 0.0), writes=[B("retSb", l)])
        def ret_stage_a(t):
            S.phase = 'ret_a'
            tok = slice(t * 128, (t + 1) * 128)
            a = t % 2
            p = ps_alloc()
            for h in range(4):
                mm(p.t[:, h * 128:(h + 1) * 128], kT[:, h, tok], obf[:, h, tok], True, True,
                   [B("kT", h), B("obf", h)], [p])
            tt(AT[a][:], p.t[:, :], DTt[:, :], ALU.mult, [p, B("c", "ret_dt")], [B("AT", a)])
            dbg_dump("r3", xT, [B("xT", c_) for c_ in range(8)])
            for h in range(4):
                tr(ptr[:, h * 128:(h + 1) * 128], kzT[:, h, tok], identb[:], [B("kzT", h), Bib], [B("ptr", 0)])
            act(ktok[a][:], ptr[:, 0:512], AF.Copy, [B("ptr", 0)], [B("ktok", a)])
            dbg_dump("r4", xT, [B("xT", c_) for c_ in range(8)])

        def ret_stage_b(t):
            S.phase = 'ret_b'
            tok = slice(t * 128, (t + 1) * 128)
            a = t % 2
            for hh in range(2):
                p = ps_alloc()
                for h2 in range(2):
                    h = hh * 2 + h2
                    for ec in range(2):
                        o_ = p.t[:, (h2 * 2 + ec) * 128:(h2 * 2 + ec + 1) * 128]
                        mm(o_, vtok[:, t, h * 256 + ec * 128:h * 256 + (ec + 1) * 128], AT[a][:, h * 128:(h + 1) * 128],
                           True, False, [B("vtok", t), B("AT", a)], [p])
                        mm(o_, retSb[l][:, h, ec * 128:(ec + 1) * 128], qT[:, h, tok], False, True,
                           [B("retSb", l), B("qT", h)], [p])
                act(osb[:, hh * 4:(hh + 1) * 4, tok], p.t[:, :].rearrange("p (c i) -> p c i", c=4), AF.Copy,
                    [p], [B("osb", c) for c in range(hh * 4, hh * 4 + 4)])
            dbg_dump("r5", xT, [B("xT", c_) for c_ in range(8)])
            for hh in range(2):
                p = ps_alloc()
                for h2 in range(2):
                    h = hh * 2 + h2
                    mm(p.t[:, h2 * 256:(h2 + 1) * 256], ktok[a][:, h * 128:(h + 1) * 128], vtok[:, t, h * 256:(h + 1) * 256],
                       True, True, [B("ktok", a), B("vtok", t)], [p])
                for h2 in range(2):
                    h = hh * 2 + h2
                    stt(retS[l][:, h, :], retS[l][:, h, :], RET_G128[h], p.t[:, h2 * 256:(h2 + 1) * 256], ALU.mult, ALU.add,
                        [B("retS", l), p], [B("retS", l)])
                act(retSb[l][:, hh * 2:hh * 2 + 2, :], retS[l][:, hh * 2:hh * 2 + 2, :], AF.Copy,
                    [B("retS", l)], [B("retSb", l)])

        ret_stage_a(0)
        for t in range(NT):
            if t + 1 < NT:
                ret_stage_a(t + 1)
            ret_stage_b(t)
        dbg_dump("r6", xT, [B("xT", c_) for c_ in range(8)])
        def ev_gs(gi):
            def f(j, p, w):
                c = gi * 2 + j
                act(sgall[:, c, :], p.t[:, :], AF.Silu, [p], [B("sgall", c)])
            return f

        def gn_pair(hp):
            S.phase = 'ret_gn'
            pms, pqs = [], []
            for h2 in range(2):
                h = hp * 2 + h2
                pm = ps_alloc()
                pq = ps_alloc()
                for ec in range(2):
                    c = h * 2 + ec
                    s = sq_rr[0] % 2
                    sq_rr[0] += 1
                    act(sqr[:, s, :], osb[:, c, :], AF.Copy, [B("osb", c)], [B("sqr", s)])
                    mm(pm.t[:, :], ones[:, :], sqr[:, s, :], ec == 0, ec == 1, [Bones, B("sqr", s)], [pm])
                    s = sq_rr[0] % 2
                    sq_rr[0] += 1
                    act(sqr[:, s, :], osb[:, c, :], AF.Square, [B("osb", c)], [B("sqr", s)])
                    mm(pq.t[:, :], ones[:, :], sqr[:, s, :], ec == 0, ec == 1, [Bones, B("sqr", s)], [pq])
                pms.append(pm)
                pqs.append(pq)
            TB = [B("tmpf"), B("tmpfB")]
            RB = [B("rstd"), B("rstdB")]
            for h2 in range(2):
                act(tmp2[:, h2, :], pms[h2].t[:, :], AF.Square, [pms[h2]], [TB[h2]], scale=1.0 / 256)
                stt(tmp2[:, h2, :], pqs[h2].t[:, :], 1.0 / 256, tmp2[:, h2, :], ALU.mult, ALU.subtract,
                    [pqs[h2], TB[h2]], [TB[h2]])
            act(tmp2[:, :, :], tmp2[:, :, :], AF.Ln, TB + [Beps(1e-5)], TB, bias=eps_ap(1e-5))
            act(rstd2[:, :, :], tmp2[:, :, :], AF.Exp, TB, RB, scale=-0.5)
            for h2 in range(2):
                stt(tmp2[:, h2, :], pms[h2].t[:, :], -1.0 / 256, rstd2[:, h2, :], ALU.mult, ALU.mult,
                    [pms[h2], RB[h2]], [TB[h2]])
            cs = list(range(hp * 4, hp * 4 + 4))
            OB = [B("osb", c) for c in cs]
            osb4 = osb[:, hp * 4:hp * 4 + 4, :].rearrange("p (h e) t -> p h e t", h=2)
            tt(osb4, osb4, rstd2[:, :, :].unsqueeze(2).to_broadcast([128, 2, 2, T]), ALU.mult, OB + RB, OB)
            tt(osb4, osb4, tmp2[:, :, :].unsqueeze(2).to_broadcast([128, 2, 2, T]), ALU.add, OB + TB, OB)
            for c in cs:
                act(osb[:, c, :], osb[:, c, :], AF.Identity, [B("osb", c), B("par", l)], [B("osb", c)],
                    scale=par[l][:, P_RETW + c:P_RETW + c + 1], bias=par[l][:, P_RETB + c:P_RETB + c + 1])
            tt(obf[:, hp * 4:hp * 4 + 4, :], osb[:, hp * 4:hp * 4 + 4, :], sgall[:, hp * 4:hp * 4 + 4, :], ALU.mult,
               OB + [B("sgall", c) for c in cs], [B("obf", c) for c in cs])

        S.phase = 'ret_rg'
        for gi in range(4):
            proj_fm(g[("rg", gi)], 2, ev_gs(gi))
        gn_pair(0)
        gn_pair(1)

    def gla_branch2(l, g, first_mt):
        TRI, SLm = ct["gla_tri"], ct["gla_sl"]
        vcopy(gaT[:, :], onesb[:, :], [B("onesb")], [B("gaT")])

        def ev_ga(j, p, w):
            act(gaT[0:16, :], p.t[0:16, :], AF.Copy, [p], [B("gaT")])
        proj_fm(g[("ga", 0)], 1, ev_ga)

        def ev_v(gi):
            def f(t, p, n):
                act(vtok[:, t, gi * 256:(gi + 1) * 256], p.t[:, 0:256], AF.Copy, [p], [B("vtok", t)])
            return f
        for gi in range(4):
            proj_tm(g[("gv", gi)], ev_v(gi))
        if first_mt:
            S.op(DVE, lambda e: e.memset(glaS[l][:], 0.0), writes=[B("glaS", l, h_) for h_ in range(4)])
            S.op(DVE, lambda e: e.memset(glaSb[l][:], 0.0), writes=[B("glaSb", l, h_) for h_ in range(4)])
        for t in range(NT):
            pz = ps_alloc()
            mm(pz.t[:, :], gaT[0:17, t * 128:(t + 1) * 128], w2b[l][0:17, :], True, True,
               [B("gaT"), B("w2b", l)], [pz])
            act(osb[:, t, :], pz.t[:, :], AF.Exp, [pz], [B("osb", t)], scale=-1.0)
            act(osb[:, t, :], osb[:, t, :], AF.Ln, [B("osb", t), Beps(1.0)], [B("osb", t)], bias=eps_ap(1.0))
            ts(osb[:, t, :], osb[:, t, :], -1.0 / 16, -1.0, ALU.mult, ALU.max, [B("osb", t)], [B("osb", t)])
        for gi in range(2):
            for j in range(2):
                h = gi * 2 + j
                pb_ = ps_alloc()
                pl_ = ps_alloc()
                for t in range(NT):
                    mm(pb_.t[:, t * 128:(t + 1) * 128], osb[:, t, h * 128:(h + 1) * 128], TRI[:, :], True, True,
                       [B("osb", t), B("c", "gla_tri")], [pb_])
                    mm(pl_.t[:, t * 128:(t + 1) * 128], osb[:, t, h * 128:(h + 1) * 128], SLm[:, :], True, True,
                       [B("osb", t), B("c", "gla_sl")], [pl_])
                act(gE[j][:], pb_.t[:, :], AF.Exp, [pb_], [B("gE", j)])
                act(gEi[j][:], pb_.t[:, :], AF.Exp, [pb_], [B("gEi", j)], scale=-1.0)
                act(gEs[j][:], pl_.t[:, :], AF.Exp, [pl_], [B("gEs", j)])
                vcopy(gdec[:, h, :], gE[j][:, :].rearrange("p (b i) -> p b i", i=64)[:, :, 63], [B("gE", j)], [B("gdec")])
            wv, wb = wget(g[("gq", gi)])
            for j in range(2):
                h = gi * 2 + j
                p = ps_alloc()
                for kc in range(8):
                    mm(p.t[:, :], wv[:, kc, j * 128:(j + 1) * 128], hT[:, kc, :], kc == 0, kc == 7, [wb, B("hT", kc)], [p])
                stt(qT[:, h, :], p.t[:, :], 128.0 ** -0.5, gE[j][:], ALU.mult, ALU.mult, [p, B("gE", j)], [B("qT", h)])
            wv, wb = wget(g[("gk", gi)])
            for j in range(2):
                h = gi * 2 + j
                p = ps_alloc()
                for kc in range(8):
                    mm(p.t[:, :], wv[:, kc, j * 128:(j + 1) * 128], hT[:, kc, :], kc == 0, kc == 7, [wb, B("hT", kc)], [p])
                tt(kT[:, h, :], p.t[:, :], gEi[j][:], ALU.mult, [p, B("gEi", j)], [B("kT", h)])
                tt(kzT[:, h, :], p.t[:, :], gEs[j][:], ALU.mult, [p, B("gEs", j)], [B("kzT", h)])
        TRIb = TRI[:, :].unsqueeze(1).to_broadcast([128, 4, 128])

        def gla_stage_a(t):
            S.phase = 'gla_a'
            tok = slice(t * 128, (t + 1) * 128)
            a = t % 2
            p = ps_alloc()
            for h in range(4):
                mm(p.t[:, h * 128:(h + 1) * 128], kT[:, h, tok], qT[:, h, tok], True, True,
                   [B("kT", h), B("qT", h)], [p])
            tt(AT[a][:].rearrange("p (h i) -> p h i", h=4), p.t[:, :].rearrange("p (h i) -> p h i", h=4), TRIb, ALU.mult,
               [p, B("c", "gla_tri")], [B("AT", a)])
            for h in range(4):
                tr(ptr[:, h * 128:(h + 1) * 128], kzT[:, h, tok], identb[:], [B("kzT", h), Bib], [B("ptr", 0)])
            act(ktok[a][:], ptr[:, 0:512], AF.Copy, [B("ptr", 0)], [B("ktok", a)])

        def gla_stage_b(t):
            S.phase = 'gla_b'
            tok = slice(t * 128, (t + 1) * 128)
            a = t % 2
            po = [ps_alloc(), ps_alloc()]
            for blk in range(2):
                bt = slice(t * 128 + blk * 64, t * 128 + blk * 64 + 64)
                for h in range(4):
                    for ec in range(2):
                        base = ((h % 2) * 2 + ec) * 128 + blk * 64
                        o_ = po[h // 2].t[:, base:base + 64]
                        mm(o_, vtok[:, t, h * 256 + ec * 128:h * 256 + (ec + 1) * 128],
                           AT[a][:, h * 128 + blk * 64:h * 128 + blk * 64 + 64],
                           True, False, [B("vtok", t), B("AT", a)], [po[h // 2]])
                        mm(o_, glaSb[l][:, h, ec * 128:(ec + 1) * 128], qT[:, h, bt],
                           False, True, [B("glaSb", l, h), B("qT", h)], [po[h // 2]])
                for hh in range(2):
                    pk = ps_alloc()
                    for h2 in range(2):
                        h = hh * 2 + h2
                        mm(pk.t[:, h2 * 256:(h2 + 1) * 256], ktok[a][blk * 64:(blk + 1) * 64, h * 128:(h + 1) * 128],
                           vtok[blk * 64:(blk + 1) * 64, t, h * 256:(h + 1) * 256], True, True,
                           [B("ktok", a), B("vtok", t)], [pk])
                    for h2 in range(2):
                        h = hh * 2 + h2
                        stt(glaS[l][:, h, :], glaS[l][:, h, :], gdec[:, h, t * 2 + blk:t * 2 + blk + 1],
                            pk.t[:, h2 * 256:(h2 + 1) * 256], ALU.mult, ALU.add,
                            [B("glaS", l, h), B("gdec"), pk], [B("glaS", l, h)])
                        act(glaSb[l][:, h, :], glaS[l][:, h, :], AF.Copy, [B("glaS", l, h)], [B("glaSb", l, h)])
            for hh in range(2):
                act(obf_raw(hh, tok), po[hh].t[:, :].rearrange("p (c i) -> p c i", c=4), AF.Copy,
                    [po[hh]], [B("osb", c) for c in range(hh * 4, hh * 4 + 4)])

        gla_stage_a(0)
        for t in range(NT):
            if t + 1 < NT:
                gla_stage_a(t + 1)
            gla_stage_b(t)
        def ev_gs(gi):
            def f(j, p, w):
                c = gi * 2 + j
                act(sgall[:, c, :], p.t[:, :], AF.Silu, [p], [B("sgall", c)])
            return f

        def rms_head(h):
            S.phase = 'gla_rms'
            pq = ps_alloc()
            for ec in range(2):
                c = h * 2 + ec
                s = sq_rr[0] % 2
                sq_rr[0] += 1
                act(sqr[:, s, :], osb[:, c, :], AF.Square, [B("osb", c)], [B("sqr", s)])
                mm(pq.t[:, :], ones[:, :], sqr[:, s, :], ec == 0, ec == 1, [Bones, B("sqr", s)], [pq])
            act(tmpf[:], pq.t[:, :], AF.Ln, [pq, Beps(1e-6)], [B("tmpf")], scale=1.0 / 256, bias=eps_ap(1e-6))
            act(rstd[:], tmpf[:], AF.Exp, [B("tmpf")], [B("rstd")], scale=-0.5)
            for ec in range(2):
                c = h * 2 + ec
                stt(osb[:, c, :], osb[:, c, :], par[l][:, P_GLAW + c:P_GLAW + c + 1], rstd[:], ALU.mult, ALU.mult,
                    [B("osb", c), B("par", l), B("rstd")], [B("osb", c)])
                tt(obf[:, c, :], osb[:, c, :], sgall[:, c, :], ALU.mult, [B("osb", c), B("sgall", c)], [B("obf", c)])

        proj_fm(g[("gg", 0)], 2, ev_gs(0))
        for h in range(4):
            if h + 1 < 4:
                proj_fm(g[("gg", h + 1)], 2, ev_gs(h + 1))
            rms_head(h)

    gdec = sb("gdec", [128, 4, 2 * NT], F32)

    def obf_raw(hh, tok):
        return osb[:, hh * 4:(hh + 1) * 4, tok]

    def swa_branch(l, g, first_mt):
        MASK = ct["swa_mask"][:, :].rearrange("p (s c k q) -> p s c k q", s=2, c=8, k=2)
        wv, wb = wget(g[("sk", 0)])
        if first_mt:
            S.op(DVE, lambda e: e.memset(KK[:, :, 0:128], 0.0), writes=[B("KK", gg) for gg in range(4)])
            S.op(DVE, lambda e: e.memset(svt[:, 0, :], 0.0), writes=[B("svt", 0)])
        else:
            vcopy(KK[:, :, 0:128], kkprev[l][:, :, :], [B("kkprev", l)], [B("KK", gg) for gg in range(4)])
            vcopy(svt[:, 0, :], svprev[l][:, :], [B("svprev", l)], [B("svt", 0)])
        for gg in range(4):
            p = ps_alloc()
            for half in range(2):
                for kc in range(8):
                    mm(p.t[half * 64:(half + 1) * 64, :], wv[:, kc, gg * 64:(gg + 1) * 64], hT[:, kc, :], kc == 0, kc == 7,
                       [wb, B("hT", kc)], [p])
            act(KK[:, gg, 128:128 + T], p.t[:, :], AF.Copy, [p], [B("KK", gg)])
        wv, wb = wget(g[("sv", 0)])
        for t in range(NT):
            p = ps_alloc()
            for kc in range(8):
                mm(p.t[:, 0:256], hT[:, kc, t * 128:(t + 1) * 128], wv[:, kc, :], kc == 0, kc == 7, [wb, B("hT", kc)], [p])
            act(svt[:, t + 1, :], p.t[:, 0:256], AF.Copy, [p], [B("svt", t + 1)])
        def qdst(c):
            return (qT, "qT", c) if c < 4 else (kT, "kT", c - 4)

        def ev_q(gi):
            def f(j, p, w):
                c = gi * 2 + j
                tl, nm, ci = qdst(c)
                act(tl[:, ci, :], p.t[:, :], AF.Copy, [p], [B(nm, ci)])
            return f
        for gi in range(4):
            proj_fm(g[("sq", gi)], 2, ev_q(gi))
        vcopy(kkprev[l][:, :, :], KK[:, :, T:T + 128], [B("KK", gg) for gg in range(4)], [B("kkprev", l)])
        vcopy(svprev[l][:, :], svt[:, NT, :], [B("svt", NT)], [B("svprev", l)])
        esb = es[l]
        for t in range(NT):
            tok = slice(t * 128, (t + 1) * 128)
            kts = [1] if (first_mt and t == 0) else [0, 1]
            for half in range(2):
                for s in range(2):
                    for pair in range(2):
                        p = ps_alloc()
                        for ci2 in range(2):
                            cc = pair * 2 + ci2
                            c = half * 4 + cc
                            tl, nm, ci = qdst(c)
                            gkv = c // 2
                            for kt in kts:
                                kcols = slice(t * 128 + kt * 128, t * 128 + kt * 128 + 128)
                                mm(p.t[:, (ci2 * 2 + kt) * 128:(ci2 * 2 + kt + 1) * 128], KK[s * 64:(s + 1) * 64, gkv, kcols],
                                   tl[s * 64:(s + 1) * 64, ci, tok], True, True, [B("KK", gkv), B(nm, ci)], [p])
                        e_ = pair
                        c0 = half * 4 + pair * 2
                        if len(kts) == 2:
                            act(ETb[e_][:], p.t[:, :], AF.Exp, [p], [B("ET", e_)], scale=0.125)
                            tt(PTs[:, s, pair * 2:pair * 2 + 2, :, :], ETb[e_][:].rearrange("p (h k q) -> p h k q", h=2, k=2),
                               MASK[:, s, c0:c0 + 2, :, :], ALU.mult, [B("ET", e_), B("c", "swa_mask")], [B("PTs", s, pair)])
                        else:
                            for ci2 in range(2):
                                act(ETb[e_][:, ci2 * 256 + 128:ci2 * 256 + 256], p.t[:, ci2 * 256 + 128:ci2 * 256 + 256], AF.Exp,
                                    [p], [B("ET", e_)], scale=0.125)
                            tt(PTs[:, s, pair * 2:pair * 2 + 2, 1, :],
                               ETb[e_][:].rearrange("p (h k q) -> p h k q", h=2, k=2)[:, :, 1, :],
                               MASK[:, s, c0:c0 + 2, 1, :], ALU.mult, [B("ET", e_), B("c", "swa_mask")], [B("PTs", s, pair)])
                po = ps_alloc()
                pd = ps_alloc()
                for s in range(2):
                    for cc in range(4):
                        c = half * 4 + cc
                        gkv = c // 2
                        for i, kt in enumerate(kts):
                            mm(po.t[s * 64:(s + 1) * 64, cc * 128:(cc + 1) * 128], svt[:, t + kt, gkv * 64:(gkv + 1) * 64],
                               PTs[:, s, cc, kt, :], i == 0, i == len(kts) - 1, [B("svt", t + kt), B("PTs", s, cc // 2)], [po])
                        for i, kt in enumerate(kts):
                            mm(pd.t[s * 64:(s + 1) * 64, cc * 128:(cc + 1) * 128], ones[:, 0:64],
                               PTs[:, s, cc, kt, :], i == 0, i == len(kts) - 1, [Bones, B("PTs", s, cc // 2)], [pd])
                tt(den[:, :, :], pd.t[:, :].rearrange("p (c q) -> p c q", c=4),
                   esb[:, half * 4:(half + 1) * 4].unsqueeze(2).to_broadcast([128, 4, 128]), ALU.add,
                   [pd, B("es", l)], [B("den")])
                recip(den[:, :, :], den[:, :, :], [B("den")], [B("den")])
                tt(obf[:, half * 4:(half + 1) * 4, tok], po.t[:, :].rearrange("p (c q) -> p c q", c=4), den[:, :, :], ALU.mult,
                   [po, B("den")], [B("obf", c) for c in range(half * 4, half * 4 + 4)])

    def branch_merge(l, g, n):
        for i in range(4):
            wvg, wbg = wget(g[("gate", n, i)])
            for j in range(2):
                c = i * 2 + j
                p = ps_alloc()
                for kc in range(8):
                    mm(p.t[:, :], wvg[:, kc, j * 128:(j + 1) * 128], hT[:, kc, :], kc == 0, kc == 7, [wbg, B("hT", kc)], [p])
                act(sgall[:, c, :], p.t[:, :], AF.Sigmoid, [p], [B("sgall", c)])
        for i in range(4):
            wvb, wbb = wget(g[("br", n, i)])
            for j in range(2):
                c = i * 2 + j
                p = ps_alloc()
                for kc in range(8):
                    mm(p.t[:, :], wvb[:, kc, j * 128:(j + 1) * 128], obf[:, kc, :], kc == 0, kc == 7, [wbb, B("obf", kc)], [p])
                if n == 0:
                    tt(merged[:, c, :], p.t[:, :], sgall[:, c, :], ALU.mult, [p, B("sgall", c)], [B("merged", c)])
                else:
                    tt(tmpf[:], p.t[:, :], sgall[:, c, :], ALU.mult, [p, B("sgall", c)], [B("tmpf")])
                    tt(merged[:, c, :], merged[:, c, :], tmpf[:], ALU.add, [B("merged", c), B("tmpf")], [B("merged", c)])

    def out_proj(l, g):
        for i in range(4):
            wv, wb = wget(g[("wo", i)])
            for j in range(2):
                c = i * 2 + j
                p = ps_alloc()
                for kc in range(8):
                    mm(p.t[:, :], wv[:, kc, j * 128:(j + 1) * 128], merged[:, kc, :], kc == 0, kc == 7,
                       [wb, B("merged", kc)], [p])
                act(osb[:, c, :], p.t[:, :], AF.Copy, [p], [B("osb", c)])

    def fdst(j):
        return (obf, "obf", j) if j < 8 else (kzT, "kzT", j - 8)

    def ffn(l, g):
        for half in range(2):
            for i in range(6):
                wvg, wbg = wget(g[("fg", half, i)])
                nj = wvg.shape[2] // 128
                for jj in range(nj):
                    j = i * 2 + jj
                    p = ps_alloc()
                    for kc in range(8):
                        mm(p.t[:, :], wvg[:, kc, jj * 128:(jj + 1) * 128], hT[:, kc, :], kc == 0, kc == 7, [wbg, B("hT", kc)], [p])
                    s = j % 2
                    act(sgr[:, s, :], p.t[:, :], AF.Silu, [p], [B("sgr", s)])
                wvu, wbu = wget(g[("fu", half, i)])
                for jj in range(nj):
                    j = i * 2 + jj
                    s = j % 2
                    tl, nm, ci = fdst(j)
                    p = ps_alloc()
                    for kc in range(8):
                        mm(p.t[:, :], wvu[:, kc, jj * 128:(jj + 1) * 128], hT[:, kc, :], kc == 0, kc == 7, [wbu, B("hT", kc)], [p])
                    tt(tl[:, ci, :], p.t[:, :], sgr[:, s, :], ALU.mult, [p, B("sgr", s)], [B(nm, ci)])
            for c in range(8):
                wv, wb = wget(g[("fd", half, c)])
                p = ps_alloc()
                for j in range(11):
                    tl, nm, ci = fdst(j)
                    mm(p.t[:, :], wv[:, j, :], tl[:, ci, :], j == 0, j == 10, [wb, B(nm, ci)], [p])
                if half == 0:
                    act(osb[:, c, :], p.t[:, :], AF.Copy, [p], [B("osb", c)])
                else:
                    tt(osb[:, c, :], osb[:, c, :], p.t[:, :], ALU.add, [B("osb", c), p], [B("osb", c)])

    def f32_stage(tile_bf16):
        return tile_bf16[:, :, :].rearrange("p c t -> p (c t)").bitcast(F32).rearrange("p (t d) -> p t d", t=2)

    ld_stage = [(f32_stage(sgall), "sgall"), (f32_stage(merged), "merged")]
    st_stage = osb[:, :, :].rearrange("p c t -> p (c t)").rearrange("p (t d) -> p t d", t=4)

    def issue_x_load(sq, mt):
        for half, (stg, nm) in enumerate(ld_stage):
            r0 = mt * T + half * 256
            dma(stg, x_d[sq, r0:r0 + 256, :].rearrange("(t p) d -> p t d", p=128), [], [B(nm, c) for c in range(8)],
                dsem_xs[half], eng=SP)

    def load_x(sq, mt):
        for t in range(NT):
            stg, nm = ld_stage[t // 2]
            for hh in range(2):
                p = ps_alloc()
                for c4 in range(4):
                    c = hh * 4 + c4
                    tr(p.t[:, c4 * 128:(c4 + 1) * 128], stg[:, t % 2, c * 128:(c + 1) * 128], identf[:],
                       [B(nm, c_) for c_ in range(8)] + [Bif], [p])
                act(xT[:, hh * 4:(hh + 1) * 4, t * 128:(t + 1) * 128], p.t[:, :].rearrange("p (c i) -> p c i", c=4), AF.Copy,
                    [p], [B("xT", c) for c in range(hh * 4, hh * 4 + 4)])

    out_deps = []

    def store_x(sq, mt):
        for t in range(NT):
            for hh in range(2):
                p = ps_alloc()
                for c4 in range(4):
                    c = hh * 4 + c4
                    tr(p.t[:, c4 * 128:(c4 + 1) * 128], xT[:, c, t * 128:(t + 1) * 128], identf[:], [B("xT", c), Bif], [p])
                act(st_stage[:, t, hh * 512:(hh + 1) * 512], p.t[:, :], AF.Copy, [p], [B("osb", c) for c in range(8)])
        r0 = mt * T
        out_deps.append(dma(out_d[sq, r0:r0 + T, :].rearrange("(t p) d -> p t d", p=128), st_stage,
                            [B("osb", c) for c in range(8)], [], dsem_o, eng=POOL))

    class StopBuild(Exception):
        pass

    def dbg_dump(stage, tile, names):
        if not dbg or dbg.get("stage") != stage:
            return
        dsem_d = S.new_dma_sem("ddbg")
        if tile.dtype == BF16:
            dbgt = sb("dbgt", [128, 4, T], F32)
            for hf in range(2):
                vcopy(dbgt[:], tile[:, hf * 4:(hf + 1) * 4, :], names, [B("dbgt")])
                dep = dma(dbg_d[:, hf * 4 * T:(hf + 1) * 4 * T], dbgt[:].rearrange("p c t -> p (c t)"), [B("dbgt")], [], dsem_d)
        else:
            dep = dma(dbg_d, tile[:].rearrange("p c t -> p (c t)"), names, [], dsem_d)
        out_deps.append(dep)
        raise StopBuild()

    groups = {}
    plan = []
    for sq in range(n_seq):
        for mt in range(n_mt):
            for l in range(n_layers):
                groups[(sq, mt, l)] = layer_groups(l, conv=(sq == 0 and mt == 0))
    only = (dbg or {}).get("only")
    c8 = list(range(8))
    try:
      order = [(sq_, mt_) for sq_ in range(n_seq) for mt_ in range(n_mt)]
      issue_x_load(*order[0])
      for oi, (sq, mt) in enumerate(order):
        if True:
            S.phase = 'load_x'
            load_x(sq, mt)
            dbg_dump("x", xT, [B("xT", c) for c in c8])
            for l in range(n_layers):
                g = groups[(sq, mt, l)]
                first = (mt == 0)
                S.phase = 'pre_norm_P_MIXPRE'
                pre_norm(l, P_MIXPRE)
                dbg_dump("h", hT, [B("hT", c) for c in c8])
                S.phase = 'ret_branch'
                ret_branch(l, g, first)
                dbg_dump("o_ret", obf, [B("obf", c) for c in c8])
                S.phase = 'branch_merge_0'
                branch_merge(l, g, 0)
                S.phase = 'gla_branch2'
                gla_branch2(l, g, first)
                dbg_dump("o_gla", obf, [B("obf", c) for c in c8])
                S.phase = 'branch_merge_1'
                branch_merge(l, g, 1)
                S.phase = 'swa_branch'
                swa_branch(l, g, first)
                dbg_dump("o_swa", obf, [B("obf", c) for c in c8])
                S.phase = 'branch_merge_2'
                branch_merge(l, g, 2)
                dbg_dump("merged", merged, [B("merged", c) for c in c8])
                S.phase = 'out_proj'
                out_proj(l, g)
                S.phase = 'post_norm_residual_P_MIXPOST'
                post_norm_residual(l, P_MIXPOST)
                dbg_dump("x1", xT, [B("xT", c) for c in c8])
                S.phase = 'pre_norm_P_FFNPRE'
                pre_norm(l, P_FFNPRE)
                if l == n_layers - 1 and oi + 1 < len(order):
                    issue_x_load(*order[oi + 1])
                S.phase = 'ffn'
                ffn(l, g)
                S.phase = 'post_norm_residual_P_FFNPOST'
                post_norm_residual(l, P_FFNPOST)
            S.phase = 'store_x'
            store_x(sq, mt)
    except StopBuild:
        pass
    S.final_wait(POOL, out_deps)
    S.emit()
    st.close()
    return nc, S


def host_inputs(inputs):
    f = np.float32
    shared = dict(make_consts())
    for k in ("w_in", "w_branch", "w_out", "ffn_w_gate", "ffn_w_up", "ffn_w_down"):
        shared[k] = np.ascontiguousarray(np.asarray(inputs[k], dtype=f))
    par = np.zeros((L, 128, NPAR), dtype=f)
    for l in range(L):
        par[l, :, P_MIXPRE:P_MIXPRE + 8] = col(inputs["norm_mix_pre"][l])
        par[l, :, P_MIXPOST:P_MIXPOST + 8] = col(inputs["norm_mix_post"][l])
        par[l, :, P_FFNPRE:P_FFNPRE + 8] = col(inputs["norm_ffn_pre"][l])
        par[l, :, P_FFNPOST:P_FFNPOST + 8] = col(inputs["norm_ffn_post"][l])
        par[l, :, P_RETW:P_RETW + 8] = col(inputs["ret_norm_w"][l])
        par[l, :, P_RETB:P_RETB + 8] = col(inputs["ret_norm_b"][l])
        par[l, :, P_GLAW:P_GLAW + 8] = col(inputs["gla_norm_w"][l])
        sk = np.asarray(inputs["attn_sinks"][l], dtype=f)
        par[l, :, P_SINK:P_SINK + 8] = sk.reshape(8, 2).T[np.arange(128) // 64, :]
    shared["par"] = par
    w2 = np.concatenate([np.asarray(inputs["gla_w_alpha2"], dtype=f),
                         np.asarray(inputs["gla_b_alpha"], dtype=f)[:, None, :]], axis=1)
    shared["w2aug"] = np.ascontiguousarray(w2)
    return shared


_CACHE = {}


def kernel(**inputs):
    x = np.asarray(inputs["x"], dtype=np.float32)
    n = 8
    shared = host_inputs(inputs)
    if "nc" not in _CACHE:
        _CACHE["nc"] = build()[0]
    nc = _CACHE["nc"]
    in_maps = []
    for i in range(n):
        m = dict(shared)
        m["x"] = np.ascontiguousarray(x[2 * i:2 * i + 2])
        in_maps.append(m)
    res = run_bass_kernel_spmd(nc, in_maps, core_ids=list(range(n)))
    out = np.concatenate([np.asarray(r["out"], dtype=np.float32) for r in res.results], axis=0)
    return out
```
